# Optimizing a Trainium2 kernel written in Bass

```python
import math
import numpy as np
import jax
import jax.numpy as jnp
from jax import lax

D_MODEL = 2048
BATCH = 4
SEQ = 4096
DEPTH = 2

D_MIX = D_MODEL
D_GDN = D_MIX // 2
D_DIFF = D_MIX - D_GDN
GDN_HEAD_DIM = 128
GDN_HEADS = D_GDN // GDN_HEAD_DIM
GDN_CONV = 5
GDN_CHUNK = 64
N_DIR = 2
DIFF_QK_DIM = 128
DIFF_V_DIM = 2 * DIFF_QK_DIM
DIFF_HEADS = D_DIFF // DIFF_V_DIM
Q_BLOCK = 128
ROPE_THETA = 10000.0
N_EXPERTS = 16
EC_CAPACITY = 2
D_EXPERT = D_MODEL
NORM_EPS = 1e-6
IN_SIZES = (D_GDN, D_GDN, D_GDN, D_GDN, N_DIR * GDN_HEADS, N_DIR * GDN_HEADS, D_DIFF, D_DIFF, D_DIFF)
N_IN = sum(IN_SIZES)
IN_SPLITS = tuple(int(v) for v in np.cumsum(IN_SIZES)[:-1])

kernel_name = 'hybrid_gdn_diffattn_ec_moe_encoder'

F32 = jnp.float32


def rms_norm(x, w):
    xf = x.astype(F32)
    y = xf * lax.rsqrt(jnp.mean(xf * xf, axis=-1, keepdims=True) + NORM_EPS)
    return (y * w.astype(F32)).astype(x.dtype)


def l2norm(t):
    tf = t.astype(F32)
    return tf * lax.rsqrt(jnp.sum(tf * tf, axis=-1, keepdims=True) + NORM_EPS)


def rope_tables(s):
    half = DIFF_QK_DIM // 2
    inv_freq = ROPE_THETA ** (-jnp.arange(half, dtype=F32) / half)
    ang = jnp.arange(s, dtype=F32)[:, None] * inv_freq[None, :]
    return jnp.cos(ang), jnp.sin(ang)


def rotary(t, cos, sin):
    half = t.shape[-1] // 2
    tf = t.astype(F32)
    t1, t2 = tf[..., :half], tf[..., half:]
    c, s_ = cos[None, :, None, :], sin[None, :, None, :]
    return jnp.concatenate([t1 * c - t2 * s_, t2 * c + t1 * s_], axis=-1).astype(t.dtype)


def centred_depthwise_conv(x, w):
    k, c = w.shape
    return lax.conv_general_dilated(
        x, w.reshape(k, 1, c).astype(x.dtype), window_strides=(1,),
        padding=[(k // 2, k // 2)], dimension_numbers=('NWC', 'WIO', 'NWC'),
        feature_group_count=c)


def gated_delta_chunked(q, k, v, beta, g):
    b, h, s, dk = q.shape
    dv = v.shape[-1]
    c = GDN_CHUNK
    n = s // c
    q, k, v = (t.astype(F32).reshape(b, h, n, c, -1) for t in (q, k, v))
    beta = beta.astype(F32).reshape(b, h, n, c)
    gc = jnp.cumsum(g.astype(F32).reshape(b, h, n, c), axis=-1)
    incl_lower = jnp.tril(jnp.ones((c, c), bool))
    strict_lower = jnp.tril(jnp.ones((c, c), bool), -1)
    gdiff = gc[..., :, None] - gc[..., None, :]
    decay = jnp.where(incl_lower, jnp.exp(jnp.where(incl_lower, gdiff, 0.0)), 0.0)
    kb = k * beta[..., None]
    lower = jnp.where(strict_lower, jnp.einsum('bhncd,bhnsd->bhncs', kb, k) * decay, 0.0)
    eye = jnp.broadcast_to(jnp.eye(c, dtype=F32), lower.shape)
    rhs = jnp.concatenate([v * beta[..., None], kb * jnp.exp(gc)[..., None]], axis=-1)
    sol = lax.linalg.triangular_solve(eye + lower, rhs, left_side=True, lower=True, unit_diagonal=True)
    u, w = sol[..., :dv], sol[..., dv:]
    qk = jnp.einsum('bhncd,bhnsd->bhncs', q, k) * decay
    qg = q * jnp.exp(gc)[..., None]
    kd = k * jnp.exp(gc[..., -1:] - gc)[..., None]
    g_last = jnp.exp(gc[..., -1])

    def step(state, xs):
        u_i, w_i, qk_i, qg_i, kd_i, gl_i = xs
        v_new = u_i - jnp.einsum('bhcd,bhde->bhce', w_i, state)
        o_i = jnp.einsum('bhcd,bhde->bhce', qg_i, state) + jnp.einsum('bhcs,bhse->bhce', qk_i, v_new)
        state = state * gl_i[..., None, None] + jnp.einsum('bhcd,bhce->bhde', kd_i, v_new)
        return state, o_i

    xs = tuple(jnp.moveaxis(t, 2, 0) for t in (u, w, qk, qg, kd, g_last))
    _, o = lax.scan(step, jnp.zeros((b, h, dk, dv), F32), xs)
    return jnp.moveaxis(o, 0, 2).reshape(b, h, s, dv)


def gdn_mixer(q, k, v, z, b_gate, a_gate, conv_w, a_log, dt_bias, norm_w):
    bsz, s, _ = q.shape
    qkv = jax.nn.silu(centred_depthwise_conv(jnp.concatenate([q, k, v], axis=-1), conv_w))
    q, k, v = jnp.split(qkv, 3, axis=-1)
    heads = lambda t: t.reshape(bsz, s, GDN_HEADS, GDN_HEAD_DIM).transpose(0, 2, 1, 3)
    q = l2norm(heads(q)) * (GDN_HEAD_DIM ** -0.5)
    k = l2norm(heads(k))
    v = heads(v)
    per_dir = lambda t: t.astype(F32).reshape(bsz, s, N_DIR, GDN_HEADS).transpose(2, 0, 3, 1)
    beta = jax.nn.sigmoid(per_dir(b_gate))
    g = -jnp.exp(a_log.astype(F32))[:, None, :, None] * jax.nn.softplus(
        per_dir(a_gate) + dt_bias.astype(F32)[:, None, :, None])
    o_fwd = gated_delta_chunked(q, k, v, beta[0], g[0])
    flip = lambda t: jnp.flip(t, axis=2)
    o_bwd = flip(gated_delta_chunked(flip(q), flip(k), flip(v), flip(beta[1]), flip(g[1])))
    o = (o_fwd + o_bwd).transpose(0, 2, 1, 3)
    zh = z.astype(F32).reshape(bsz, s, GDN_HEADS, GDN_HEAD_DIM)
    o = rms_norm(o, norm_w) * jax.nn.silu(zh)
    return o.reshape(bsz, s, D_GDN).astype(z.dtype)


def diff_attention(q, k, v, lam_vecs, subln_w, lambda_init, cos, sin):
    bsz, s, _ = q.shape

    def qk_heads(t):
        t = rotary(t.reshape(bsz, s, 2 * DIFF_HEADS, DIFF_QK_DIM), cos, sin)
        return t.reshape(bsz, s, DIFF_HEADS, 2, DIFF_QK_DIM).transpose(0, 2, 3, 1, 4)

    q = qk_heads(q) * (DIFF_QK_DIM ** -0.5)
    k = qk_heads(k)
    v = v.reshape(bsz, s, DIFF_HEADS, DIFF_V_DIM).transpose(0, 2, 1, 3)
    lf = lam_vecs.astype(F32)
    lam = jnp.exp(jnp.sum(lf[0] * lf[1])) - jnp.exp(jnp.sum(lf[2] * lf[3])) + lambda_init
    nb = s // Q_BLOCK
    qb = jnp.moveaxis(q.reshape(bsz, DIFF_HEADS, 2, nb, Q_BLOCK, DIFF_QK_DIM), 3, 0)

    def block(q_blk):
        scores = jnp.einsum('bhtqd,bhtkd->bhtqk', q_blk, k, preferred_element_type=F32)
        p = jax.nn.softmax(scores, axis=-1)
        wts = (p[:, :, 0] - lam * p[:, :, 1]).astype(v.dtype)
        return jnp.einsum('bhqk,bhkd->bhqd', wts, v)

    o = lax.map(block, qb)
    o = jnp.moveaxis(o, 0, 2).reshape(bsz, DIFF_HEADS, s, DIFF_V_DIM)
    o = rms_norm(o, subln_w) * (1.0 - lambda_init)
    return o.transpose(0, 2, 1, 3).reshape(bsz, s, D_DIFF)


def expert_choice_ffn(x, w_router, w_gate, w_up, w_down):
    bsz, s, d = x.shape
    cap = EC_CAPACITY * s // N_EXPERTS
    logits = jnp.einsum('bsd,de->bse', x, w_router, preferred_element_type=F32)
    aff = jax.nn.softmax(logits, axis=-1)
    gates, idx = lax.top_k(jnp.swapaxes(aff, 1, 2), cap)
    xs = jax.vmap(lambda xb, ib: xb[ib])(x, idx)
    hid = jax.nn.silu(jnp.einsum('becd,edf->becf', xs, w_gate)) * jnp.einsum('becd,edf->becf', xs, w_up)
    y = jnp.einsum('becf,efd->becd', hid, w_down) * gates[..., None].astype(x.dtype)
    return jax.vmap(lambda ib, yb: jnp.zeros((s, d), yb.dtype).at[ib.reshape(-1)].add(yb.reshape(-1, d)))(idx, y)


def setup_inputs(seed: int = 0) -> dict:
    key = jax.random.key(seed)
    ks = jax.random.split(key, 17)
    nrm = lambda k, shape, scale: jax.random.normal(k, shape, F32) * scale
    x = nrm(ks[0], (BATCH, SEQ, D_MODEL), 1.0)
    norm1_w = 1.0 + nrm(ks[1], (DEPTH, D_MODEL), 0.02)
    w_in = nrm(ks[2], (DEPTH, D_MODEL, N_IN), D_MODEL ** -0.5)
    conv_w = nrm(ks[3], (DEPTH, GDN_CONV, 3 * D_GDN), GDN_CONV ** -0.5)
    a_log = jnp.log(jax.random.uniform(ks[4], (DEPTH, N_DIR, GDN_HEADS), F32, 1.0, 16.0))
    dt = jnp.exp(jax.random.uniform(ks[5], (DEPTH, N_DIR, GDN_HEADS), F32, math.log(1e-3), math.log(1e-1)))
    dt_bias = dt + jnp.log(-jnp.expm1(-dt))
    gdn_norm_w = 1.0 + nrm(ks[6], (DEPTH, GDN_HEAD_DIM), 0.02)
    diff_lambda = nrm(ks[7], (DEPTH, 4, DIFF_QK_DIM), 0.1)
    diff_subln_w = 1.0 + nrm(ks[8], (DEPTH, DIFF_V_DIM), 0.02)
    w_out = nrm(ks[9], (DEPTH, D_MIX, D_MODEL), D_MIX ** -0.5)
    norm2_w = 1.0 + nrm(ks[10], (DEPTH, D_MODEL), 0.02)
    w_router = nrm(ks[11], (DEPTH, D_MODEL, N_EXPERTS), D_MODEL ** -0.5)
    w_gate = nrm(ks[12], (DEPTH, N_EXPERTS, D_MODEL, D_EXPERT), D_MODEL ** -0.5)
    w_up = nrm(ks[13], (DEPTH, N_EXPERTS, D_MODEL, D_EXPERT), D_MODEL ** -0.5)
    w_down = nrm(ks[14], (DEPTH, N_EXPERTS, D_EXPERT, D_MODEL), D_EXPERT ** -0.5)
    final_norm_w = 1.0 + nrm(ks[15], (D_MODEL,), 0.02)
    return {'x': x, 'norm1_w': norm1_w, 'w_in': w_in, 'conv_w': conv_w, 'a_log': a_log,
            'dt_bias': dt_bias, 'gdn_norm_w': gdn_norm_w, 'diff_lambda': diff_lambda,
            'diff_subln_w': diff_subln_w, 'w_out': w_out, 'norm2_w': norm2_w,
            'w_router': w_router, 'w_gate': w_gate, 'w_up': w_up, 'w_down': w_down,
            'final_norm_w': final_norm_w}


def reference(x, norm1_w, w_in, conv_w, a_log, dt_bias, gdn_norm_w, diff_lambda,
              diff_subln_w, w_out, norm2_w, w_router, w_gate, w_up, w_down, final_norm_w):
    cos, sin = rope_tables(x.shape[1])
    for l in range(DEPTH):
        n = rms_norm(x, norm1_w[l])
        proj = jnp.einsum('bsd,dn->bsn', n, w_in[l])
        qa, ka, va, za, ba, aa, qb, kb, vb = jnp.split(proj, IN_SPLITS, axis=-1)
        out_a = gdn_mixer(qa, ka, va, za, ba, aa, conv_w[l], a_log[l], dt_bias[l], gdn_norm_w[l])
        lambda_init = 0.8 - 0.6 * math.exp(-0.3 * l)
        out_b = diff_attention(qb, kb, vb, diff_lambda[l], diff_subln_w[l], lambda_init, cos, sin)
        mixed = jnp.concatenate([out_a, out_b], axis=-1)
        x = x + jnp.einsum('bsm,md->bsd', mixed, w_out[l])
        x = x + expert_choice_ffn(rms_norm(x, norm2_w[l]), w_router[l], w_gate[l], w_up[l], w_down[l])
    return rms_norm(x, final_norm_w)
```

```python
import contextlib
import math
import numpy as np
import ml_dtypes
import concourse.bass as bass
import concourse.mybir as mybir
from concourse.bass_utils import run_bass_kernel_spmd

F32 = mybir.dt.float32
BF16 = mybir.dt.bfloat16
I32 = mybir.dt.int32
AF = mybir.ActivationFunctionType
ALU = mybir.AluOpType
AX = mybir.AxisListType

S = 4096
D = 2048
KC = 16
NT = 32
DEPTH = 2
EPS = 1e-6
NCOL = 3600
GH = 4
DH = 2
NE = 8
CAP = 512


class Res:
    __slots__ = ("name", "w", "r", "dsem", "dname", "dcnt")

    def __init__(self, name):
        self.name = name
        self.w = None
        self.r = {}
        self.dsem = None
        self.dname = None
        self.dcnt = 0


class Tracker:
    def __init__(self, nc, es):
        self.nc = nc
        self.es = es
        self.eng = {"pe": nc.tensor, "dve": nc.vector, "act": nc.scalar, "pool": nc.gpsimd, "sp": nc.sync}
        self.sems = {}
        self.cnt = {}
        for k in ("pe", "dve", "act", "pool"):
            self.sems[k] = es.enter_context(nc.semaphore("e_" + k))
            self.cnt[k] = 0
        self.waited = {k: {} for k in self.eng}
        self.dcount = {}
        self.dfree = []
        self.ninst = 0

    def res(self, name):
        return Res(name)

    def _need(self, need, en, tk, raw):
        sname, val, owner = tk
        if owner == en and (en == "pe" or not raw):
            return
        if self.waited[en].get(sname, 0) >= val:
            return
        if need.get(sname, 0) < val:
            need[sname] = val

    def _emit(self, en, need):
        for sname, val in need.items():
            self.waited[en][sname] = val
            self.eng[en].wait_ge(self.sems[sname], val)
            self.ninst += 1

    def _wait(self, en, tk, raw):
        need = {}
        self._need(need, en, tk, raw)
        self._emit(en, need)

    def _deps(self, en, R, W, nowaw=False):
        need = {}
        for r in R:
            if r.w is not None:
                self._need(need, en, r.w, True)
        for w in W:
            if w.w is not None and not (nowaw and w.w[2] is None):
                self._need(need, en, w.w, False)
            for tk in w.r.values():
                self._need(need, en, tk, False)
        self._emit(en, need)

    def op(self, en, fn, R=(), W=()):
        self._deps(en, R, W)
        inst = fn(self.eng[en])
        self.cnt[en] += 1
        inst.then_inc(self.sems[en], 1)
        self.ninst += 1
        tk = (en, self.cnt[en], en)
        for r in R:
            r.r[en] = tk
        for w in W:
            w.w = tk
            w.r = {}
        return tk

    def dma(self, q, out, in_, R=(), W=(), nowaw=True, **kw):
        self._deps(q, R, W, nowaw=nowaw)
        inst = self.eng[q].dma_start(out=out, in_=in_, **kw)
        self._dma_done(inst, R, W)

    def _dsem(self, w):
        if w.dsem is None:
            if self.dfree:
                w.dname = self.dfree.pop()
            else:
                w.dname = "d_%d" % len(self.dcount)
                self.sems[w.dname] = self.es.enter_context(self.nc.semaphore(w.dname))
                self.dcount[w.dname] = 0
            w.dsem = self.sems[w.dname]

    def release(self, w):
        if w.dsem is not None:
            self.dfree.append(w.dname)
            w.dsem = None
            w.dname = None

    def _dma_done(self, inst, R, W, inc=16):
        w = W[0]
        self._dsem(w)
        self.dcount[w.dname] += inc
        if inc == 16:
            inst.then_inc(w.dsem, 16)
        else:
            inst.then_inc(w.dsem)
        self.ninst += 1
        tk = (w.dname, self.dcount[w.dname], None)
        for r in R:
            r.r[w.dname] = tk
        for ww in W:
            ww.w = tk
            ww.r = {}

    def barrier(self):
        for en in self.eng:
            need = {}
            for k in ("pe", "dve", "act", "pool"):
                if k != en and self.cnt[k] > 0:
                    self._need(need, en, (k, self.cnt[k], k), True)
            for nm, c in self.dcount.items():
                if c > 0:
                    self._need(need, en, (nm, c, None), True)
            self._emit(en, need)


def bf(a):
    return np.ascontiguousarray(a.astype(ml_dtypes.bfloat16))


def host_consts():
    c = {}
    c["ident_bf"] = bf(np.eye(128, dtype=np.float32))
    c["ident_f"] = np.eye(128, dtype=np.float32)
    rot = np.zeros((128, 128), np.float32)
    for m in range(64):
        rot[m + 64, m] = -1.0
        rot[m, m + 64] = 1.0
    c["rotT"] = bf(rot)
    c["ones_bf"] = bf(np.ones((128, 128), np.float32))
    half = 64
    inv_freq = (np.float32(10000.0) ** (-(np.arange(half, dtype=np.float32) / np.float32(half)))).astype(np.float32)
    ang = (np.arange(S, dtype=np.float32)[None, :] * inv_freq[:, None]).astype(np.float32)
    c["cos2"] = np.ascontiguousarray(np.concatenate([np.cos(ang), np.cos(ang)], 0).astype(np.float32))
    c["sin2"] = np.ascontiguousarray(np.concatenate([np.sin(ang), np.sin(ang)], 0).astype(np.float32))
    s = np.arange(128)[:, None]
    cc = np.arange(128)[None, :]
    same = (s // 64) == (cc // 64)
    m = np.zeros((9, 128, 128), np.float32)
    m[0] = same & (s < cc)
    m[1] = same & (s <= cc)
    m[2] = same & (s > cc)
    m[3] = same & (s >= cc)
    m[4] = same
    m[5] = (s < 64) & (cc >= 0)
    m[6] = (s >= 64) & (cc >= 0)
    m[7] = 1.0
    m[8] = (s < cc)
    c["masks"] = np.ascontiguousarray(m.transpose(1, 0, 2))
    c["iota"] = np.ascontiguousarray(np.tile(np.arange(512, dtype=np.float32)[None, :], (128, 1)))
    return c


def core_inputs(inputs, b, hf, consts):
    d = dict(consts)
    d["x"] = np.ascontiguousarray(inputs["x"][b])
    g0 = 512 * hf
    for l in range(DEPTH):
        w = inputs["w_in"][l]
        base = [0, 1024, 2048, 3072]
        cols = []
        cols += list(range(0 + g0, 0 + g0 + 512))
        cols += list(range(1024 + g0, 1024 + g0 + 512))
        cols += list(range(2048 + g0, 2048 + g0 + 512))
        dq0 = 4096 + 32
        cols += list(range(dq0 + g0, dq0 + g0 + 512))
        cols += list(range(dq0 + 1024 + g0, dq0 + 1024 + g0 + 512))
        cols += list(range(3072 + g0, 3072 + g0 + 512))
        cols += list(range(dq0 + 2048 + g0, dq0 + 2048 + g0 + 512))
        hb = [4096 + dd * 8 + 4 * hf + h for dd in range(2) for h in range(4)]
        ha = [4096 + 16 + dd * 8 + 4 * hf + h for dd in range(2) for h in range(4)]
        cols += hb + ha
        d["w_in%d" % l] = np.ascontiguousarray(w[:, cols])
        cw = inputs["conv_w"][l]
        ccols = []
        for grp in range(3):
            ccols += list(range(grp * 1024 + g0, grp * 1024 + g0 + 512))
        cws = cw[:, ccols]
        d["conv%d" % l] = np.ascontiguousarray(cws.reshape(5, 12, 128).transpose(2, 1, 0))
        al = inputs["a_log"][l][:, 4 * hf:4 * hf + 4].reshape(8, 1)
        dtb = inputs["dt_bias"][l][:, 4 * hf:4 * hf + 4].reshape(8, 1)
        d["alog%d" % l] = np.ascontiguousarray(al)
        d["dtb%d" % l] = np.ascontiguousarray(dtb)
        d["n1w%d" % l] = np.ascontiguousarray(inputs["norm1_w"][l].reshape(1, D))
        d["dlam%d" % l] = np.ascontiguousarray(inputs["diff_lambda"][l].reshape(1, 512))
        d["subw%d" % l] = np.ascontiguousarray(inputs["diff_subln_w"][l].reshape(1, 256))
        d["gnw%d" % l] = np.ascontiguousarray(inputs["gdn_norm_w"][l].reshape(1, 128))
        rows = list(range(0, 512)) + list(range(1024, 1536)) + list(range(512, 1024)) + list(range(1536, 2048))
        d["w_out%d" % l] = np.ascontiguousarray(inputs["w_out"][l][rows, :])
        ecols = list(range(8 * hf, 8 * hf + 8)) + list(range(8 * (1 - hf), 8 * (1 - hf) + 8))
        d["w_r%d" % l] = np.ascontiguousarray(inputs["w_router"][l][:, ecols])
        d["n2w%d" % l] = np.ascontiguousarray(inputs["norm2_w"][l].reshape(1, D))
        d["w_g%d" % l] = np.ascontiguousarray(inputs["w_gate"][l][8 * hf:8 * hf + 8])
        d["w_u%d" % l] = np.ascontiguousarray(inputs["w_up"][l][8 * hf:8 * hf + 8])
        d["w_d%d" % l] = np.ascontiguousarray(inputs["w_down"][l][8 * hf:8 * hf + 8])
    d["fnw"] = np.ascontiguousarray(inputs["final_norm_w"].reshape(1, D))
    return d


class Prog:
    def __init__(self, test_outputs=(), test_inputs=()):
        self.nc = bass.Bass("TRN2", target_bir_lowering=False)
        self.es = contextlib.ExitStack()
        self.T = Tracker(self.nc, self.es)
        self.test_outputs = set(test_outputs)
        self.test_inputs = set(test_inputs)
        self.dram = {}
        self.dres = {}

    def din(self, name, shape, dt):
        t = self.nc.dram_tensor(name, list(shape), dt, kind="ExternalInput").ap()
        self.dram[name] = t
        self.dres[name] = Res(name)
        return t

    def dscr(self, name, shape, dt, out=False):
        kind = "ExternalOutput" if (out or name in self.test_outputs) else "Internal"
        if name in self.test_inputs:
            kind = "ExternalInput"
        t = self.nc.dram_tensor(name, list(shape), dt, kind=kind).ap()
        self.dram[name] = t
        self.dres[name] = Res(name)
        return t


class Pool_:
    def __init__(self, P):
        self.P = P
        self.es = contextlib.ExitStack()
        self.created = []

    def __enter__(self):
        self.es.__enter__()
        return self

    def __exit__(self, *a):
        if a[0] is None:
            self.P.T.barrier()
            for r in self.created:
                self.P.T.release(r)
        return self.es.__exit__(*a)

    _uid = [0]

    def sb(self, name, shape, dt):
        Pool_._uid[0] += 1
        name = "%s_u%d" % (name, Pool_._uid[0])
        t = self.es.enter_context(self.P.nc.sbuf_tensor(name, list(shape), dt))
        r = Res(name)
        self.created.append(r)
        return t, r

    def ps(self, name, shape, dt):
        Pool_._uid[0] += 1
        name = "%s_u%d" % (name, Pool_._uid[0])
        t = self.es.enter_context(self.P.nc.psum_tensor(name, list(shape), dt))
        return t, Res(name)


def declare_io(P, layers=range(DEPTH)):
    P.din("x", [S, D], F32)
    P.din("ident_bf", [128, 128], BF16)
    P.din("ident_f", [128, 128], F32)
    P.din("rotT", [128, 128], BF16)
    P.din("ones_bf", [128, 128], BF16)
    P.din("cos2", [128, S], F32)
    P.din("sin2", [128, S], F32)
    P.din("masks", [128, 9, 128], F32)
    P.din("iota", [128, 512], F32)
    P.din("fnw", [1, D], F32)
    for l in layers:
        P.din("w_in%d" % l, [D, NCOL], F32)
        P.din("conv%d" % l, [128, 12, 5], F32)
        P.din("alog%d" % l, [8, 1], F32)
        P.din("dtb%d" % l, [8, 1], F32)
        P.din("n1w%d" % l, [1, D], F32)
        P.din("dlam%d" % l, [1, 512], F32)
        P.din("subw%d" % l, [1, 256], F32)
        P.din("gnw%d" % l, [1, 128], F32)
        P.din("w_out%d" % l, [D, D], F32)
        P.din("w_r%d" % l, [D, 16], F32)
        P.din("n2w%d" % l, [1, D], F32)
        P.din("w_g%d" % l, [NE, D, D], F32)
        P.din("w_u%d" % l, [NE, D, D], F32)
        P.din("w_d%d" % l, [NE, D, D], F32)


def declare_scratch(P):
    P.dscr("gqT", [GH, 128, S], BF16)
    P.dscr("gkT", [GH, 128, S], BF16)
    P.dscr("gk_tm", [GH, S, 128], BF16)
    P.dscr("gv_tm", [GH, S, 128], BF16)
    P.dscr("dqT", [4, 128, S], BF16)
    P.dscr("dkT", [4, 128, S], BF16)
    P.dscr("siluz", [S, 512], BF16)
    P.dscr("dv", [S, 512], BF16)
    P.dscr("gsc", [6, 128, NT * 8], F32)
    P.dscr("mixed_half", [S, 1024], BF16)
    P.dscr("mg", [4, 2, 1024, 1024], BF16)
    P.dscr("x1", [S, D], F32)
    P.dscr("x2", [S, D], BF16)
    P.dscr("affd", [128, NT * 16], F32)
    P.dscr("ypart", [S, D], BF16)
    P.dscr("yg", [8, 2, 512, D], BF16)
    P.dscr("xa", [S, D], F32)
    P.dscr("xb", [S, D], F32)
    P.dscr("out", [S, D], F32, out=True)


def load_consts(P, pool):
    T = P.T
    C = {}
    for nm, shape, dt in (("ident_bf", [128, 128], BF16), ("ident_f", [128, 128], F32), ("rotT", [128, 128], BF16),
                          ("ones_bf", [128, 128], BF16), ("masks", [128, 9, 128], F32)):
        t, r = pool.sb("c_" + nm, shape, dt)
        T.dma("sp", t[:], P.dram[nm], R=[P.dres[nm]], W=[r])
        C[nm] = (t, r)
    for nm, val in (("epsc", EPS), ("onec", 1.0)):
        t, r = pool.sb("c_" + nm, [128, 1], F32)
        T.op("dve", lambda e: e.memset(t[:], val), W=[r])
        C[nm] = (t, r)
    return C


def phase1(P, C, l, xsrc, upto=None, sections=("gdn", "rope", "tm", "bd"), dbg=None):
    T = P.T
    nc = P.nc
    ident_bf, r_ident_bf = C["ident_bf"]
    ident_f, r_ident_f = C["ident_f"]
    rotT, r_rotT = C["rotT"]
    ones_bf, r_ones = C["ones_bf"]
    masks, r_masks = C["masks"]
    epsc, r_epsc = C["epsc"]
    onec, r_onec = C["onec"]
    xd = P.dram[xsrc]
    xr = P.dres[xsrc]
    wd = P.dram["w_in%d" % l].rearrange("(kc p) n -> p kc n", p=128)
    wr = P.dres["w_in%d" % l]
    with Pool_(P) as pl:
        nT, r_nT = pl.sb("nT", [128, KC, S], BF16)
        with Pool_(P) as pa:
            w1b, r_w1b = pa.sb("w1b", [128, D], F32)
            T.dma("sp", w1b[:], P.dram["n1w%d" % l].partition_broadcast(128), R=[P.dres["n1w%d" % l]], W=[r_w1b])
            xt = [pa.sb("xt%d" % i, [128, D], F32) for i in range(2)]
            nt = [pa.sb("nt%d" % i, [128, D], BF16) for i in range(2)]
            junk, r_junk = pa.sb("junk", [128, D], BF16)
            ss = [pa.sb("ss%d" % i, [128, 1], F32) for i in range(2)]
            rstd = [pa.sb("rstd%d" % i, [128, 1], F32) for i in range(2)]
            ptr = [pa.ps("ptr%d" % i, [128, 8, 128], BF16) for i in range(2)]
            for tt in range(NT):
                i = tt % 2
                x_t, x_r = xt[i]
                n_t, n_r = nt[i]
                T.dma("sp", x_t[:], xd[tt * 128:(tt + 1) * 128, :], R=[xr], W=[x_r])
                T.op("dve", lambda e: e.memset(ss[i][0][:], 0.0), W=[ss[i][1]])
                T.op("act", lambda e: e.activation(out=junk[:], in_=x_t[:], func=AF.Square, accum_out=ss[i][0][:]),
                     R=[x_r], W=[r_junk, ss[i][1]])
                T.op("act", lambda e: e.activation(out=rstd[i][0][:], in_=ss[i][0][:], func=AF.Sqrt, bias=epsc[:],
                                                   scale=1.0 / D), R=[ss[i][1], r_epsc], W=[rstd[i][1]])
                T.op("dve", lambda e: e.reciprocal(out=rstd[i][0][:], in_=rstd[i][0][:]), R=[rstd[i][1]], W=[rstd[i][1]])
                T.op("dve", lambda e: e.scalar_tensor_tensor(out=n_t[:], in0=x_t[:], scalar=rstd[i][0][:], in1=w1b[:],
                                                             op0=ALU.mult, op1=ALU.mult),
                     R=[x_r, rstd[i][1], r_w1b], W=[n_r])
                for hh in range(2):
                    p_t, p_r = ptr[hh]
                    for k in range(8):
                        kc = hh * 8 + k
                        T.op("pe", lambda e: e.transpose(out=p_t[:, k, :], in_=n_t[:, kc * 128:(kc + 1) * 128],
                                                         identity=ident_bf[:]),
                             R=[n_r, r_ident_bf], W=[p_r])
                    en = "act" if hh == 0 else "dve"
                    if en == "act":
                        T.op("act", lambda e: e.copy(out=nT[:, hh * 8:(hh + 1) * 8, tt * 128:(tt + 1) * 128], in_=p_t[:]),
                             R=[p_r], W=[r_nT])
                    else:
                        T.op("dve", lambda e: e.tensor_copy(out=nT[:, hh * 8:(hh + 1) * 8, tt * 128:(tt + 1) * 128],
                                                            in_=p_t[:]), R=[p_r], W=[r_nT])
        if upto == "1a":
            if "dbg_nT" in P.dram:
                T.dma("sp", P.dram["dbg_nT"], nT[:], R=[r_nT], W=[P.dres["dbg_nT"]])
            return
        with Pool_(P) as pb:
            wblk = [pb.sb("wblk%d" % i, [128, KC, 256], BF16) for i in range(2)]
            raw = [pb.sb("raw%d" % i, [128, S + 4], BF16) for i in range(2)]
            acc = [pb.sb("acc%d" % i, [128, 1024], F32) for i in range(2)]
            sl = [pb.sb("sl%d" % i, [128, 1024], F32) for i in range(2)]
            sq = [pb.sb("sq%d" % i, [128, 1024], BF16) for i in range(1)] * 2
            rn = [pb.sb("rn%d" % i, [128, 1024], F32) for i in range(1)] * 2
            ob = [pb.sb("ob%d" % i, [128, 1024], BF16) for i in range(2)]
            tm = [pb.sb("tm%d" % i, [128, 8, 128], BF16) for i in range(1)] * 2
            cst = [pb.sb("cst%d" % i, [128, 512], F32) for i in range(1)] * 2
            snt = [pb.sb("snt%d" % i, [128, 512], F32) for i in range(1)] * 2
            tbf = [pb.sb("tbf%d" % i, [128, 512], BF16) for i in range(1)] * 2
            ra = [pb.sb("ra%d" % i, [128, 512], F32) for i in range(1)] * 2
            rb = [pb.sb("rb%d" % i, [128, 512], F32) for i in range(1)] * 2
            convw, r_convw = pb.sb("convw", [128, 12, 5], F32)
            T.dma("sp", convw[:], P.dram["conv%d" % l], R=[P.dres["conv%d" % l]], W=[r_convw])
            pacc = [pb.ps("pacc%d" % i, [128, 512], F32) for i in range(2)]
            prot = [pb.ps("prot%d" % i, [128, 512], F32) for i in range(1)]
            pss = [pb.ps("pss%d" % i, [128, 1024], F32) for i in range(1)]
            ptm = [pb.ps("ptm%d" % i, [128, 8, 128], BF16) for i in range(2)]
            for i in range(2):
                T.op("dve", lambda e: e.memset(raw[i][0][:], 0.0), W=[raw[i][1]])
            nblk = [0]

            def load_w(c0, ncols):
                i = nblk[0] % 2
                nblk[0] += 1
                w_t, w_r = wblk[i]
                for q4 in range(4):
                    T.dma("pool", w_t[:, q4 * 4:(q4 + 1) * 4, 0:ncols], wd[:, q4 * 4:(q4 + 1) * 4, c0:c0 + ncols],
                          R=[wr], W=[w_r])
                return w_t, w_r

            cnt = {"pacc": 0, "ptm": 0, "st": 0, "rp": 0}

            def proj_fm(w_t, w_r, cw, tb, m=128):
                i = cnt["pacc"] % 2
                cnt["pacc"] += 1
                p_t, p_r = pacc[i]
                for kc in range(KC):
                    T.op("pe", lambda e: e.matmul(p_t[0:m, :], lhsT=w_t[:, kc, cw:cw + m],
                                                  rhs=nT[:, kc, tb * 512:(tb + 1) * 512],
                                                  start=(kc == 0), stop=(kc == KC - 1)),
                         R=[w_r, r_nT], W=[p_r])
                return p_t, p_r

            for j in (range(12) if "gdn" in sections else ()):
                grp, h = j // 4, j % 4
                if j % 2 == 0:
                    w_t, w_r = load_w(j * 128, 256)
                cw = (j % 2) * 128
                r_t, r_r = raw[j % 2]
                for tb in range(8):
                    p_t, p_r = proj_fm(w_t, w_r, cw, tb)
                    T.op("act", lambda e: e.copy(out=r_t[:, 2 + tb * 512:2 + (tb + 1) * 512], in_=p_t[:]),
                         R=[p_r], W=[r_r])
                if j == 0 and "dbg_raw" in P.dram:
                    T.dma("sp", P.dram["dbg_raw"], r_t[:], R=[r_r], W=[P.dres["dbg_raw"]])
                    return
                for blk in range(4):
                    si = cnt["st"] % 2
                    cnt["st"] += 1
                    a_t, a_r = acc[si]
                    s_t, s_r = sl[si]
                    o_t, o_r = ob[si]
                    t0 = blk * 1024
                    T.op("dve", lambda e: e.tensor_scalar(out=a_t[:], in0=r_t[:, t0:t0 + 1024], scalar1=convw[:, j, 0:1],
                                                          scalar2=None, op0=ALU.mult), R=[r_r, r_convw], W=[a_r])
                    for k in range(1, 5):
                        T.op("dve", lambda e: e.scalar_tensor_tensor(out=a_t[:], in0=r_t[:, t0 + k:t0 + k + 1024],
                                                                     scalar=convw[:, j, k:k + 1], in1=a_t[:],
                                                                     op0=ALU.mult, op1=ALU.add),
                             R=[r_r, r_convw, a_r], W=[a_r])
                    if grp == 2:
                        T.op("act", lambda e: e.activation(out=o_t[:], in_=a_t[:], func=AF.Silu), R=[a_r], W=[o_r])
                    else:
                        q_t, q_r = sq[si]
                        n_t, n_r = rn[si]
                        T.op("act", lambda e: e.activation(out=s_t[:], in_=a_t[:], func=AF.Silu), R=[a_r], W=[s_r])
                        T.op("act", lambda e: e.activation(out=q_t[:], in_=s_t[:], func=AF.Square), R=[s_r], W=[q_r])
                        ps_t, ps_r = pss[0]
                        for hh in range(2):
                            T.op("pe", lambda e: e.matmul(ps_t[:, hh * 512:(hh + 1) * 512], lhsT=ones_bf[:],
                                                          rhs=q_t[:, hh * 512:(hh + 1) * 512], start=True, stop=True),
                                 R=[r_ones, q_r], W=[ps_r])
                        T.op("act", lambda e: e.activation(out=n_t[:], in_=ps_t[:], func=AF.Sqrt, bias=epsc[:]),
                             R=[ps_r, r_epsc], W=[n_r])
                        T.op("dve", lambda e: e.reciprocal(out=n_t[:], in_=n_t[:]), R=[n_r], W=[n_r])
                        qs = (128.0 ** -0.5) if grp == 0 else 1.0
                        T.op("dve", lambda e: e.scalar_tensor_tensor(out=o_t[:], in0=s_t[:], scalar=qs, in1=n_t[:],
                                                                     op0=ALU.mult, op1=ALU.mult),
                             R=[s_r, n_r], W=[o_r])
                    if grp < 2:
                        dst = P.dram["gqT" if grp == 0 else "gkT"]
                        T.dma("sp", dst[h, :, t0:t0 + 1024], o_t[:], R=[o_r],
                              W=[P.dres["gqT" if grp == 0 else "gkT"]])
                    if grp >= 1:
                        pi = cnt["ptm"] % 2
                        cnt["ptm"] += 1
                        pt_t, pt_r = ptm[pi]
                        tm_t, tm_r = tm[pi]
                        for k in range(8):
                            T.op("pe", lambda e: e.transpose(out=pt_t[:, k, :], in_=o_t[:, k * 128:(k + 1) * 128],
                                                             identity=ident_bf[:]), R=[o_r, r_ident_bf], W=[pt_r])
                        T.op("act", lambda e: e.copy(out=tm_t[:], in_=pt_t[:]), R=[pt_r], W=[tm_r])
                        nm = "gk_tm" if grp == 1 else "gv_tm"
                        T.dma("sp", P.dram[nm][h, t0:t0 + 1024, :].rearrange("(t p) d -> p t d", p=128), tm_t[:],
                              R=[tm_r], W=[P.dres[nm]])
            if upto == "gdn":
                return
            for j in (dbg["rope_j"] if dbg else (range(12, 20) if "rope" in sections else ())):
                jj = j - 12
                isq, cj = (jj < 4), jj % 4
                if j % 2 == 0:
                    w_t, w_r = load_w(j * 128, 256)
                cw = (j % 2) * 128
                nm = "dqT" if isq else "dkT"
                for tb in (dbg["rope_tb"] if dbg else range(8)):
                    i = cnt["rp"] % 2
                    cnt["rp"] += 1
                    c_t, c_r = cst[i]
                    s_t, s_r = snt[i]
                    sk = dbg.get("skip", "") if dbg else ""
                    if "c" in sk:
                        T.op("dve", lambda e: e.memset(c_t[:], 1.0), W=[c_r])
                        T.op("dve", lambda e: e.memset(s_t[:], 0.0), W=[s_r])
                    else:
                        T.dma("sp", c_t[:], P.dram["cos2"][:, tb * 512:(tb + 1) * 512], R=[P.dres["cos2"]], W=[c_r])
                        T.dma("sp", s_t[:], P.dram["sin2"][:, tb * 512:(tb + 1) * 512], R=[P.dres["sin2"]], W=[s_r])
                    p_t, p_r = proj_fm(w_t, w_r, cw, tb)
                    b_t, b_r = tbf[i]
                    T.op("act", lambda e: e.copy(out=b_t[:], in_=p_t[:]), R=[p_r], W=[b_r])
                    pr_t, pr_r = prot[0]
                    if "r" in sk:
                        T.op("pe", lambda e: e.matmul(pr_t[:], lhsT=ones_bf[:], rhs=b_t[:], start=True, stop=True),
                             R=[r_ones, b_r], W=[pr_r])
                    else:
                        T.op("pe", lambda e: e.matmul(pr_t[:], lhsT=rotT[:], rhs=b_t[:], start=True, stop=True),
                             R=[r_rotT, b_r], W=[pr_r])
                    a_t, a_r = ra[i]
                    bb_t, bb_r = rb[i]
                    o_t, o_r = ob[i]
                    if "m" in sk:
                        T.op("act", lambda e: e.copy(out=o_t[:, 0:512], in_=pr_t[:]), R=[pr_r], W=[o_r])
                    elif "1" in sk:
                        T.op("dve", lambda e: e.tensor_mul(out=a_t[:], in0=p_t[:], in1=c_t[:]), R=[p_r, c_r], W=[a_r])
                        T.op("act", lambda e: e.copy(out=o_t[:, 0:512], in_=a_t[:]), R=[a_r], W=[o_r])
                    elif "3" in sk:
                        T.op("dve", lambda e: e.tensor_mul(out=a_t[:], in0=c_t[:], in1=c_t[:]), R=[c_r], W=[a_r])
                        T.op("act", lambda e: e.copy(out=o_t[:, 0:512], in_=a_t[:]), R=[a_r], W=[o_r])
                    elif "2" in sk:
                        T.op("dve", lambda e: e.tensor_mul(out=a_t[:], in0=p_t[:], in1=c_t[:]), R=[p_r, c_r], W=[a_r])
                        T.op("dve", lambda e: e.tensor_mul(out=bb_t[:], in0=pr_t[:], in1=s_t[:]), R=[pr_r, s_r], W=[bb_r])
                        T.op("act", lambda e: e.copy(out=o_t[:, 0:512], in_=bb_t[:]), R=[a_r, bb_r], W=[o_r])
                    else:
                        T.op("act", lambda e: e.copy(out=a_t[:], in_=p_t[:]), R=[p_r], W=[a_r])
                        T.op("act", lambda e: e.copy(out=bb_t[:], in_=pr_t[:]), R=[pr_r], W=[bb_r])
                        T.op("dve", lambda e: e.tensor_mul(out=a_t[:], in0=a_t[:], in1=c_t[:]),
                             R=[a_r, c_r], W=[a_r])
                        T.op("dve", lambda e: e.tensor_mul(out=bb_t[:], in0=bb_t[:], in1=s_t[:]),
                             R=[bb_r, s_r], W=[bb_r])
                        T.op("dve", lambda e: e.tensor_add(out=o_t[:, 0:512], in0=a_t[:], in1=bb_t[:]),
                             R=[a_r, bb_r], W=[o_r])
                    T.dma("sp", P.dram[nm][cj, :, tb * 512:(tb + 1) * 512], o_t[:, 0:512], R=[o_r], W=[P.dres[nm]])
            if upto == "rope":
                return
            for g in range(2):
                nm = "siluz" if g == 0 else "dv"
                for cb in range(2):
                    c0 = 2560 + g * 512 + cb * 256
                    w_t, w_r = load_w(c0, 256)
                    for tt in range(NT):
                        i = cnt["pacc"] % 2
                        cnt["pacc"] += 1
                        p_t, p_r = pacc[i]
                        for kc in range(KC):
                            T.op("pe", lambda e: e.matmul(p_t[:, 0:256], lhsT=nT[:, kc, tt * 128:(tt + 1) * 128],
                                                          rhs=w_t[:, kc, 0:256], start=(kc == 0), stop=(kc == KC - 1)),
                                 R=[w_r, r_nT], W=[p_r])
                        si = cnt["st"] % 2
                        cnt["st"] += 1
                        o_t, o_r = ob[si]
                        T.op("act", lambda e: e.activation(out=o_t[:, 0:256], in_=p_t[:, 0:256],
                                                           func=(AF.Silu if g == 0 else AF.Copy)), R=[p_r], W=[o_r])
                        T.dma("sp", P.dram[nm][tt * 128:(tt + 1) * 128, cb * 256:(cb + 1) * 256], o_t[:, 0:256],
                              R=[o_r], W=[P.dres[nm]])
        if upto == "tm":
            return
        with Pool_(P) as pg:
            w_t, w_r = pg.sb("wsm", [128, KC, 16], BF16)
            T.dma("pool", w_t[:], wd[:, :, 3584:3600], R=[wr], W=[w_r])
            pacc2 = [pg.ps("pacc2_%d" % i, [128, 512], F32) for i in range(2)]
            psm = [pg.ps("psm0", [128, 512], F32)]
            pcnt = [0]

            def proj_fm(w_t, w_r, cw, tb, m=128):
                i = pcnt[0] % 2
                pcnt[0] += 1
                p_t, p_r = pacc2[i]
                for kc in range(KC):
                    T.op("pe", lambda e: e.matmul(p_t[0:m, :], lhsT=w_t[:, kc, cw:cw + m],
                                                  rhs=nT[:, kc, tb * 512:(tb + 1) * 512],
                                                  start=(kc == 0), stop=(kc == KC - 1)),
                         R=[w_r, r_nT], W=[p_r])
                return p_t, p_r
            bfm, r_bfm = pg.sb("bfm", [8, S], F32)
            gfm, r_gfm = pg.sb("gfm", [8, S], F32)
            alog, r_alog = pg.sb("alog", [8, 1], F32)
            dtb, r_dtb = pg.sb("dtb", [8, 1], F32)
            nega, r_nega = pg.sb("nega", [8, 1], F32)
            T.dma("sp", alog[:], P.dram["alog%d" % l], R=[P.dres["alog%d" % l]], W=[r_alog])
            T.dma("sp", dtb[:], P.dram["dtb%d" % l], R=[P.dres["dtb%d" % l]], W=[r_dtb])
            T.op("act", lambda e: e.activation(out=nega[:], in_=alog[:], func=AF.Exp), R=[r_alog], W=[r_nega])
            T.op("dve", lambda e: e.tensor_scalar(out=nega[:], in0=nega[:], scalar1=-1.0, scalar2=None, op0=ALU.mult),
                 R=[r_nega], W=[r_nega])
            for tb in range(8):
                p_t, p_r = proj_fm(w_t, w_r, 0, tb, m=8)
                T.op("act", lambda e: e.activation(out=bfm[:, tb * 512:(tb + 1) * 512], in_=p_t[0:8, :],
                                                   func=AF.Sigmoid), R=[p_r], W=[r_bfm])
            for tb in range(8):
                p_t, p_r = proj_fm(w_t, w_r, 8, tb, m=8)
                T.op("act", lambda e: e.activation(out=gfm[:, tb * 512:(tb + 1) * 512], in_=p_t[0:8, :],
                                                   func=AF.Exp, bias=dtb[:]), R=[p_r, r_dtb], W=[r_gfm])
            T.op("act", lambda e: e.activation(out=gfm[:], in_=gfm[:], func=AF.Ln, bias=onec[0:8, :]), R=[r_gfm, r_onec], W=[r_gfm])
            T.op("dve", lambda e: e.tensor_scalar(out=gfm[:], in0=gfm[:], scalar1=nega[:], scalar2=None, op0=ALU.mult),
                 R=[r_gfm, r_nega], W=[r_gfm])
            btm, r_btm = pg.sb("btm", [128, NT, 8], F32)
            gtm, r_gtm = pg.sb("gtm", [128, NT, 8], F32)
            gcs, r_gcs = pg.sb("gcs", [128, NT, 8], F32)
            gto, r_gto = pg.sb("gto", [128, NT, 8], F32)
            ex1, r_ex1 = pg.sb("ex1", [128, NT, 8], F32)
            ex2, r_ex2 = pg.sb("ex2", [128, NT, 8], F32)
            ex3, r_ex3 = pg.sb("ex3", [128, 2, NT, 8], F32)
            ps_t, ps_r = psm[0]
            psv = ps_t[:, 0:256].rearrange("p (t h) -> p t h", h=8)
            for src, r_src, dst, r_dst in ((bfm, r_bfm, btm, r_btm), (gfm, r_gfm, gtm, r_gtm)):
                for tt in range(NT):
                    T.op("pe", lambda e: e.transpose(out=psv[:, tt, :], in_=src[:, tt * 128:(tt + 1) * 128],
                                                     identity=ident_f[0:8, 0:8]), R=[r_src, r_ident_f], W=[ps_r])
                T.op("dve", lambda e: e.tensor_copy(out=dst[:], in_=psv), R=[ps_r], W=[r_dst])
            T.op("pe", lambda e: e.matmul(psv[:, :, 0:4], lhsT=masks[:, 1, :], rhs=gtm[:, :, 0:4], start=True, stop=True),
                 R=[r_masks, r_gtm], W=[ps_r])
            T.op("pe", lambda e: e.matmul(psv[:, :, 4:8], lhsT=masks[:, 3, :], rhs=gtm[:, :, 4:8], start=True, stop=True),
                 R=[r_masks, r_gtm], W=[ps_r])
            T.op("dve", lambda e: e.tensor_copy(out=gcs[:], in_=psv), R=[ps_r], W=[r_gcs])
            T.op("pe", lambda e: e.matmul(ps_t[:, 0:256], lhsT=masks[:, 4, :], rhs=gtm[:].rearrange("p t h -> p (t h)"),
                                          start=True, stop=True), R=[r_masks, r_gtm], W=[ps_r])
            T.op("dve", lambda e: e.tensor_copy(out=gto[:], in_=psv), R=[ps_r], W=[r_gto])
            T.op("act", lambda e: e.activation(out=ex1[:], in_=gcs[:], func=AF.Exp), R=[r_gcs], W=[r_ex1])
            T.op("dve", lambda e: e.tensor_tensor(out=ex2[:], in0=gto[:], in1=gcs[:], op=ALU.subtract),
                 R=[r_gto, r_gcs], W=[r_ex2])
            T.op("act", lambda e: e.activation(out=ex2[:], in_=ex2[:], func=AF.Exp), R=[r_ex2], W=[r_ex2])
            for ab in range(2):
                T.op("pe", lambda e: e.matmul(ps_t[:, 0:256], lhsT=masks[:, 5 + ab, :],
                                              rhs=gtm[:].rearrange("p t h -> p (t h)"), start=True, stop=True),
                     R=[r_masks, r_gtm], W=[ps_r])
                T.op("act", lambda e: e.activation(out=ex3[:, ab], in_=psv, func=AF.Exp), R=[ps_r], W=[r_ex3])
            gsc = P.dram["gsc"]
            rg = P.dres["gsc"]
            for k, (src, r_src) in enumerate(((btm, r_btm), (gcs, r_gcs), (ex1, r_ex1), (ex2, r_ex2))):
                T.dma("sp", gsc[k], src[:].rearrange("p t h -> p (t h)"), R=[r_src], W=[rg])
            for ab in range(2):
                T.dma("sp", gsc[4 + ab], ex3[:, ab].rearrange("p t h -> p (t h)"), R=[r_ex3], W=[rg])
    T.barrier()


def phase_attn(P, C, l):
    T = P.T
    lam_init = 0.8 - 0.6 * math.exp(-0.3 * l)
    scale = 128.0 ** -0.5
    epsc, r_epsc = C["epsc"]
    mh = P.dram["mixed_half"]
    r_mh = P.dres["mixed_half"]
    with Pool_(P) as pl:
        dlb, r_dlb = pl.sb("dlb", [128, 512], F32)
        prod, r_prod = pl.sb("prod", [128, 256], F32)
        s12, r_s12 = pl.sb("s12", [128, 2], F32)
        nlam, r_nlam = pl.sb("nlam", [128, 1], F32)
        subw, r_subw = pl.sb("subw", [128, 256], F32)
        T.dma("sp", dlb[:], P.dram["dlam%d" % l].partition_broadcast(128), R=[P.dres["dlam%d" % l]], W=[r_dlb])
        T.dma("sp", subw[:], P.dram["subw%d" % l].partition_broadcast(128), R=[P.dres["subw%d" % l]], W=[r_subw])
        T.op("dve", lambda e: e.tensor_mul(out=prod[:, 0:128], in0=dlb[:, 0:128], in1=dlb[:, 128:256]), R=[r_dlb], W=[r_prod])
        T.op("dve", lambda e: e.tensor_mul(out=prod[:, 128:256], in0=dlb[:, 256:384], in1=dlb[:, 384:512]), R=[r_dlb], W=[r_prod])
        T.op("dve", lambda e: e.reduce_sum(out=s12[:, 0:1], in_=prod[:, 0:128], axis=AX.X), R=[r_prod], W=[r_s12])
        T.op("dve", lambda e: e.reduce_sum(out=s12[:, 1:2], in_=prod[:, 128:256], axis=AX.X), R=[r_prod], W=[r_s12])
        T.op("act", lambda e: e.activation(out=s12[:], in_=s12[:], func=AF.Exp), R=[r_s12], W=[r_s12])
        T.op("dve", lambda e: e.tensor_sub(out=nlam[:], in0=s12[:, 1:2], in1=s12[:, 0:1]), R=[r_s12], W=[r_nlam])
        T.op("dve", lambda e: e.tensor_scalar(out=nlam[:], in0=nlam[:], scalar1=-lam_init, scalar2=None, op0=ALU.add),
             R=[r_nlam], W=[r_nlam])
        T.op("dve", lambda e: e.tensor_scalar(out=subw[:], in0=subw[:], scalar1=1.0 - lam_init, scalar2=None, op0=ALU.mult),
             R=[r_subw], W=[r_subw])
        vp, r_vp = pl.sb("vp", [128, NT, 257], BF16)
        qT2, r_qT2 = pl.sb("qT2", [128, 2, S], BF16)
        kT2, r_kT2 = pl.sb("kT2", [128, 2, S], BF16)
        pT = [pl.sb("pT%d" % i, [128, 512], BF16) for i in range(2)]
        ev = [pl.sb("ev%d" % i, [128, 257], F32) for i in range(2)]
        o0 = [pl.sb("o0_%d" % i, [128, 256], F32) for i in range(4)]
        oc, r_oc = pl.sb("oc", [128, 256], F32)
        junk, r_junk = pl.sb("junk2", [128, 256], F32)
        rc, r_rc = pl.sb("rc", [128, 1], F32)
        ssq, r_ssq = pl.sb("ssq", [128, 1], F32)
        onb = [pl.sb("onb%d" % i, [128, 256], BF16) for i in range(2)]
        pss = [pl.ps("pss%d" % i, [128, 512], F32) for i in range(2)]
        po = [pl.ps("po%d" % i, [128, 512], F32) for i in range(4)]
        n_on = 0
        for h in range(2):
            T.op("dve", lambda e: e.memset(vp[:, :, 256:257], 1.0), W=[r_vp])
            T.dma("sp", vp[:, :, 0:256], P.dram["dv"][:, h * 256:(h + 1) * 256].rearrange("(t p) d -> p t d", p=128),
                  R=[P.dres["dv"]], W=[r_vp], nowaw=False)
            for half in range(2):
                T.dma("sp", qT2[:, half, :], P.dram["dqT"][2 * h + half], R=[P.dres["dqT"]], W=[r_qT2])
                T.dma("sp", kT2[:, half, :], P.dram["dkT"][2 * h + half], R=[P.dres["dkT"]], W=[r_kT2])
            for qb in range(8):
                for half in range(2):
                    for kt in range(NT):
                        ps_t, ps_r = pss[kt % 2]
                        p_t, p_r = pT[kt % 2]
                        T.op("pe", lambda e: e.matmul(ps_t[:], lhsT=kT2[:, half, kt * 128:(kt + 1) * 128],
                                                      rhs=qT2[:, half, qb * 512:(qb + 1) * 512], start=True, stop=True),
                             R=[r_kT2, r_qT2], W=[ps_r])
                        T.op("act", lambda e: e.activation(out=p_t[:], in_=ps_t[:], func=AF.Exp, scale=scale),
                             R=[ps_r], W=[p_r])
                        for qs in range(4):
                            T.op("pe", lambda e: e.matmul(po[qs][0][:, 0:257], lhsT=p_t[:, qs * 128:(qs + 1) * 128],
                                                          rhs=vp[:, kt, :], start=(kt == 0), stop=(kt == NT - 1)),
                                 R=[p_r, r_vp], W=[po[qs][1]])
                    for qs in range(4):
                        e_t, e_r = ev[qs % 2]
                        T.op("act", lambda e: e.copy(out=e_t[:], in_=po[qs][0][:, 0:257]), R=[po[qs][1]], W=[e_r])
                        T.op("dve", lambda e: e.reciprocal(out=rc[:], in_=e_t[:, 256:257]), R=[e_r], W=[r_rc])
                        if half == 0:
                            T.op("dve", lambda e: e.tensor_scalar(out=o0[qs][0][:], in0=e_t[:, 0:256], scalar1=rc[:],
                                                                  scalar2=None, op0=ALU.mult), R=[e_r, r_rc], W=[o0[qs][1]])
                        else:
                            T.op("dve", lambda e: e.tensor_mul(out=rc[:], in0=rc[:], in1=nlam[:]), R=[r_rc, r_nlam], W=[r_rc])
                            T.op("dve", lambda e: e.scalar_tensor_tensor(out=oc[:], in0=e_t[:, 0:256], scalar=rc[:],
                                                                         in1=o0[qs][0][:], op0=ALU.mult, op1=ALU.add),
                                 R=[e_r, r_rc, o0[qs][1]], W=[r_oc])
                            T.op("dve", lambda e: e.memset(ssq[:], 0.0), W=[r_ssq])
                            T.op("act", lambda e: e.activation(out=junk[:], in_=oc[:], func=AF.Square, accum_out=ssq[:]),
                                 R=[r_oc], W=[r_junk, r_ssq])
                            T.op("act", lambda e: e.activation(out=ssq[:], in_=ssq[:], func=AF.Sqrt, bias=epsc[:],
                                                               scale=1.0 / 256), R=[r_ssq, r_epsc], W=[r_ssq])
                            T.op("dve", lambda e: e.reciprocal(out=ssq[:], in_=ssq[:]), R=[r_ssq], W=[r_ssq])
                            ob_t, ob_r = onb[n_on % 2]
                            n_on += 1
                            T.op("dve", lambda e: e.scalar_tensor_tensor(out=ob_t[:], in0=oc[:], scalar=ssq[:], in1=subw[:],
                                                                         op0=ALU.mult, op1=ALU.mult),
                                 R=[r_oc, r_ssq, r_subw], W=[ob_r])
                            t0 = (qb * 4 + qs) * 128
                            T.dma("sp", mh[t0:t0 + 128, 512 + h * 256:512 + (h + 1) * 256], ob_t[:], R=[ob_r], W=[r_mh])


def phase_gdn(P, C, l, heads=range(GH), tiles=NT):
    T = P.T
    ident_bf, r_ident_bf = C["ident_bf"]
    ident_f, r_ident_f = C["ident_f"]
    masks, r_masks = C["masks"]
    epsc, r_epsc = C["epsc"]
    mh = P.dram["mixed_half"]
    r_mh = P.dres["mixed_half"]
    with Pool_(P) as pl:
        gs = []
        for k in range(6):
            t, r = pl.sb("gs%d" % k, [128, NT * 8], F32)
            T.dma("sp", t[:], P.dram["gsc"][k], R=[P.dres["gsc"]], W=[r])
            gs.append((t, r))
        (beta, r_beta), (gc, r_gc), (egc, r_egc), (ekd, r_ekd), (glA, r_glA), (glB, r_glB) = gs
        nbeta, r_nbeta = pl.sb("nbeta", [128, NT * 8], F32)
        T.op("dve", lambda e: e.tensor_scalar(out=nbeta[:], in0=beta[:], scalar1=-1.0, scalar2=None, op0=ALU.mult),
             R=[r_beta], W=[r_nbeta])
        gnw, r_gnw = pl.sb("gnw", [128, 128], F32)
        T.dma("sp", gnw[:], P.dram["gnw%d" % l].partition_broadcast(128), R=[P.dres["gnw%d" % l]], W=[r_gnw])
        ones_f, r_ones_f = pl.sb("ones_f", [128, 128], F32)
        T.op("dve", lambda e: e.memset(ones_f[:], 1.0), W=[r_ones_f])
        qT, r_qT = pl.sb("g_qT", [128, S], BF16)
        kT, r_kT = pl.sb("g_kT", [128, S], BF16)
        ktm, r_ktm = pl.sb("g_ktm", [128, NT, 128], BF16)
        vtm, r_vtm = pl.sb("g_vtm", [128, NT, 128], BF16)
        obuf, r_obuf = pl.sb("g_obuf", [128, NT, 128], F32)
        Sf, r_Sf = pl.sb("g_S", [128, 128], F32)
        Sb, r_Sb = pl.sb("g_Sb", [128, 128], BF16)

        def sbt(name, dt=F32, shape=(128, 128)):
            return pl.sb("g_" + name, list(shape), dt)

        Ig = sbt("Ig")
        tmp = sbt("tmp")
        DT = sbt("DT")
        DTM = sbt("DTM")
        kk_sb = sbt("kk")
        qk_sb = sbt("qk")
        F_bf = [sbt("F%d" % i, BF16) for i in range(2)]
        FT_bf = [sbt("FT%d" % i, BF16) for i in range(2)]
        X = sbt("X")
        X_bf = sbt("Xb", BF16)
        xt_ = sbt("xtmp")
        qkm = sbt("qkm", BF16)
        kg = sbt("kg", BF16)
        kd = sbt("kd", BF16)
        nwT = sbt("nwT", BF16)
        vn = sbt("vn", BF16)
        ta = sbt("ta")
        tb_ = sbt("tb")
        ts = sbt("ts")
        pA = [pl.ps("g_pA%d" % i, [128, 128], F32) for i in range(3)]
        pB = [pl.ps("g_pB%d" % i, [128, 128], F32) for i in range(3)]
        pTr = pl.ps("g_pTr", [128, 128], BF16)
        zt, r_zt = pl.sb("g_zt", [128, NT, 128], BF16)
        ssq, r_ssq = pl.sb("g_ssq", [128, 1], F32)
        junk, r_junk = pl.sb("g_junk", [128, 128], F32)
        on_, r_on = pl.sb("g_on", [128, 128], F32)
        onb = [pl.sb("g_onb%d" % i, [128, 128], BF16) for i in range(2)]

        def cp(en, out, in_, R, W, scale=None):
            if en == "act":
                if scale is None:
                    T.op("act", lambda e: e.copy(out=out, in_=in_), R=R, W=W)
                else:
                    T.op("act", lambda e: e.activation(out=out, in_=in_, func=AF.Copy, scale=scale), R=R, W=W)
            else:
                T.op("dve", lambda e: e.tensor_copy(out=out, in_=in_), R=R, W=W)

        for h in heads:
            T.dma("sp", qT[:], P.dram["gqT"][h], R=[P.dres["gqT"]], W=[r_qT])
            T.dma("sp", kT[:], P.dram["gkT"][h], R=[P.dres["gkT"]], W=[r_kT])
            T.dma("sp", ktm[:], P.dram["gk_tm"][h].rearrange("(t p) d -> p t d", p=128), R=[P.dres["gk_tm"]], W=[r_ktm])
            T.dma("sp", vtm[:], P.dram["gv_tm"][h].rearrange("(t p) d -> p t d", p=128), R=[P.dres["gv_tm"]], W=[r_vtm])
            T.dma("sp", zt[:], P.dram["siluz"][:, h * 128:(h + 1) * 128].rearrange("(t p) d -> p t d", p=128),
                  R=[P.dres["siluz"]], W=[r_zt])
            for d in range(2):
                T.op("dve", lambda e: e.memset(Sf[:], 0.0), W=[r_Sf])
                T.op("dve", lambda e: e.memset(Sb[:], 0.0), W=[r_Sb])
                ms, mi = (0, 1) if d == 0 else (2, 3)
                order = list(range(tiles)) if d == 0 else list(range(tiles - 1, -1, -1))
                for tt in order:
                    col = tt * 8 + d * 4 + h
                    tsl = slice(tt * 128, (tt + 1) * 128)
                    gcc = gc[:, col:col + 1]
                    T.op("pe", lambda e: e.matmul(pA[0][0][:], lhsT=kT[:, tsl], rhs=kT[:, tsl], start=True, stop=True),
                         R=[r_kT], W=[pA[0][1]])
                    T.op("pe", lambda e: e.matmul(pA[1][0][:], lhsT=kT[:, tsl], rhs=qT[:, tsl], start=True, stop=True),
                         R=[r_kT, r_qT], W=[pA[1][1]])
                    cp("act", kk_sb[0][:], pA[0][0][:], [pA[0][1]], [kk_sb[1]])
                    cp("act", qk_sb[0][:], pA[1][0][:], [pA[1][1]], [qk_sb[1]])
                    T.op("dve", lambda e: e.tensor_scalar(out=Ig[0][:], in0=ident_f[:], scalar1=gcc, scalar2=None, op0=ALU.mult),
                         R=[r_ident_f, r_gc], W=[Ig[1]])
                    T.op("pe", lambda e: e.matmul(pA[2][0][:], lhsT=ones_f[:], rhs=Ig[0][:], start=True, stop=True),
                         R=[r_ones_f, Ig[1]], W=[pA[2][1]])
                    T.op("dve", lambda e: e.tensor_scalar(out=tmp[0][:], in0=pA[2][0][:], scalar1=gcc, scalar2=0.0,
                                                          op0=ALU.subtract, op1=ALU.min), R=[pA[2][1], r_gc], W=[tmp[1]])
                    T.op("act", lambda e: e.activation(out=DT[0][:], in_=tmp[0][:], func=AF.Exp), R=[tmp[1]], W=[DT[1]])
                    T.op("dve", lambda e: e.tensor_mul(out=DTM[0][:], in0=DT[0][:], in1=masks[:, ms, :]),
                         R=[DT[1], r_masks], W=[DTM[1]])
                    T.op("dve", lambda e: e.scalar_tensor_tensor(out=X[0][:], in0=kk_sb[0][:], scalar=nbeta[:, col:col + 1],
                                                                 in1=DTM[0][:], op0=ALU.mult, op1=ALU.mult),
                         R=[kk_sb[1], r_nbeta, DTM[1]], W=[X[1]])
                    fi = 0
                    cp("act", F_bf[fi][0][:], X[0][:], [X[1]], [F_bf[fi][1]])
                    T.op("pe", lambda e: e.transpose(out=pTr[0][:], in_=F_bf[fi][0][:], identity=ident_bf[:]),
                         R=[F_bf[fi][1], r_ident_bf], W=[pTr[1]])
                    cp("act", FT_bf[fi][0][:], pTr[0][:], [pTr[1]], [FT_bf[fi][1]])
                    T.op("dve", lambda e: e.tensor_add(out=X[0][:], in0=X[0][:], in1=ident_f[:]), R=[X[1], r_ident_f], W=[X[1]])
                    cp("act", X_bf[0][:], X[0][:], [X[1]], [X_bf[1]])
                    T.op("dve", lambda e: e.tensor_mul(out=DTM[0][:], in0=DT[0][:], in1=masks[:, mi, :]),
                         R=[DT[1], r_masks], W=[DTM[1]])
                    T.op("dve", lambda e: e.tensor_mul(out=qkm[0][:], in0=qk_sb[0][:], in1=DTM[0][:]),
                         R=[qk_sb[1], DTM[1]], W=[qkm[1]])
                    T.op("dve", lambda e: e.tensor_scalar(out=kg[0][:], in0=ktm[:, tt, :], scalar1=egc[:, col:col + 1],
                                                          scalar2=None, op0=ALU.mult), R=[r_ktm, r_egc], W=[kg[1]])
                    T.op("dve", lambda e: e.tensor_scalar(out=kd[0][:], in0=ktm[:, tt, :], scalar1=ekd[:, col:col + 1],
                                                          scalar2=None, op0=ALU.mult), R=[r_ktm, r_ekd], W=[kd[1]])
                    for k in range(5):
                        fo = 1 - fi
                        T.op("pe", lambda e: e.matmul(pB[0][0][:], lhsT=FT_bf[fi][0][:], rhs=F_bf[fi][0][:], start=True, stop=True),
                             R=[FT_bf[fi][1], F_bf[fi][1]], W=[pB[0][1]])
                        T.op("pe", lambda e: e.matmul(pB[1][0][:], lhsT=F_bf[fi][0][:], rhs=FT_bf[fi][0][:], start=True, stop=True),
                             R=[FT_bf[fi][1], F_bf[fi][1]], W=[pB[1][1]])
                        cp("act", F_bf[fo][0][:], pB[0][0][:], [pB[0][1]], [F_bf[fo][1]])
                        cp("dve", FT_bf[fo][0][:], pB[1][0][:], [pB[1][1]], [FT_bf[fo][1]])
                        T.op("pe", lambda e: e.matmul(pB[2][0][:], lhsT=FT_bf[fo][0][:], rhs=X_bf[0][:], start=True, stop=True),
                             R=[FT_bf[fo][1], X_bf[1]], W=[pB[2][1]])
                        cp("act", xt_[0][:], pB[2][0][:], [pB[2][1]], [xt_[1]])
                        T.op("dve", lambda e: e.tensor_add(out=X[0][:], in0=X[0][:], in1=xt_[0][:]), R=[X[1], xt_[1]], W=[X[1]])
                        cp("act", X_bf[0][:], X[0][:], [X[1]], [X_bf[1]])
                        fi = fo
                    T.op("pe", lambda e: e.matmul(pA[0][0][:], lhsT=kg[0][:], rhs=X_bf[0][:], start=True, stop=True),
                         R=[kg[1], X_bf[1]], W=[pA[0][1]])
                    cp("act", nwT[0][:], pA[0][0][:], [pA[0][1]], [nwT[1]], scale=-1.0)
                    for ch in ((0, 1) if d == 0 else (1, 0)):
                        ps_ = slice(ch * 64, ch * 64 + 64)
                        csl = slice(tt * 128 + ch * 64, tt * 128 + ch * 64 + 64)
                        glc = (glA if ch == 0 else glB)[:, col:col + 1]
                        r_gl = r_glA if ch == 0 else r_glB
                        T.op("pe", lambda e: e.matmul(pA[1][0][0:64, :], lhsT=X_bf[0][ps_, ps_], rhs=vtm[ps_, tt, :],
                                                      start=True, stop=False), R=[X_bf[1], r_vtm], W=[pA[1][1]])
                        T.op("pe", lambda e: e.matmul(pA[1][0][0:64, :], lhsT=nwT[0][:, ps_], rhs=Sb[:],
                                                      start=False, stop=True), R=[nwT[1], r_Sb], W=[pA[1][1]])
                        T.op("dve", lambda e: e.tensor_scalar(out=vn[0][ps_, :], in0=pA[1][0][0:64, :],
                                                              scalar1=beta[ps_, col:col + 1], scalar2=None, op0=ALU.mult),
                             R=[pA[1][1], r_beta], W=[vn[1]])
                        T.op("pe", lambda e: e.matmul(pA[2][0][0:64, :], lhsT=qT[:, csl], rhs=Sb[:], start=True, stop=True),
                             R=[r_qT, r_Sb], W=[pA[2][1]])
                        T.op("pe", lambda e: e.matmul(pB[0][0][0:64, :], lhsT=qkm[0][ps_, ps_], rhs=vn[0][ps_, :],
                                                      start=True, stop=True), R=[qkm[1], vn[1]], W=[pB[0][1]])
                        T.op("pe", lambda e: e.matmul(pB[1][0][:], lhsT=kd[0][ps_, :], rhs=vn[0][ps_, :], start=True, stop=True),
                             R=[kd[1], vn[1]], W=[pB[1][1]])
                        cp("act", ta[0][ps_, :], pA[2][0][0:64, :], [pA[2][1]], [ta[1]])
                        cp("act", tb_[0][ps_, :], pB[0][0][0:64, :], [pB[0][1]], [tb_[1]])
                        if d == 1:
                            T.op("dve", lambda e: e.tensor_add(out=tb_[0][ps_, :], in0=tb_[0][ps_, :], in1=obuf[ps_, tt, :]),
                                 R=[tb_[1], r_obuf], W=[tb_[1]])
                        T.op("dve", lambda e: e.scalar_tensor_tensor(out=obuf[ps_, tt, :], in0=ta[0][ps_, :],
                                                                     scalar=egc[ps_, col:col + 1], in1=tb_[0][ps_, :],
                                                                     op0=ALU.mult, op1=ALU.add),
                             R=[ta[1], r_egc, tb_[1]], W=[r_obuf])
                        cp("act", ts[0][:], pB[1][0][:], [pB[1][1]], [ts[1]])
                        T.op("dve", lambda e: e.scalar_tensor_tensor(out=Sf[:], in0=Sf[:], scalar=glc, in1=ts[0][:],
                                                                     op0=ALU.mult, op1=ALU.add),
                             R=[r_Sf, r_gl, ts[1]], W=[r_Sf])
                        cp("act", Sb[:], Sf[:], [r_Sf], [r_Sb])
            for tt in range(tiles):
                T.op("dve", lambda e: e.memset(ssq[:], 0.0), W=[r_ssq])
                T.op("act", lambda e: e.activation(out=junk[:], in_=obuf[:, tt, :], func=AF.Square, accum_out=ssq[:]),
                     R=[r_obuf], W=[r_junk, r_ssq])
                T.op("act", lambda e: e.activation(out=ssq[:], in_=ssq[:], func=AF.Sqrt, bias=epsc[:], scale=1.0 / 128),
                     R=[r_ssq, r_epsc], W=[r_ssq])
                T.op("dve", lambda e: e.reciprocal(out=ssq[:], in_=ssq[:]), R=[r_ssq], W=[r_ssq])
                T.op("dve", lambda e: e.scalar_tensor_tensor(out=on_[:], in0=obuf[:, tt, :], scalar=ssq[:], in1=gnw[:],
                                                             op0=ALU.mult, op1=ALU.mult), R=[r_obuf, r_ssq, r_gnw], W=[r_on])
                ob_t, ob_r = onb[tt % 2]
                T.op("dve", lambda e: e.tensor_mul(out=ob_t[:], in0=on_[:], in1=zt[:, tt, :]), R=[r_on, r_zt], W=[ob_r])
                T.dma("sp", mh[tt * 128:(tt + 1) * 128, h * 128:(h + 1) * 128], ob_t[:], R=[ob_r], W=[r_mh])


PAIRS = [[0, 1], [2, 3], [4, 5], [6, 7]]


def pair_allgather(P, src, dst, nrows, rows_per_call):
    T = P.T
    sd, rs = P.dram[src], P.dres[src]
    dd, rd = P.dram[dst], P.dres[dst]
    ncall = nrows // rows_per_call
    for c in range(ncall):
        T._deps("pool", [rs], [rd], nowaw=True)
        inst = T.eng["pool"].collective_compute(
            "AllGather", ALU.bypass, replica_groups=PAIRS,
            ins=[sd[c * rows_per_call:(c + 1) * rows_per_call, :].opt()],
            outs=[dd[c].rearrange("r t w -> (r t) w").opt()])
        T._dma_done(inst, [rs], [rd], inc=1)


def phase3(P, C, l, xsrc):
    T = P.T
    ident_bf, r_ident_bf = C["ident_bf"]
    ident_f, r_ident_f = C["ident_f"]
    epsc, r_epsc = C["epsc"]
    pair_allgather(P, "mixed_half", "mg", S, 1024)
    mg, r_mg = P.dram["mg"], P.dres["mg"]
    xd, xr = P.dram[xsrc], P.dres[xsrc]
    x1d, x1r = P.dram["x1"], P.dres["x1"]
    x2d, x2r = P.dram["x2"], P.dres["x2"]
    wo = P.dram["w_out%d" % l].rearrange("(kc p) n -> p kc n", p=128)
    with Pool_(P) as pl:
        wob, r_wob = pl.sb("wob", [128, KC, D], BF16)
        for q4 in range(4):
            T.dma("pool", wob[:, q4 * 4:(q4 + 1) * 4, :], wo[:, q4 * 4:(q4 + 1) * 4, :], R=[P.dres["w_out%d" % l]], W=[r_wob])
        wr_f, r_wrf = pl.sb("wr_f", [128, KC, 16], F32)
        T.dma("sp", wr_f[:], P.dram["w_r%d" % l].rearrange("(kc p) n -> p kc n", p=128), R=[P.dres["w_r%d" % l]], W=[r_wrf])
        n2b, r_n2b = pl.sb("n2b", [128, D], F32)
        T.dma("sp", n2b[:], P.dram["n2w%d" % l].partition_broadcast(128), R=[P.dres["n2w%d" % l]], W=[r_n2b])
        aff, r_aff = pl.sb("aff", [128, NT, 16], F32)
        mt = [pl.sb("mt%d" % i, [128, D], BF16) for i in range(2)]
        mT = [pl.sb("mT%d" % i, [128, KC, 128], BF16) for i in range(2)]
        xt = [pl.sb("p3xt%d" % i, [128, D], F32) for i in range(2)]
        x1t = [pl.sb("x1t%d" % i, [128, D], F32) for i in range(2)]
        x2f, r_x2f = pl.sb("x2f", [128, D], F32)
        x2b = [pl.sb("x2b%d" % i, [128, D], BF16) for i in range(2)]
        x2T, r_x2T = pl.sb("x2T", [128, KC, 128], F32)
        junk, r_junk = pl.sb("p3junk", [128, D], BF16)
        ss, r_ss = pl.sb("p3ss", [128, 1], F32)
        mx, r_mx = pl.sb("p3mx", [128, 1], F32)
        lg, r_lg = pl.sb("p3lg", [128, 16], F32)
        ptr = [pl.ps("p3ptr%d" % i, [128, 8, 128], BF16) for i in range(2)]
        pacc = [pl.ps("p3acc%d" % i, [128, 512], F32) for i in range(2)]
        ptf = [pl.ps("p3ptf%d" % i, [128, 4, 128], F32) for i in range(2)]
        plg = pl.ps("p3lg", [128, 16], F32)
        for tt in range(NT):
            i = tt % 2
            m_t, m_r = mt[i]
            cidx, r0 = (tt * 128) // 1024, (tt * 128) % 1024
            for rk in range(2):
                T.dma("sp", m_t[:, rk * 1024:(rk + 1) * 1024], mg[cidx, rk, r0:r0 + 128, :], R=[r_mg], W=[m_r])
            x_t, x_r = xt[i]
            T.dma("sp", x_t[:], xd[tt * 128:(tt + 1) * 128, :], R=[xr], W=[x_r])
            mT_t, mT_r = mT[i]
            for hh in range(2):
                p_t, p_r = ptr[hh]
                for k in range(8):
                    kc = hh * 8 + k
                    T.op("pe", lambda e: e.transpose(out=p_t[:, k, :], in_=m_t[:, kc * 128:(kc + 1) * 128], identity=ident_bf[:]),
                         R=[m_r, r_ident_bf], W=[p_r])
                T.op("act", lambda e: e.copy(out=mT_t[:, hh * 8:(hh + 1) * 8, :], in_=p_t[:]), R=[p_r], W=[mT_r])
            x1_t, x1_r = x1t[i]
            for cb in range(4):
                pa_t, pa_r = pacc[cb % 2]
                for kc in range(KC):
                    T.op("pe", lambda e: e.matmul(pa_t[:], lhsT=mT_t[:, kc, :], rhs=wob[:, kc, cb * 512:(cb + 1) * 512],
                                                  start=(kc == 0), stop=(kc == KC - 1)), R=[mT_r, r_wob], W=[pa_r])
                T.op("act", lambda e: e.copy(out=x1_t[:, cb * 512:(cb + 1) * 512], in_=pa_t[:]), R=[pa_r], W=[x1_r])
            T.op("dve", lambda e: e.tensor_add(out=x1_t[:], in0=x1_t[:], in1=x_t[:]), R=[x1_r, x_r], W=[x1_r])
            T.dma("sp", x1d[tt * 128:(tt + 1) * 128, :], x1_t[:], R=[x1_r], W=[x1r])
            T.op("dve", lambda e: e.memset(ss[:], 0.0), W=[r_ss])
            T.op("act", lambda e: e.activation(out=junk[:], in_=x1_t[:], func=AF.Square, accum_out=ss[:]), R=[x1_r], W=[r_junk, r_ss])
            T.op("act", lambda e: e.activation(out=ss[:], in_=ss[:], func=AF.Sqrt, bias=epsc[:], scale=1.0 / D), R=[r_ss, r_epsc], W=[r_ss])
            T.op("dve", lambda e: e.reciprocal(out=ss[:], in_=ss[:]), R=[r_ss], W=[r_ss])
            T.op("dve", lambda e: e.scalar_tensor_tensor(out=x2f[:], in0=x1_t[:], scalar=ss[:], in1=n2b[:], op0=ALU.mult, op1=ALU.mult),
                 R=[x1_r, r_ss, r_n2b], W=[r_x2f])
            xb_t, xb_r = x2b[i]
            T.op("act", lambda e: e.copy(out=xb_t[:], in_=x2f[:]), R=[r_x2f], W=[xb_r])
            T.dma("sp", x2d[tt * 128:(tt + 1) * 128, :], xb_t[:], R=[xb_r], W=[x2r])
            for g4 in range(4):
                pf_t, pf_r = ptf[g4 % 2]
                for k in range(4):
                    kc = g4 * 4 + k
                    T.op("pe", lambda e: e.transpose(out=pf_t[:, k, :], in_=x2f[:, kc * 128:(kc + 1) * 128], identity=ident_f[:]),
                         R=[r_x2f, r_ident_f], W=[pf_r])
                T.op("act", lambda e: e.copy(out=x2T[:, g4 * 4:(g4 + 1) * 4, :], in_=pf_t[:]), R=[pf_r], W=[r_x2T])
            for kc in range(KC):
                T.op("pe", lambda e: e.matmul(plg[0][:], lhsT=x2T[:, kc, :], rhs=wr_f[:, kc, :], start=(kc == 0), stop=(kc == KC - 1)),
                     R=[r_x2T, r_wrf], W=[plg[1]])
            T.op("act", lambda e: e.copy(out=lg[:], in_=plg[0][:]), R=[plg[1]], W=[r_lg])
            T.op("dve", lambda e: e.reduce_max(out=mx[:], in_=lg[:], axis=AX.X), R=[r_lg], W=[r_mx])
            T.op("dve", lambda e: e.tensor_scalar(out=mx[:], in0=mx[:], scalar1=-1.0, scalar2=None, op0=ALU.mult), R=[r_mx], W=[r_mx])
            T.op("dve", lambda e: e.memset(ss[:], 0.0), W=[r_ss])
            T.op("act", lambda e: e.activation(out=lg[:], in_=lg[:], func=AF.Exp, bias=mx[:], accum_out=ss[:]), R=[r_lg, r_mx], W=[r_lg, r_ss])
            T.op("dve", lambda e: e.reciprocal(out=ss[:], in_=ss[:]), R=[r_ss], W=[r_ss])
            T.op("dve", lambda e: e.tensor_scalar(out=aff[:, tt, :], in0=lg[:], scalar1=ss[:], scalar2=None, op0=ALU.mult),
                 R=[r_lg, r_ss], W=[r_aff])
        T.dma("sp", P.dram["affd"], aff[:].rearrange("p t e -> p (t e)"), R=[r_aff], W=[P.dres["affd"]])


def phase4(P, C, l, xdst):
    T = P.T
    ident_bf, r_ident_bf = C["ident_bf"]
    masks, r_masks = C["masks"]
    x2d, x2r = P.dram["x2"], P.dres["x2"]
    wg = P.dram["w_g%d" % l]
    wu = P.dram["w_u%d" % l]
    wdn = P.dram["w_d%d" % l]
    with Pool_(P) as pl:
        aff, r_aff = pl.sb("aff4", [128, NT, 16], F32)
        T.dma("sp", aff[:].rearrange("p t e -> p (t e)"), P.dram["affd"], R=[P.dres["affd"]], W=[r_aff])
        ones_f, r_ones_f = pl.sb("ones4", [128, 128], F32)
        T.op("dve", lambda e: e.memset(ones_f[:], 1.0), W=[r_ones_f])
        iota, r_iota = pl.sb("iota", [128, 512], F32)
        T.dma("sp", iota[:], P.dram["iota"], R=[P.dres["iota"]], W=[r_iota])
        lo, r_lo = pl.sb("lo", [128, NE], F32)
        hi, r_hi = pl.sb("hi", [128, NE], F32)
        mid, r_mid = pl.sb("mid", [128, NE], F32)
        cnt, r_cnt = pl.sb("cnt", [128, NE], F32)
        ge, r_ge = pl.sb("ge", [128, NE], F32)
        dl_, r_dl = pl.sb("dl_", [128, NE], F32)
        cmp, r_cmp = pl.sb("cmp", [128, NE, NT], F32)
        msk, r_msk = pl.sb("msk", [128, NT, NE], F32)
        gat, r_gat = pl.sb("gat", [128, NT, NE], F32)
        pos, r_pos = pl.sb("pos", [128, NT, NE], F32)
        tot, r_tot = pl.sb("tot", [128, NT, NE], F32)
        offs, r_offs = pl.sb("offs", [128, NT, NE], F32)
        prs = Pool_(P)
        prs.__enter__()
        pcn = prs.ps("pcn", [128, NE], F32)
        ppos = prs.ps("ppos", [128, NT * NE], F32)
        T.op("dve", lambda e: e.memset(lo[:], 0.0), W=[r_lo])
        T.op("dve", lambda e: e.memset(hi[:], 1.0), W=[r_hi])
        for it in range(26):
            T.op("dve", lambda e: e.tensor_add(out=mid[:], in0=lo[:], in1=hi[:]), R=[r_lo, r_hi], W=[r_mid])
            T.op("dve", lambda e: e.tensor_scalar(out=mid[:], in0=mid[:], scalar1=0.5, scalar2=None, op0=ALU.mult), R=[r_mid], W=[r_mid])
            for ex in range(NE):
                T.op("dve", lambda e: e.tensor_scalar(out=cmp[:, ex, :], in0=aff[:, :, ex], scalar1=mid[:, ex:ex + 1], scalar2=None,
                                                      op0=ALU.is_ge), R=[r_aff, r_mid], W=[r_cmp])
            T.op("dve", lambda e: e.reduce_sum(out=cnt[:], in_=cmp[:], axis=AX.X), R=[r_cmp], W=[r_cnt])
            T.op("pe", lambda e: e.matmul(pcn[0][:], lhsT=ones_f[:], rhs=cnt[:], start=True, stop=True), R=[r_ones_f, r_cnt], W=[pcn[1]])
            T.op("dve", lambda e: e.tensor_scalar(out=ge[:], in0=pcn[0][:], scalar1=float(CAP), scalar2=None, op0=ALU.is_ge),
                 R=[pcn[1]], W=[r_ge])
            T.op("dve", lambda e: e.tensor_sub(out=dl_[:], in0=mid[:], in1=lo[:]), R=[r_mid, r_lo], W=[r_dl])
            T.op("dve", lambda e: e.tensor_mul(out=dl_[:], in0=dl_[:], in1=ge[:]), R=[r_dl, r_ge], W=[r_dl])
            T.op("dve", lambda e: e.tensor_add(out=lo[:], in0=lo[:], in1=dl_[:]), R=[r_lo, r_dl], W=[r_lo])
            T.op("dve", lambda e: e.tensor_sub(out=dl_[:], in0=hi[:], in1=mid[:]), R=[r_mid, r_hi], W=[r_dl])
            T.op("dve", lambda e: e.tensor_mul(out=dl_[:], in0=dl_[:], in1=ge[:]), R=[r_dl, r_ge], W=[r_dl])
            T.op("dve", lambda e: e.tensor_add(out=hi[:], in0=mid[:], in1=dl_[:]), R=[r_mid, r_dl], W=[r_hi])
        for ex in range(NE):
            T.op("dve", lambda e: e.tensor_scalar(out=msk[:, :, ex], in0=aff[:, :, ex], scalar1=lo[:, ex:ex + 1], scalar2=None,
                                                  op0=ALU.is_ge), R=[r_aff, r_lo], W=[r_msk])
        T.op("dve", lambda e: e.tensor_mul(out=gat[:], in0=msk[:], in1=aff[:, :, 0:NE]), R=[r_msk, r_aff], W=[r_gat])
        mflat = msk[:].rearrange("p t e -> p (t e)")
        T.op("pe", lambda e: e.matmul(ppos[0][:], lhsT=masks[:, 8, :], rhs=mflat, start=True, stop=True), R=[r_masks, r_msk], W=[ppos[1]])
        T.op("act", lambda e: e.copy(out=pos[:].rearrange("p t e -> p (t e)"), in_=ppos[0][:]), R=[ppos[1]], W=[r_pos])
        T.op("pe", lambda e: e.matmul(ppos[0][:], lhsT=ones_f[:], rhs=mflat, start=True, stop=True), R=[r_ones_f, r_msk], W=[ppos[1]])
        T.op("act", lambda e: e.copy(out=tot[:].rearrange("p t e -> p (t e)"), in_=ppos[0][:]), R=[ppos[1]], W=[r_tot])
        T.op("dve", lambda e: e.memset(offs[:, 0, :], 0.0), W=[r_offs])
        for tt in range(1, NT):
            T.op("dve", lambda e: e.tensor_add(out=offs[:, tt, :], in0=offs[:, tt - 1, :], in1=tot[:, tt - 1, :]), R=[r_offs, r_tot], W=[r_offs])
        T.op("dve", lambda e: e.tensor_add(out=pos[:], in0=pos[:], in1=offs[:]), R=[r_pos, r_offs], W=[r_pos])
        prs.__exit__(None, None, None)
        ye = [pl.sb("ye%d" % ex, [128, 4, D], BF16) for ex in range(NE)]
        with Pool_(P) as pa:
            x2t = [pa.sb("x2t%d" % i, [128, D], BF16) for i in range(1)] * 2
            sel = [pa.sb("sel%d" % i, [128, 512], BF16) for i in range(2)]
            xsT, r_xsT = pa.sb("xsT", [128, KC, 512], BF16)
            hT, r_hT = pa.sb("hT", [128, KC, 512], BF16)
            wgb = [pa.sb("wgb%d" % i, [128, KC, 128], BF16) for i in range(1)] * 2
            wub = [pa.sb("wub%d" % i, [128, KC, 128], BF16) for i in range(1)] * 2
            wdb = [pa.sb("wdb%d" % i, [128, KC, 128], BF16) for i in range(2)]
            sg, r_sg = pa.sb("sg", [128, 512], F32)
            su, r_su = pa.sb("su", [128, 512], F32)
            psel = [pa.ps("psel%d" % i, [128, 512], F32) for i in range(8)]
            for ex in range(NE):
                wge = wg[ex].rearrange("(kc p) n -> p kc n", p=128)
                wue = wu[ex].rearrange("(kc p) n -> p kc n", p=128)
                wde = wdn[ex].rearrange("(kc p) n -> p kc n", p=128)
                for half in range(2):
                    for tt in range(NT):
                        i = tt % 2
                        xt_t, xt_r = x2t[i]
                        s_t, s_r = sel[i]
                        T.dma("sp", xt_t[:], x2d[tt * 128:(tt + 1) * 128, :], R=[x2r], W=[xt_r])
                        T.op("dve", lambda e: e.tensor_scalar(out=s_t[:], in0=iota[:], scalar1=pos[:, tt, ex:ex + 1],
                                                              scalar2=msk[:, tt, ex:ex + 1], op0=ALU.is_equal, op1=ALU.mult),
                             R=[r_iota, r_pos, r_msk], W=[s_r])
                        for k in range(8):
                            kc = half * 8 + k
                            T.op("pe", lambda e: e.matmul(psel[k][0][:], lhsT=xt_t[:, kc * 128:(kc + 1) * 128], rhs=s_t[:],
                                                          start=(tt == 0), stop=(tt == NT - 1)), R=[xt_r, s_r], W=[psel[k][1]])
                    for k in range(8):
                        kc = half * 8 + k
                        T.op("act", lambda e: e.copy(out=xsT[:, kc, :], in_=psel[k][0][:]), R=[psel[k][1]], W=[r_xsT])
                for f in range(KC):
                    i = f % 2
                    g_t, g_r = wgb[i]
                    u_t, u_r = wub[i]
                    T.dma("pool", g_t[:], wge[:, :, f * 128:(f + 1) * 128], R=[P.dres["w_g%d" % l]], W=[g_r])
                    T.dma("pool", u_t[:], wue[:, :, f * 128:(f + 1) * 128], R=[P.dres["w_u%d" % l]], W=[u_r])
                    pg_t, pg_r = psel[(2 * f) % 8]
                    pu_t, pu_r = psel[(2 * f + 1) % 8]
                    for kc in range(KC):
                        T.op("pe", lambda e: e.matmul(pg_t[:], lhsT=g_t[:, kc, :], rhs=xsT[:, kc, :], start=(kc == 0), stop=(kc == KC - 1)),
                             R=[g_r, r_xsT], W=[pg_r])
                    for kc in range(KC):
                        T.op("pe", lambda e: e.matmul(pu_t[:], lhsT=u_t[:, kc, :], rhs=xsT[:, kc, :], start=(kc == 0), stop=(kc == KC - 1)),
                             R=[u_r, r_xsT], W=[pu_r])
                    T.op("act", lambda e: e.activation(out=sg[:], in_=pg_t[:], func=AF.Silu), R=[pg_r], W=[r_sg])
                    T.op("act", lambda e: e.copy(out=su[:], in_=pu_t[:]), R=[pu_r], W=[r_su])
                    T.op("dve", lambda e: e.tensor_mul(out=hT[:, f, :], in0=sg[:], in1=su[:]), R=[r_sg, r_su], W=[r_hT])
                for cb in range(16):
                    d_t, d_r = wdb[cb % 2]
                    T.dma("pool", d_t[:], wde[:, :, cb * 128:(cb + 1) * 128], R=[P.dres["w_d%d" % l]], W=[d_r])
                    for cs in range(4):
                        py_t, py_r = psel[(cb * 4 + cs) % 8]
                        for f in range(KC):
                            T.op("pe", lambda e: e.matmul(py_t[:, 0:128], lhsT=hT[:, f, cs * 128:(cs + 1) * 128], rhs=d_t[:, f, :],
                                                          start=(f == 0), stop=(f == KC - 1)), R=[r_hT, d_r], W=[py_r])
                        T.op("act", lambda e: e.copy(out=ye[ex][0][:, cs, cb * 128:(cb + 1) * 128], in_=py_t[:, 0:128]), R=[py_r], W=[ye[ex][1]])
        with Pool_(P) as pb:
            selg = [pb.sb("selg%d" % i, [128, 512], BF16) for i in range(2)]
            selT = [pb.sb("selT%d" % i, [128, 4, 128], BF16) for i in range(2)]
            yt = [pb.sb("yt%d" % i, [128, D], BF16) for i in range(2)]
            pT4 = [pb.ps("pT4_%d" % i, [128, 4, 128], BF16) for i in range(2)]
            py = [pb.ps("py%d" % i, [128, 512], F32) for i in range(4)]
            n = 0
            for tt in range(NT):
                for ex in range(NE):
                    i = n % 2
                    n += 1
                    s_t, s_r = selg[i]
                    T.op("dve", lambda e: e.tensor_scalar(out=s_t[:], in0=iota[:], scalar1=pos[:, tt, ex:ex + 1],
                                                          scalar2=gat[:, tt, ex:ex + 1], op0=ALU.is_equal, op1=ALU.mult),
                         R=[r_iota, r_pos, r_gat], W=[s_r])
                    p_t, p_r = pT4[i]
                    for cs in range(4):
                        T.op("pe", lambda e: e.transpose(out=p_t[:, cs, :], in_=s_t[:, cs * 128:(cs + 1) * 128], identity=ident_bf[:]),
                             R=[s_r, r_ident_bf], W=[p_r])
                    st_t, st_r = selT[i]
                    T.op("act", lambda e: e.copy(out=st_t[:], in_=p_t[:]), R=[p_r], W=[st_r])
                    for cb in range(4):
                        for cs in range(4):
                            T.op("pe", lambda e: e.matmul(py[cb][0][:], lhsT=st_t[:, cs, :], rhs=ye[ex][0][:, cs, cb * 512:(cb + 1) * 512],
                                                          start=(ex == 0 and cs == 0), stop=(ex == NE - 1 and cs == 3)),
                                 R=[st_r, ye[ex][1]], W=[py[cb][1]])
                y_t, y_r = yt[tt % 2]
                for cb in range(4):
                    T.op("act", lambda e: e.copy(out=y_t[:, cb * 512:(cb + 1) * 512], in_=py[cb][0][:]), R=[py[cb][1]], W=[y_r])
                T.dma("sp", P.dram["ypart"][tt * 128:(tt + 1) * 128, :], y_t[:], R=[y_r], W=[P.dres["ypart"]])
    pair_allgather(P, "ypart", "yg", S, 512)
    with Pool_(P) as pl:
        xt = [pl.sb("p5x%d" % i, [128, D], F32) for i in range(2)]
        ya = [pl.sb("p5a%d" % i, [128, 2, D], BF16) for i in range(2)]
        for tt in range(NT):
            i = tt % 2
            x_t, x_r = xt[i]
            a_t, a_r = ya[i]
            cidx, r0 = (tt * 128) // 512, (tt * 128) % 512
            T.dma("sp", x_t[:], P.dram["x1"][tt * 128:(tt + 1) * 128, :], R=[P.dres["x1"]], W=[x_r])
            for rk in range(2):
                T.dma("sp", a_t[:, rk, :], P.dram["yg"][cidx, rk, r0:r0 + 128, :], R=[P.dres["yg"]], W=[a_r])
            T.op("dve", lambda e: e.tensor_add(out=x_t[:], in0=x_t[:], in1=a_t[:, 0, :]), R=[x_r, a_r], W=[x_r])
            T.op("dve", lambda e: e.tensor_add(out=x_t[:], in0=x_t[:], in1=a_t[:, 1, :]), R=[x_r, a_r], W=[x_r])
            T.dma("sp", P.dram[xdst][tt * 128:(tt + 1) * 128, :], x_t[:], R=[x_r], W=[P.dres[xdst]])


def phase_final(P, C, xsrc):
    T = P.T
    epsc, r_epsc = C["epsc"]
    with Pool_(P) as pl:
        fw, r_fw = pl.sb("fw", [128, D], F32)
        T.dma("sp", fw[:], P.dram["fnw"].partition_broadcast(128), R=[P.dres["fnw"]], W=[r_fw])
        xt = [pl.sb("p6x%d" % i, [128, D], F32) for i in range(2)]
        junk, r_junk = pl.sb("p6j", [128, D], BF16)
        ss, r_ss = pl.sb("p6s", [128, 1], F32)
        for tt in range(NT):
            x_t, x_r = xt[tt % 2]
            T.dma("sp", x_t[:], P.dram[xsrc][tt * 128:(tt + 1) * 128, :], R=[P.dres[xsrc]], W=[x_r])
            T.op("dve", lambda e: e.memset(ss[:], 0.0), W=[r_ss])
            T.op("act", lambda e: e.activation(out=junk[:], in_=x_t[:], func=AF.Square, accum_out=ss[:]), R=[x_r], W=[r_junk, r_ss])
            T.op("act", lambda e: e.activation(out=ss[:], in_=ss[:], func=AF.Sqrt, bias=epsc[:], scale=1.0 / D), R=[r_ss, r_epsc], W=[r_ss])
            T.op("dve", lambda e: e.reciprocal(out=ss[:], in_=ss[:]), R=[r_ss], W=[r_ss])
            T.op("dve", lambda e: e.scalar_tensor_tensor(out=x_t[:], in0=x_t[:], scalar=ss[:], in1=fw[:], op0=ALU.mult, op1=ALU.mult),
                 R=[x_r, r_ss, r_fw], W=[x_r])
            T.dma("sp", P.dram["out"][tt * 128:(tt + 1) * 128, :], x_t[:], R=[x_r], W=[P.dres["out"]])


def build_program(layers=range(DEPTH), final=True, test_outputs=()):
    P = Prog(test_outputs=test_outputs)
    with P.es:
        declare_io(P, layers=layers)
        declare_scratch(P)
        with Pool_(P) as pc:
            C = load_consts(P, pc)
            src = "x"
            for l in layers:
                dst = "xa" if src != "xa" else "xb"
                phase1(P, C, l, src)
                phase_attn(P, C, l)
                phase_gdn(P, C, l)
                phase3(P, C, l, src)
                phase4(P, C, l, dst)
                src = dst
            if final:
                phase_final(P, C, src)
        P.T.barrier()
    return P


_CACHE = {}


def kernel(**inputs):
    inputs = {k: np.asarray(v) for k, v in inputs.items()}
    if "P" not in _CACHE:
        _CACHE["P"] = build_program()
    P = _CACHE["P"]
    consts = host_consts()
    need = set(k for k in P.dram.keys())
    in_maps = []
    for c in range(8):
        m = core_inputs(inputs, c // 2, c % 2, consts)
        in_maps.append({k: v for k, v in m.items() if k in need})
    res = run_bass_kernel_spmd(P.nc, in_maps, core_ids=list(range(8)))
    out = np.stack([np.asarray(res.results[2 * b]["out"]) for b in range(4)], 0)
    return out.astype(np.float32)
```

```python
import contextlib
import math
import numpy as np
import ml_dtypes
import concourse.bass as bass
import concourse.mybir as mybir
from concourse.bass_utils import run_bass_kernel_spmd

F32 = mybir.dt.float32
BF16 = mybir.dt.bfloat16
I32 = mybir.dt.int32
AF = mybir.ActivationFunctionType
ALU = mybir.AluOpType
AX = mybir.AxisListType

S = 4096
D = 2048
KC = 16
NT = 32
DEPTH = 2
EPS = 1e-6
NCOL = 3600
GH = 4
DH = 2
NE = 8
CAP = 512


class Res:
    __slots__ = ("name", "w", "r", "dsem", "dname", "dcnt")

    def __init__(self, name):
        self.name = name
        self.w = None
        self.r = {}
        self.dsem = None
        self.dname = None
        self.dcnt = 0


class Tracker:
    def __init__(self, nc, es):
        self.nc = nc
        self.es = es
        self.eng = {"pe": nc.tensor, "dve": nc.vector, "act": nc.scalar, "pool": nc.gpsimd, "sp": nc.sync}
        self.sems = {}
        self.cnt = {}
        for k in ("pe", "dve", "act", "pool"):
            self.sems[k] = es.enter_context(nc.semaphore("e_" + k))
            self.cnt[k] = 0
        self.waited = {k: {} for k in self.eng}
        self.dcount = {}
        self.dfree = []
        self.ninst = 0

    def res(self, name):
        return Res(name)

    def _need(self, need, en, tk, raw):
        sname, val, owner = tk
        if owner == en and (en == "pe" or not raw):
            return
        if self.waited[en].get(sname, 0) >= val:
            return
        if need.get(sname, 0) < val:
            need[sname] = val

    def _emit(self, en, need):
        for sname, val in need.items():
            self.waited[en][sname] = val
            self.eng[en].wait_ge(self.sems[sname], val)
            self.ninst += 1

    def _wait(self, en, tk, raw):
        need = {}
        self._need(need, en, tk, raw)
        self._emit(en, need)

    def _deps(self, en, R, W, nowaw=False):
        need = {}
        for r in R:
            if r.w is not None:
                self._need(need, en, r.w, True)
        for w in W:
            if w.w is not None and not (nowaw and w.w[2] is None):
                self._need(need, en, w.w, False)
            for tk in w.r.values():
                self._need(need, en, tk, False)
        self._emit(en, need)

    def op(self, en, fn, R=(), W=()):
        self._deps(en, R, W)
        inst = fn(self.eng[en])
        self.cnt[en] += 1
        inst.then_inc(self.sems[en], 1)
        self.ninst += 1
        tk = (en, self.cnt[en], en)
        for r in R:
            r.r[en] = tk
        for w in W:
            w.w = tk
            w.r = {}
        return tk

    def dma(self, q, out, in_, R=(), W=(), nowaw=True, **kw):
        self._deps(q, R, W, nowaw=nowaw)
        inst = self.eng[q].dma_start(out=out, in_=in_, **kw)
        self._dma_done(inst, R, W)

    def _dsem(self, w):
        if w.dsem is None:
            if self.dfree:
                w.dname = self.dfree.pop()
            else:
                w.dname = "d_%d" % len(self.dcount)
                self.sems[w.dname] = self.es.enter_context(self.nc.semaphore(w.dname))
                self.dcount[w.dname] = 0
            w.dsem = self.sems[w.dname]

    def release(self, w):
        if w.dsem is not None:
            self.dfree.append(w.dname)
            w.dsem = None
            w.dname = None

    def _dma_done(self, inst, R, W, inc=16):
        w = W[0]
        self._dsem(w)
        self.dcount[w.dname] += inc
        if inc == 16:
            inst.then_inc(w.dsem, 16)
        else:
            inst.then_inc(w.dsem)
        self.ninst += 1
        tk = (w.dname, self.dcount[w.dname], None)
        for r in R:
            r.r[w.dname] = tk
        for ww in W:
            ww.w = tk
            ww.r = {}

    def barrier(self):
        for en in self.eng:
            need = {}
            for k in ("pe", "dve", "act", "pool"):
                if k != en and self.cnt[k] > 0:
                    self._need(need, en, (k, self.cnt[k], k), True)
            for nm, c in self.dcount.items():
                if c > 0:
                    self._need(need, en, (nm, c, None), True)
            self._emit(en, need)


def bf(a):
    return np.ascontiguousarray(a.astype(ml_dtypes.bfloat16))


def host_consts():
    c = {}
    c["ident_bf"] = bf(np.eye(128, dtype=np.float32))
    c["ident_f"] = np.eye(128, dtype=np.float32)
    rot = np.zeros((128, 128), np.float32)
    for m in range(64):
        rot[m + 64, m] = -1.0
        rot[m, m + 64] = 1.0
    c["rotT"] = bf(rot)
    c["ones_bf"] = bf(np.ones((128, 128), np.float32))
    half = 64
    inv_freq = (np.float32(10000.0) ** (-(np.arange(half, dtype=np.float32) / np.float32(half)))).astype(np.float32)
    ang = (np.arange(S, dtype=np.float32)[None, :] * inv_freq[:, None]).astype(np.float32)
    c["cos2"] = np.ascontiguousarray(np.concatenate([np.cos(ang), np.cos(ang)], 0).astype(np.float32))
    c["sin2"] = np.ascontiguousarray(np.concatenate([np.sin(ang), np.sin(ang)], 0).astype(np.float32))
    s = np.arange(128)[:, None]
    cc = np.arange(128)[None, :]
    same = (s // 64) == (cc // 64)
    m = np.zeros((9, 128, 128), np.float32)
    m[0] = same & (s < cc)
    m[1] = same & (s <= cc)
    m[2] = same & (s > cc)
    m[3] = same & (s >= cc)
    m[4] = same
    m[5] = (s < 64) & (cc >= 0)
    m[6] = (s >= 64) & (cc >= 0)
    m[7] = 1.0
    m[8] = (s < cc)
    c["masks"] = np.ascontiguousarray(m.transpose(1, 0, 2))
    c["iota"] = np.ascontiguousarray(np.tile(np.arange(512, dtype=np.float32)[None, :], (128, 1)))
    return c


def core_inputs(inputs, b, hf, consts):
    d = dict(consts)
    d["x"] = np.ascontiguousarray(inputs["x"][b])
    g0 = 512 * hf
    for l in range(DEPTH):
        w = inputs["w_in"][l]
        base = [0, 1024, 2048, 3072]
        cols = []
        cols += list(range(0 + g0, 0 + g0 + 512))
        cols += list(range(1024 + g0, 1024 + g0 + 512))
        cols += list(range(2048 + g0, 2048 + g0 + 512))
        dq0 = 4096 + 32
        cols += list(range(dq0 + g0, dq0 + g0 + 512))
        cols += list(range(dq0 + 1024 + g0, dq0 + 1024 + g0 + 512))
        cols += list(range(3072 + g0, 3072 + g0 + 512))
        cols += list(range(dq0 + 2048 + g0, dq0 + 2048 + g0 + 512))
        hb = [4096 + dd * 8 + 4 * hf + h for dd in range(2) for h in range(4)]
        ha = [4096 + 16 + dd * 8 + 4 * hf + h for dd in range(2) for h in range(4)]
        cols += hb + ha
        d["w_in%d" % l] = np.ascontiguousarray(w[:, cols])
        cw = inputs["conv_w"][l]
        ccols = []
        for grp in range(3):
            ccols += list(range(grp * 1024 + g0, grp * 1024 + g0 + 512))
        cws = cw[:, ccols]
        d["conv%d" % l] = np.ascontiguousarray(cws.reshape(5, 12, 128).transpose(2, 1, 0))
        al = inputs["a_log"][l][:, 4 * hf:4 * hf + 4].reshape(8, 1)
        dtb = inputs["dt_bias"][l][:, 4 * hf:4 * hf + 4].reshape(8, 1)
        d["alog%d" % l] = np.ascontiguousarray(al)
        d["dtb%d" % l] = np.ascontiguousarray(dtb)
        d["n1w%d" % l] = np.ascontiguousarray(inputs["norm1_w"][l].reshape(1, D))
        d["dlam%d" % l] = np.ascontiguousarray(inputs["diff_lambda"][l].reshape(1, 512))
        d["subw%d" % l] = np.ascontiguousarray(inputs["diff_subln_w"][l].reshape(1, 256))
        d["gnw%d" % l] = np.ascontiguousarray(inputs["gdn_norm_w"][l].reshape(1, 128))
        rows = list(range(0, 512)) + list(range(1024, 1536)) + list(range(512, 1024)) + list(range(1536, 2048))
        d["w_out%d" % l] = np.ascontiguousarray(inputs["w_out"][l][rows, :])
        ecols = list(range(8 * hf, 8 * hf + 8)) + list(range(8 * (1 - hf), 8 * (1 - hf) + 8))
        d["w_r%d" % l] = np.ascontiguousarray(inputs["w_router"][l][:, ecols])
        d["n2w%d" % l] = np.ascontiguousarray(inputs["norm2_w"][l].reshape(1, D))
        d["w_g%d" % l] = np.ascontiguousarray(inputs["w_gate"][l][8 * hf:8 * hf + 8])
        d["w_u%d" % l] = np.ascontiguousarray(inputs["w_up"][l][8 * hf:8 * hf + 8])
        d["w_d%d" % l] = np.ascontiguousarray(inputs["w_down"][l][8 * hf:8 * hf + 8])
    d["fnw"] = np.ascontiguousarray(inputs["final_norm_w"].reshape(1, D))
    return d


class Prog:
    def __init__(self, test_outputs=(), test_inputs=()):
        self.nc = bass.Bass("TRN2", target_bir_lowering=False)
        self.es = contextlib.ExitStack()
        self.T = Tracker(self.nc, self.es)
        self.test_outputs = set(test_outputs)
        self.test_inputs = set(test_inputs)
        self.dram = {}
        self.dres = {}

    def din(self, name, shape, dt):
        t = self.nc.dram_tensor(name, list(shape), dt, kind="ExternalInput").ap()
        self.dram[name] = t
        self.dres[name] = Res(name)
        return t

    def dscr(self, name, shape, dt, out=False):
        kind = "ExternalOutput" if (out or name in self.test_outputs) else "Internal"
        if name in self.test_inputs:
            kind = "ExternalInput"
        t = self.nc.dram_tensor(name, list(shape), dt, kind=kind).ap()
        self.dram[name] = t
        self.dres[name] = Res(name)
        return t


class Pool_:
    def __init__(self, P):
        self.P = P
        self.es = contextlib.ExitStack()
        self.created = []

    def __enter__(self):
        self.es.__enter__()
        return self

    def __exit__(self, *a):
        if a[0] is None:
            self.P.T.barrier()
            for r in self.created:
                self.P.T.release(r)
        return self.es.__exit__(*a)

    _uid = [0]

    def sb(self, name, shape, dt):
        Pool_._uid[0] += 1
        name = "%s_u%d" % (name, Pool_._uid[0])
        t = self.es.enter_context(self.P.nc.sbuf_tensor(name, list(shape), dt))
        r = Res(name)
        self.created.append(r)
        return t, r

    def ps(self, name, shape, dt):
        Pool_._uid[0] += 1
        name = "%s_u%d" % (name, Pool_._uid[0])
        t = self.es.enter_context(self.P.nc.psum_tensor(name, list(shape), dt))
        return t, Res(name)


def declare_io(P, layers=range(DEPTH)):
    P.din("x", [S, D], F32)
    P.din("ident_bf", [128, 128], BF16)
    P.din("ident_f", [128, 128], F32)
    P.din("rotT", [128, 128], BF16)
    P.din("ones_bf", [128, 128], BF16)
    P.din("cos2", [128, S], F32)
    P.din("sin2", [128, S], F32)
    P.din("masks", [128, 9, 128], F32)
    P.din("iota", [128, 512], F32)
    P.din("fnw", [1, D], F32)
    for l in layers:
        P.din("w_in%d" % l, [D, NCOL], F32)
        P.din("conv%d" % l, [128, 12, 5], F32)
        P.din("alog%d" % l, [8, 1], F32)
        P.din("dtb%d" % l, [8, 1], F32)
        P.din("n1w%d" % l, [1, D], F32)
        P.din("dlam%d" % l, [1, 512], F32)
        P.din("subw%d" % l, [1, 256], F32)
        P.din("gnw%d" % l, [1, 128], F32)
        P.din("w_out%d" % l, [D, D], F32)
        P.din("w_r%d" % l, [D, 16], F32)
        P.din("n2w%d" % l, [1, D], F32)
        P.din("w_g%d" % l, [NE, D, D], F32)
        P.din("w_u%d" % l, [NE, D, D], F32)
        P.din("w_d%d" % l, [NE, D, D], F32)


def declare_scratch(P):
    P.dscr("gqT", [GH, 128, S], BF16)
    P.dscr("gkT", [GH, 128, S], BF16)
    P.dscr("gk_tm", [GH, S, 128], BF16)
    P.dscr("gv_tm", [GH, S, 128], BF16)
    P.dscr("dqT", [4, 128, S], BF16)
    P.dscr("dkT", [4, 128, S], BF16)
    P.dscr("siluz", [S, 512], BF16)
    P.dscr("dv", [S, 512], BF16)
    P.dscr("gsc", [6, 128, NT * 8], F32)
    P.dscr("mixed_half", [S, 1024], BF16)
    P.dscr("mg", [4, 2, 1024, 1024], BF16)
    P.dscr("x1", [S, D], F32)
    P.dscr("x2", [S, D], BF16)
    P.dscr("affd", [128, NT * 16], F32)
    P.dscr("ypart", [S, D], BF16)
    P.dscr("yg", [8, 2, 512, D], BF16)
    P.dscr("xa", [S, D], F32)
    P.dscr("xb", [S, D], F32)
    P.dscr("out", [S, D], F32, out=True)


def load_consts(P, pool):
    T = P.T
    C = {}
    for nm, shape, dt in (("ident_bf", [128, 128], BF16), ("ident_f", [128, 128], F32), ("rotT", [128, 128], BF16),
                          ("ones_bf", [128, 128], BF16), ("masks", [128, 9, 128], F32)):
        t, r = pool.sb("c_" + nm, shape, dt)
        T.dma("sp", t[:], P.dram[nm], R=[P.dres[nm]], W=[r])
        C[nm] = (t, r)
    for nm, val in (("epsc", EPS), ("onec", 1.0)):
        t, r = pool.sb("c_" + nm, [128, 1], F32)
        T.op("dve", lambda e: e.memset(t[:], val), W=[r])
        C[nm] = (t, r)
    return C


def phase1(P, C, l, xsrc, upto=None, sections=("gdn", "rope", "tm", "bd"), dbg=None):
    T = P.T
    nc = P.nc
    ident_bf, r_ident_bf = C["ident_bf"]
    ident_f, r_ident_f = C["ident_f"]
    rotT, r_rotT = C["rotT"]
    ones_bf, r_ones = C["ones_bf"]
    masks, r_masks = C["masks"]
    epsc, r_epsc = C["epsc"]
    onec, r_onec = C["onec"]
    xd = P.dram[xsrc]
    xr = P.dres[xsrc]
    wd = P.dram["w_in%d" % l].rearrange("(kc p) n -> p kc n", p=128)
    wr = P.dres["w_in%d" % l]
    with Pool_(P) as pl:
        nT, r_nT = pl.sb("nT", [128, KC, S], BF16)
        with Pool_(P) as pa:
            w1b, r_w1b = pa.sb("w1b", [128, D], F32)
            T.dma("sp", w1b[:], P.dram["n1w%d" % l].partition_broadcast(128), R=[P.dres["n1w%d" % l]], W=[r_w1b])
            xt = [pa.sb("xt%d" % i, [128, D], F32) for i in range(2)]
            nt = [pa.sb("nt%d" % i, [128, D], BF16) for i in range(2)]
            junk, r_junk = pa.sb("junk", [128, D], BF16)
            ss = [pa.sb("ss%d" % i, [128, 1], F32) for i in range(2)]
            rstd = [pa.sb("rstd%d" % i, [128, 1], F32) for i in range(2)]
            ptr = [pa.ps("ptr%d" % i, [128, 8, 128], BF16) for i in range(2)]
            for tt in range(NT):
                i = tt % 2
                x_t, x_r = xt[i]
                n_t, n_r = nt[i]
                T.dma("sp", x_t[:], xd[tt * 128:(tt + 1) * 128, :], R=[xr], W=[x_r])
                T.op("dve", lambda e: e.memset(ss[i][0][:], 0.0), W=[ss[i][1]])
                T.op("act", lambda e: e.activation(out=junk[:], in_=x_t[:], func=AF.Square, accum_out=ss[i][0][:]),
                     R=[x_r], W=[r_junk, ss[i][1]])
                T.op("act", lambda e: e.activation(out=rstd[i][0][:], in_=ss[i][0][:], func=AF.Sqrt, bias=epsc[:],
                                                   scale=1.0 / D), R=[ss[i][1], r_epsc], W=[rstd[i][1]])
                T.op("dve", lambda e: e.reciprocal(out=rstd[i][0][:], in_=rstd[i][0][:]), R=[rstd[i][1]], W=[rstd[i][1]])
                T.op("dve", lambda e: e.scalar_tensor_tensor(out=n_t[:], in0=x_t[:], scalar=rstd[i][0][:], in1=w1b[:],
                                                             op0=ALU.mult, op1=ALU.mult),
                     R=[x_r, rstd[i][1], r_w1b], W=[n_r])
                for hh in range(2):
                    p_t, p_r = ptr[hh]
                    for k in range(8):
                        kc = hh * 8 + k
                        T.op("pe", lambda e: e.transpose(out=p_t[:, k, :], in_=n_t[:, kc * 128:(kc + 1) * 128],
                                                         identity=ident_bf[:]),
                             R=[n_r, r_ident_bf], W=[p_r])
                    en = "act" if hh == 0 else "dve"
                    if en == "act":
                        T.op("act", lambda e: e.copy(out=nT[:, hh * 8:(hh + 1) * 8, tt * 128:(tt + 1) * 128], in_=p_t[:]),
                             R=[p_r], W=[r_nT])
                    else:
                        T.op("dve", lambda e: e.tensor_copy(out=nT[:, hh * 8:(hh + 1) * 8, tt * 128:(tt + 1) * 128],
                                                            in_=p_t[:]), R=[p_r], W=[r_nT])
        if upto == "1a":
            if "dbg_nT" in P.dram:
                T.dma("sp", P.dram["dbg_nT"], nT[:], R=[r_nT], W=[P.dres["dbg_nT"]])
            return
        with Pool_(P) as pb:
            wblk = [pb.sb("wblk%d" % i, [128, KC, 256], BF16) for i in range(2)]
            raw = [pb.sb("raw%d" % i, [128, S + 4], BF16) for i in range(2)]
            acc = [pb.sb("acc%d" % i, [128, 1024], F32) for i in range(2)]
            sl = [pb.sb("sl%d" % i, [128, 1024], F32) for i in range(2)]
            sq = [pb.sb("sq%d" % i, [128, 1024], BF16) for i in range(1)] * 2
            rn = [pb.sb("rn%d" % i, [128, 1024], F32) for i in range(1)] * 2
            ob = [pb.sb("ob%d" % i, [128, 1024], BF16) for i in range(2)]
            tm = [pb.sb("tm%d" % i, [128, 8, 128], BF16) for i in range(1)] * 2
            cst = [pb.sb("cst%d" % i, [128, 512], F32) for i in range(1)] * 2
            snt = [pb.sb("snt%d" % i, [128, 512], F32) for i in range(1)] * 2
            tbf = [pb.sb("tbf%d" % i, [128, 512], BF16) for i in range(1)] * 2
            ra = [pb.sb("ra%d" % i, [128, 512], F32) for i in range(1)] * 2
            rb = [pb.sb("rb%d" % i, [128, 512], F32) for i in range(1)] * 2
            convw, r_convw = pb.sb("convw", [128, 12, 5], F32)
            T.dma("sp", convw[:], P.dram["conv%d" % l], R=[P.dres["conv%d" % l]], W=[r_convw])
            pacc = [pb.ps("pacc%d" % i, [128, 512], F32) for i in range(2)]
            prot = [pb.ps("prot%d" % i, [128, 512], F32) for i in range(1)]
            pss = [pb.ps("pss%d" % i, [128, 1024], F32) for i in range(1)]
            ptm = [pb.ps("ptm%d" % i, [128, 8, 128], BF16) for i in range(2)]
            for i in range(2):
                T.op("dve", lambda e: e.memset(raw[i][0][:], 0.0), W=[raw[i][1]])
            nblk = [0]

            def load_w(c0, ncols):
                i = nblk[0] % 2
                nblk[0] += 1
                w_t, w_r = wblk[i]
                for q4 in range(4):
                    T.dma("pool", w_t[:, q4 * 4:(q4 + 1) * 4, 0:ncols], wd[:, q4 * 4:(q4 + 1) * 4, c0:c0 + ncols],
                          R=[wr], W=[w_r])
                return w_t, w_r

            cnt = {"pacc": 0, "ptm": 0, "st": 0, "rp": 0}

            def proj_fm(w_t, w_r, cw, tb, m=128):
                i = cnt["pacc"] % 2
                cnt["pacc"] += 1
                p_t, p_r = pacc[i]
                for kc in range(KC):
                    T.op("pe", lambda e: e.matmul(p_t[0:m, :], lhsT=w_t[:, kc, cw:cw + m],
                                                  rhs=nT[:, kc, tb * 512:(tb + 1) * 512],
                                                  start=(kc == 0), stop=(kc == KC - 1)),
                         R=[w_r, r_nT], W=[p_r])
                return p_t, p_r

            for j in (range(12) if "gdn" in sections else ()):
                grp, h = j // 4, j % 4
                if j % 2 == 0:
                    w_t, w_r = load_w(j * 128, 256)
                cw = (j % 2) * 128
                r_t, r_r = raw[j % 2]
                for tb in range(8):
                    p_t, p_r = proj_fm(w_t, w_r, cw, tb)
                    T.op("act", lambda e: e.copy(out=r_t[:, 2 + tb * 512:2 + (tb + 1) * 512], in_=p_t[:]),
                         R=[p_r], W=[r_r])
                if j == 0 and "dbg_raw" in P.dram:
                    T.dma("sp", P.dram["dbg_raw"], r_t[:], R=[r_r], W=[P.dres["dbg_raw"]])
                    return
                for blk in range(4):
                    si = cnt["st"] % 2
                    cnt["st"] += 1
                    a_t, a_r = acc[si]
                    s_t, s_r = sl[si]
                    o_t, o_r = ob[si]
                    t0 = blk * 1024
                    T.op("dve", lambda e: e.tensor_scalar(out=a_t[:], in0=r_t[:, t0:t0 + 1024], scalar1=convw[:, j, 0:1],
                                                          scalar2=None, op0=ALU.mult), R=[r_r, r_convw], W=[a_r])
                    for k in range(1, 5):
                        T.op("dve", lambda e: e.scalar_tensor_tensor(out=a_t[:], in0=r_t[:, t0 + k:t0 + k + 1024],
                                                                     scalar=convw[:, j, k:k + 1], in1=a_t[:],
                                                                     op0=ALU.mult, op1=ALU.add),
                             R=[r_r, r_convw, a_r], W=[a_r])
                    if grp == 2:
                        T.op("act", lambda e: e.activation(out=o_t[:], in_=a_t[:], func=AF.Silu), R=[a_r], W=[o_r])
                    else:
                        q_t, q_r = sq[si]
                        n_t, n_r = rn[si]
                        T.op("act", lambda e: e.activation(out=s_t[:], in_=a_t[:], func=AF.Silu), R=[a_r], W=[s_r])
                        T.op("act", lambda e: e.activation(out=q_t[:], in_=s_t[:], func=AF.Square), R=[s_r], W=[q_r])
                        ps_t, ps_r = pss[0]
                        for hh in range(2):
                            T.op("pe", lambda e: e.matmul(ps_t[:, hh * 512:(hh + 1) * 512], lhsT=ones_bf[:],
                                                          rhs=q_t[:, hh * 512:(hh + 1) * 512], start=True, stop=True),
                                 R=[r_ones, q_r], W=[ps_r])
                        T.op("act", lambda e: e.activation(out=n_t[:], in_=ps_t[:], func=AF.Sqrt, bias=epsc[:]),
                             R=[ps_r, r_epsc], W=[n_r])
                        T.op("dve", lambda e: e.reciprocal(out=n_t[:], in_=n_t[:]), R=[n_r], W=[n_r])
                        qs = (128.0 ** -0.5) if grp == 0 else 1.0
                        T.op("dve", lambda e: e.scalar_tensor_tensor(out=o_t[:], in0=s_t[:], scalar=qs, in1=n_t[:],
                                                                     op0=ALU.mult, op1=ALU.mult),
                             R=[s_r, n_r], W=[o_r])
                    if grp < 2:
                        dst = P.dram["gqT" if grp == 0 else "gkT"]
                        T.dma("sp", dst[h, :, t0:t0 + 1024], o_t[:], R=[o_r],
                              W=[P.dres["gqT" if grp == 0 else "gkT"]])
                    if grp >= 1:
                        pi = cnt["ptm"] % 2
                        cnt["ptm"] += 1
                        pt_t, pt_r = ptm[pi]
                        tm_t, tm_r = tm[pi]
                        for k in range(8):
                            T.op("pe", lambda e: e.transpose(out=pt_t[:, k, :], in_=o_t[:, k * 128:(k + 1) * 128],
                                                             identity=ident_bf[:]), R=[o_r, r_ident_bf], W=[pt_r])
                        T.op("act", lambda e: e.copy(out=tm_t[:], in_=pt_t[:]), R=[pt_r], W=[tm_r])
                        nm = "gk_tm" if grp == 1 else "gv_tm"
                        T.dma("sp", P.dram[nm][h, t0:t0 + 1024, :].rearrange("(t p) d -> p t d", p=128), tm_t[:],
                              R=[tm_r], W=[P.dres[nm]])
            if upto == "gdn":
                return
            for j in (dbg["rope_j"] if dbg else (range(12, 20) if "rope" in sections else ())):
                jj = j - 12
                isq, cj = (jj < 4), jj % 4
                if j % 2 == 0:
                    w_t, w_r = load_w(j * 128, 256)
                cw = (j % 2) * 128
                nm = "dqT" if isq else "dkT"
                for tb in (dbg["rope_tb"] if dbg else range(8)):
                    i = cnt["rp"] % 2
                    cnt["rp"] += 1
                    c_t, c_r = cst[i]
                    s_t, s_r = snt[i]
                    sk = dbg.get("skip", "") if dbg else ""
                    if "c" in sk:
                        T.op("dve", lambda e: e.memset(c_t[:], 1.0), W=[c_r])
                        T.op("dve", lambda e: e.memset(s_t[:], 0.0), W=[s_r])
                    else:
                        T.dma("sp", c_t[:], P.dram["cos2"][:, tb * 512:(tb + 1) * 512], R=[P.dres["cos2"]], W=[c_r])
                        T.dma("sp", s_t[:], P.dram["sin2"][:, tb * 512:(tb + 1) * 512], R=[P.dres["sin2"]], W=[s_r])
                    p_t, p_r = proj_fm(w_t, w_r, cw, tb)
                    b_t, b_r = tbf[i]
                    T.op("act", lambda e: e.copy(out=b_t[:], in_=p_t[:]), R=[p_r], W=[b_r])
                    pr_t, pr_r = prot[0]
                    if "r" in sk:
                        T.op("pe", lambda e: e.matmul(pr_t[:], lhsT=ones_bf[:], rhs=b_t[:], start=True, stop=True),
                             R=[r_ones, b_r], W=[pr_r])
                    else:
                        T.op("pe", lambda e: e.matmul(pr_t[:], lhsT=rotT[:], rhs=b_t[:], start=True, stop=True),
                             R=[r_rotT, b_r], W=[pr_r])
                    a_t, a_r = ra[i]
                    bb_t, bb_r = rb[i]
                    o_t, o_r = ob[i]
                    if "m" in sk:
                        T.op("act", lambda e: e.copy(out=o_t[:, 0:512], in_=pr_t[:]), R=[pr_r], W=[o_r])
                    elif "1" in sk:
                        T.op("dve", lambda e: e.tensor_mul(out=a_t[:], in0=p_t[:], in1=c_t[:]), R=[p_r, c_r], W=[a_r])
                        T.op("act", lambda e: e.copy(out=o_t[:, 0:512], in_=a_t[:]), R=[a_r], W=[o_r])
                    elif "3" in sk:
                        T.op("dve", lambda e: e.tensor_mul(out=a_t[:], in0=c_t[:], in1=c_t[:]), R=[c_r], W=[a_r])
                        T.op("act", lambda e: e.copy(out=o_t[:, 0:512], in_=a_t[:]), R=[a_r], W=[o_r])
                    elif "2" in sk:
                        T.op("dve", lambda e: e.tensor_mul(out=a_t[:], in0=p_t[:], in1=c_t[:]), R=[p_r, c_r], W=[a_r])
                        T.op("dve", lambda e: e.tensor_mul(out=bb_t[:], in0=pr_t[:], in1=s_t[:]), R=[pr_r, s_r], W=[bb_r])
                        T.op("act", lambda e: e.copy(out=o_t[:, 0:512], in_=bb_t[:]), R=[a_r, bb_r], W=[o_r])
                    else:
                        T.op("act", lambda e: e.copy(out=a_t[:], in_=p_t[:]), R=[p_r], W=[a_r])
                        T.op("act", lambda e: e.copy(out=bb_t[:], in_=pr_t[:]), R=[pr_r], W=[bb_r])
                        T.op("dve", lambda e: e.tensor_mul(out=a_t[:], in0=a_t[:], in1=c_t[:]),
                             R=[a_r, c_r], W=[a_r])
                        T.op("dve", lambda e: e.tensor_mul(out=bb_t[:], in0=bb_t[:], in1=s_t[:]),
                             R=[bb_r, s_r], W=[bb_r])
                        T.op("dve", lambda e: e.tensor_add(out=o_t[:, 0:512], in0=a_t[:], in1=bb_t[:]),
                             R=[a_r, bb_r], W=[o_r])
                    T.dma("sp", P.dram[nm][cj, :, tb * 512:(tb + 1) * 512], o_t[:, 0:512], R=[o_r], W=[P.dres[nm]])
            if upto == "rope":
                return
            for g in range(2):
                nm = "siluz" if g == 0 else "dv"
                for cb in range(2):
                    c0 = 2560 + g * 512 + cb * 256
                    w_t, w_r = load_w(c0, 256)
                    for tt in range(NT):
                        i = cnt["pacc"] % 2
                        cnt["pacc"] += 1
                        p_t, p_r = pacc[i]
                        for kc in range(KC):
                            T.op("pe", lambda e: e.matmul(p_t[:, 0:256], lhsT=nT[:, kc, tt * 128:(tt + 1) * 128],
                                                          rhs=w_t[:, kc, 0:256], start=(kc == 0), stop=(kc == KC - 1)),
                                 R=[w_r, r_nT], W=[p_r])
                        si = cnt["st"] % 2
                        cnt["st"] += 1
                        o_t, o_r = ob[si]
                        T.op("act", lambda e: e.activation(out=o_t[:, 0:256], in_=p_t[:, 0:256],
                                                           func=(AF.Silu if g == 0 else AF.Copy)), R=[p_r], W=[o_r])
                        T.dma("sp", P.dram[nm][tt * 128:(tt + 1) * 128, cb * 256:(cb + 1) * 256], o_t[:, 0:256],
                              R=[o_r], W=[P.dres[nm]])
        if upto == "tm":
            return
        with Pool_(P) as pg:
            w_t, w_r = pg.sb("wsm", [128, KC, 16], BF16)
            T.dma("pool", w_t[:], wd[:, :, 3584:3600], R=[wr], W=[w_r])
            pacc2 = [pg.ps("pacc2_%d" % i, [128, 512], F32) for i in range(2)]
            psm = [pg.ps("psm0", [128, 512], F32)]
            pcnt = [0]

            def proj_fm(w_t, w_r, cw, tb, m=128):
                i = pcnt[0] % 2
                pcnt[0] += 1
                p_t, p_r = pacc2[i]
                for kc in range(KC):
                    T.op("pe", lambda e: e.matmul(p_t[0:m, :], lhsT=w_t[:, kc, cw:cw + m],
                                                  rhs=nT[:, kc, tb * 512:(tb + 1) * 512],
                                                  start=(kc == 0), stop=(kc == KC - 1)),
                         R=[w_r, r_nT], W=[p_r])
                return p_t, p_r
            bfm, r_bfm = pg.sb("bfm", [8, S], F32)
            gfm, r_gfm = pg.sb("gfm", [8, S], F32)
            alog, r_alog = pg.sb("alog", [8, 1], F32)
            dtb, r_dtb = pg.sb("dtb", [8, 1], F32)
            nega, r_nega = pg.sb("nega", [8, 1], F32)
            T.dma("sp", alog[:], P.dram["alog%d" % l], R=[P.dres["alog%d" % l]], W=[r_alog])
            T.dma("sp", dtb[:], P.dram["dtb%d" % l], R=[P.dres["dtb%d" % l]], W=[r_dtb])
            T.op("act", lambda e: e.activation(out=nega[:], in_=alog[:], func=AF.Exp), R=[r_alog], W=[r_nega])
            T.op("dve", lambda e: e.tensor_scalar(out=nega[:], in0=nega[:], scalar1=-1.0, scalar2=None, op0=ALU.mult),
                 R=[r_nega], W=[r_nega])
            for tb in range(8):
                p_t, p_r = proj_fm(w_t, w_r, 0, tb, m=8)
                T.op("act", lambda e: e.activation(out=bfm[:, tb * 512:(tb + 1) * 512], in_=p_t[0:8, :],
                                                   func=AF.Sigmoid), R=[p_r], W=[r_bfm])
            for tb in range(8):
                p_t, p_r = proj_fm(w_t, w_r, 8, tb, m=8)
                T.op("act", lambda e: e.activation(out=gfm[:, tb * 512:(tb + 1) * 512], in_=p_t[0:8, :],
                                                   func=AF.Exp, bias=dtb[:]), R=[p_r, r_dtb], W=[r_gfm])
            T.op("act", lambda e: e.activation(out=gfm[:], in_=gfm[:], func=AF.Ln, bias=onec[0:8, :]), R=[r_gfm, r_onec], W=[r_gfm])
            T.op("dve", lambda e: e.tensor_scalar(out=gfm[:], in0=gfm[:], scalar1=nega[:], scalar2=None, op0=ALU.mult),
                 R=[r_gfm, r_nega], W=[r_gfm])
            btm, r_btm = pg.sb("btm", [128, NT, 8], F32)
            gtm, r_gtm = pg.sb("gtm", [128, NT, 8], F32)
            gcs, r_gcs = pg.sb("gcs", [128, NT, 8], F32)
            gto, r_gto = pg.sb("gto", [128, NT, 8], F32)
            ex1, r_ex1 = pg.sb("ex1", [128, NT, 8], F32)
            ex2, r_ex2 = pg.sb("ex2", [128, NT, 8], F32)
            ex3, r_ex3 = pg.sb("ex3", [128, 2, NT, 8], F32)
            ps_t, ps_r = psm[0]
            psv = ps_t[:, 0:256].rearrange("p (t h) -> p t h", h=8)
            for src, r_src, dst, r_dst in ((bfm, r_bfm, btm, r_btm), (gfm, r_gfm, gtm, r_gtm)):
                for tt in range(NT):
                    T.op("pe", lambda e: e.transpose(out=psv[:, tt, :], in_=src[:, tt * 128:(tt + 1) * 128],
                                                     identity=ident_f[0:8, 0:8]), R=[r_src, r_ident_f], W=[ps_r])
                T.op("dve", lambda e: e.tensor_copy(out=dst[:], in_=psv), R=[ps_r], W=[r_dst])
            T.op("pe", lambda e: e.matmul(psv[:, :, 0:4], lhsT=masks[:, 1, :], rhs=gtm[:, :, 0:4], start=True, stop=True),
                 R=[r_masks, r_gtm], W=[ps_r])
            T.op("pe", lambda e: e.matmul(psv[:, :, 4:8], lhsT=masks[:, 3, :], rhs=gtm[:, :, 4:8], start=True, stop=True),
                 R=[r_masks, r_gtm], W=[ps_r])
            T.op("dve", lambda e: e.tensor_copy(out=gcs[:], in_=psv), R=[ps_r], W=[r_gcs])
            T.op("pe", lambda e: e.matmul(ps_t[:, 0:256], lhsT=masks[:, 4, :], rhs=gtm[:].rearrange("p t h -> p (t h)"),
                                          start=True, stop=True), R=[r_masks, r_gtm], W=[ps_r])
            T.op("dve", lambda e: e.tensor_copy(out=gto[:], in_=psv), R=[ps_r], W=[r_gto])
            T.op("act", lambda e: e.activation(out=ex1[:], in_=gcs[:], func=AF.Exp), R=[r_gcs], W=[r_ex1])
            T.op("dve", lambda e: e.tensor_tensor(out=ex2[:], in0=gto[:], in1=gcs[:], op=ALU.subtract),
                 R=[r_gto, r_gcs], W=[r_ex2])
            T.op("act", lambda e: e.activation(out=ex2[:], in_=ex2[:], func=AF.Exp), R=[r_ex2], W=[r_ex2])
            for ab in range(2):
                T.op("pe", lambda e: e.matmul(ps_t[:, 0:256], lhsT=masks[:, 5 + ab, :],
                                              rhs=gtm[:].rearrange("p t h -> p (t h)"), start=True, stop=True),
                     R=[r_masks, r_gtm], W=[ps_r])
                T.op("act", lambda e: e.activation(out=ex3[:, ab], in_=psv, func=AF.Exp), R=[ps_r], W=[r_ex3])
            gsc = P.dram["gsc"]
            rg = P.dres["gsc"]
            for k, (src, r_src) in enumerate(((btm, r_btm), (gcs, r_gcs), (ex1, r_ex1), (ex2, r_ex2))):
                T.dma("sp", gsc[k], src[:].rearrange("p t h -> p (t h)"), R=[r_src], W=[rg])
            for ab in range(2):
                T.dma("sp", gsc[4 + ab], ex3[:, ab].rearrange("p t h -> p (t h)"), R=[r_ex3], W=[rg])
    T.barrier()


def phase_attn(P, C, l):
    T = P.T
    lam_init = 0.8 - 0.6 * math.exp(-0.3 * l)
    scale = 128.0 ** -0.5
    epsc, r_epsc = C["epsc"]
    mh = P.dram["mixed_half"]
    r_mh = P.dres["mixed_half"]
    with Pool_(P) as pl:
        dlb, r_dlb = pl.sb("dlb", [128, 512], F32)
        prod, r_prod = pl.sb("prod", [128, 256], F32)
        s12, r_s12 = pl.sb("s12", [128, 2], F32)
        nlam, r_nlam = pl.sb("nlam", [128, 1], F32)
        subw, r_subw = pl.sb("subw", [128, 256], F32)
        T.dma("sp", dlb[:], P.dram["dlam%d" % l].partition_broadcast(128), R=[P.dres["dlam%d" % l]], W=[r_dlb])
        T.dma("sp", subw[:], P.dram["subw%d" % l].partition_broadcast(128), R=[P.dres["subw%d" % l]], W=[r_subw])
        T.op("dve", lambda e: e.tensor_mul(out=prod[:, 0:128], in0=dlb[:, 0:128], in1=dlb[:, 128:256]), R=[r_dlb], W=[r_prod])
        T.op("dve", lambda e: e.tensor_mul(out=prod[:, 128:256], in0=dlb[:, 256:384], in1=dlb[:, 384:512]), R=[r_dlb], W=[r_prod])
        T.op("dve", lambda e: e.reduce_sum(out=s12[:, 0:1], in_=prod[:, 0:128], axis=AX.X), R=[r_prod], W=[r_s12])
        T.op("dve", lambda e: e.reduce_sum(out=s12[:, 1:2], in_=prod[:, 128:256], axis=AX.X), R=[r_prod], W=[r_s12])
        T.op("act", lambda e: e.activation(out=s12[:], in_=s12[:], func=AF.Exp), R=[r_s12], W=[r_s12])
        T.op("dve", lambda e: e.tensor_sub(out=nlam[:], in0=s12[:, 1:2], in1=s12[:, 0:1]), R=[r_s12], W=[r_nlam])
        T.op("dve", lambda e: e.tensor_scalar(out=nlam[:], in0=nlam[:], scalar1=-lam_init, scalar2=None, op0=ALU.add),
             R=[r_nlam], W=[r_nlam])
        T.op("dve", lambda e: e.tensor_scalar(out=subw[:], in0=subw[:], scalar1=1.0 - lam_init, scalar2=None, op0=ALU.mult),
             R=[r_subw], W=[r_subw])
        vp, r_vp = pl.sb("vp", [128, NT, 257], BF16)
        qT2, r_qT2 = pl.sb("qT2", [128, 2, S], BF16)
        kT2, r_kT2 = pl.sb("kT2", [128, 2, S], BF16)
        pT = [pl.sb("pT%d" % i, [128, 512], BF16) for i in range(2)]
        ev = [pl.sb("ev%d" % i, [128, 257], F32) for i in range(2)]
        o0 = [pl.sb("o0_%d" % i, [128, 256], F32) for i in range(4)]
        oc, r_oc = pl.sb("oc", [128, 256], F32)
        junk, r_junk = pl.sb("junk2", [128, 256], F32)
        rc, r_rc = pl.sb("rc", [128, 1], F32)
        ssq, r_ssq = pl.sb("ssq", [128, 1], F32)
        onb = [pl.sb("onb%d" % i, [128, 256], BF16) for i in range(2)]
        pss = [pl.ps("pss%d" % i, [128, 512], F32) for i in range(2)]
        po = [pl.ps("po%d" % i, [128, 512], F32) for i in range(4)]
        n_on = 0
        for h in range(2):
            T.op("dve", lambda e: e.memset(vp[:, :, 256:257], 1.0), W=[r_vp])
            T.dma("sp", vp[:, :, 0:256], P.dram["dv"][:, h * 256:(h + 1) * 256].rearrange("(t p) d -> p t d", p=128),
                  R=[P.dres["dv"]], W=[r_vp], nowaw=False)
            for half in range(2):
                T.dma("sp", qT2[:, half, :], P.dram["dqT"][2 * h + half], R=[P.dres["dqT"]], W=[r_qT2])
                T.dma("sp", kT2[:, half, :], P.dram["dkT"][2 * h + half], R=[P.dres["dkT"]], W=[r_kT2])
            for qb in range(8):
                for half in range(2):
                    def s_mm(kt_):
                        ps_t_, ps_r_ = pss[kt_ % 2]
                        T.op("pe", lambda e: e.matmul(ps_t_[:], lhsT=kT2[:, half, kt_ * 128:(kt_ + 1) * 128],
                                                      rhs=qT2[:, half, qb * 512:(qb + 1) * 512], start=True, stop=True),
                             R=[r_kT2, r_qT2], W=[ps_r_])
                    s_mm(0)
                    for kt in range(NT):
                        ps_t, ps_r = pss[kt % 2]
                        p_t, p_r = pT[kt % 2]
                        if kt + 1 < NT:
                            s_mm(kt + 1)
                        T.op("act", lambda e: e.activation(out=p_t[:], in_=ps_t[:], func=AF.Exp, scale=scale),
                             R=[ps_r], W=[p_r])
                        for qs in range(4):
                            T.op("pe", lambda e: e.matmul(po[qs][0][:, 0:257], lhsT=p_t[:, qs * 128:(qs + 1) * 128],
                                                          rhs=vp[:, kt, :], start=(kt == 0), stop=(kt == NT - 1)),
                                 R=[p_r, r_vp], W=[po[qs][1]])
                    for qs in range(4):
                        e_t, e_r = ev[qs % 2]
                        T.op("act", lambda e: e.copy(out=e_t[:], in_=po[qs][0][:, 0:257]), R=[po[qs][1]], W=[e_r])
                        T.op("dve", lambda e: e.reciprocal(out=rc[:], in_=e_t[:, 256:257]), R=[e_r], W=[r_rc])
                        if half == 0:
                            T.op("dve", lambda e: e.tensor_scalar(out=o0[qs][0][:], in0=e_t[:, 0:256], scalar1=rc[:],
                                                                  scalar2=None, op0=ALU.mult), R=[e_r, r_rc], W=[o0[qs][1]])
                        else:
                            T.op("dve", lambda e: e.tensor_mul(out=rc[:], in0=rc[:], in1=nlam[:]), R=[r_rc, r_nlam], W=[r_rc])
                            T.op("dve", lambda e: e.scalar_tensor_tensor(out=oc[:], in0=e_t[:, 0:256], scalar=rc[:],
                                                                         in1=o0[qs][0][:], op0=ALU.mult, op1=ALU.add),
                                 R=[e_r, r_rc, o0[qs][1]], W=[r_oc])
                            T.op("dve", lambda e: e.memset(ssq[:], 0.0), W=[r_ssq])
                            T.op("act", lambda e: e.activation(out=junk[:], in_=oc[:], func=AF.Square, accum_out=ssq[:]),
                                 R=[r_oc], W=[r_junk, r_ssq])
                            T.op("act", lambda e: e.activation(out=ssq[:], in_=ssq[:], func=AF.Sqrt, bias=epsc[:],
                                                               scale=1.0 / 256), R=[r_ssq, r_epsc], W=[r_ssq])
                            T.op("dve", lambda e: e.reciprocal(out=ssq[:], in_=ssq[:]), R=[r_ssq], W=[r_ssq])
                            ob_t, ob_r = onb[n_on % 2]
                            n_on += 1
                            T.op("dve", lambda e: e.scalar_tensor_tensor(out=ob_t[:], in0=oc[:], scalar=ssq[:], in1=subw[:],
                                                                         op0=ALU.mult, op1=ALU.mult),
                                 R=[r_oc, r_ssq, r_subw], W=[ob_r])
                            t0 = (qb * 4 + qs) * 128
                            T.dma("sp", mh[t0:t0 + 128, 512 + h * 256:512 + (h + 1) * 256], ob_t[:], R=[ob_r], W=[r_mh])


def phase_gdn(P, C, l, heads=range(GH), tiles=NT):
    T = P.T
    ident_bf, r_ident_bf = C["ident_bf"]
    ident_f, r_ident_f = C["ident_f"]
    masks, r_masks = C["masks"]
    epsc, r_epsc = C["epsc"]
    mh = P.dram["mixed_half"]
    r_mh = P.dres["mixed_half"]
    with Pool_(P) as pl:
        gs = []
        for k in range(6):
            t, r = pl.sb("gs%d" % k, [128, NT * 8], F32)
            T.dma("sp", t[:], P.dram["gsc"][k], R=[P.dres["gsc"]], W=[r])
            gs.append((t, r))
        (beta, r_beta), (gc, r_gc), (egc, r_egc), (ekd, r_ekd), (glA, r_glA), (glB, r_glB) = gs
        nbeta, r_nbeta = pl.sb("nbeta", [128, NT * 8], F32)
        T.op("dve", lambda e: e.tensor_scalar(out=nbeta[:], in0=beta[:], scalar1=-1.0, scalar2=None, op0=ALU.mult),
             R=[r_beta], W=[r_nbeta])
        gnw, r_gnw = pl.sb("gnw", [128, 128], F32)
        T.dma("sp", gnw[:], P.dram["gnw%d" % l].partition_broadcast(128), R=[P.dres["gnw%d" % l]], W=[r_gnw])
        ones_f, r_ones_f = pl.sb("ones_f", [128, 128], F32)
        T.op("dve", lambda e: e.memset(ones_f[:], 1.0), W=[r_ones_f])
        qT, r_qT = pl.sb("g_qT", [128, S], BF16)
        kT, r_kT = pl.sb("g_kT", [128, S], BF16)
        ktm, r_ktm = pl.sb("g_ktm", [128, NT, 128], BF16)
        vtm, r_vtm = pl.sb("g_vtm", [128, NT, 128], BF16)
        BUF = []
        for d_ in range(2):
            B = {}
            B["obuf"] = pl.sb("g_obuf%d" % d_, [128, NT, 128], F32)
            B["Sf"] = pl.sb("g_S%d" % d_, [128, 128], F32)
            B["Sb"] = pl.sb("g_Sb%d" % d_, [128, 128], BF16)
            for nm in ("Ig", "tmp", "DT", "DTM", "kk", "qk", "X", "xtmp", "ta", "tb", "ts"):
                B[nm] = pl.sb("g_%s%d" % (nm, d_), [128, 128], F32)
            for nm in ("F0", "F1", "FT0", "FT1", "Xb", "qkm", "kg", "kd", "nwT", "vn"):
                B[nm] = pl.sb("g_%s%d" % (nm, d_), [128, 128], BF16)
            pq = []
            for k in range(4):
                t_, r_ = pl.ps("g_pq%d_%d" % (d_, k), [128, 128], F32)
                pq.append((t_[:], r_))
            B["pA"] = [pq[0], pq[1], pq[2]]
            B["pB"] = [pq[0], pq[3], pq[2]]
            B["pTr"] = pq[3]
            BUF.append(B)
        zt, r_zt = pl.sb("g_zt", [128, NT, 128], BF16)
        ssq, r_ssq = pl.sb("g_ssq", [128, 1], F32)
        junk, r_junk = pl.sb("g_junk", [128, 128], F32)
        on_, r_on = pl.sb("g_on", [128, 128], F32)
        onb = [pl.sb("g_onb%d" % i, [128, 128], BF16) for i in range(2)]

        def cp(en, out, in_, R, W, scale=None):
            if en == "act":
                if scale is None:
                    T.op("act", lambda e: e.copy(out=out, in_=in_), R=R, W=W)
                else:
                    T.op("act", lambda e: e.activation(out=out, in_=in_, func=AF.Copy, scale=scale), R=R, W=W)
            else:
                T.op("dve", lambda e: e.tensor_copy(out=out, in_=in_), R=R, W=W)

        def tile_step(h, d, tt, B):
            ms, mi = (0, 1) if d == 0 else (2, 3)
            col = tt * 8 + d * 4 + h
            tsl = slice(tt * 128, (tt + 1) * 128)
            gcc = gc[:, col:col + 1]
            pA, pB, pTr = B["pA"], B["pB"], B["pTr"]
            Ig, tmp, DT, DTM, kk_sb, qk_sb, X, xt_ = (B[k] for k in ("Ig", "tmp", "DT", "DTM", "kk", "qk", "X", "xtmp"))
            F_bf = [B["F0"], B["F1"]]
            FT_bf = [B["FT0"], B["FT1"]]
            X_bf, qkm, kg, kd, nwT, vn, ta, tb_, ts = (B[k] for k in ("Xb", "qkm", "kg", "kd", "nwT", "vn", "ta", "tb", "ts"))
            obuf, r_obuf = B["obuf"]
            Sf, r_Sf = B["Sf"]
            Sb, r_Sb = B["Sb"]
            T.op("pe", lambda e: e.matmul(pA[0][0], lhsT=kT[:, tsl], rhs=kT[:, tsl], start=True, stop=True),
                 R=[r_kT], W=[pA[0][1]])
            T.op("pe", lambda e: e.matmul(pA[1][0], lhsT=kT[:, tsl], rhs=qT[:, tsl], start=True, stop=True),
                 R=[r_kT, r_qT], W=[pA[1][1]])
            cp("act", kk_sb[0][:], pA[0][0], [pA[0][1]], [kk_sb[1]])
            cp("act", qk_sb[0][:], pA[1][0], [pA[1][1]], [qk_sb[1]])
            T.op("dve", lambda e: e.tensor_scalar(out=Ig[0][:], in0=ident_f[:], scalar1=gcc, scalar2=None, op0=ALU.mult),
                 R=[r_ident_f, r_gc], W=[Ig[1]])
            T.op("pe", lambda e: e.matmul(pA[2][0], lhsT=ones_f[:], rhs=Ig[0][:], start=True, stop=True),
                 R=[r_ones_f, Ig[1]], W=[pA[2][1]])
            T.op("dve", lambda e: e.tensor_scalar(out=tmp[0][:], in0=pA[2][0], scalar1=gcc, scalar2=0.0,
                                                  op0=ALU.subtract, op1=ALU.min), R=[pA[2][1], r_gc], W=[tmp[1]])
            T.op("act", lambda e: e.activation(out=DT[0][:], in_=tmp[0][:], func=AF.Exp), R=[tmp[1]], W=[DT[1]])
            T.op("dve", lambda e: e.tensor_mul(out=DTM[0][:], in0=DT[0][:], in1=masks[:, ms, :]),
                 R=[DT[1], r_masks], W=[DTM[1]])
            T.op("dve", lambda e: e.scalar_tensor_tensor(out=X[0][:], in0=kk_sb[0][:], scalar=nbeta[:, col:col + 1],
                                                         in1=DTM[0][:], op0=ALU.mult, op1=ALU.mult),
                 R=[kk_sb[1], r_nbeta, DTM[1]], W=[X[1]])
            fi = 0
            cp("act", F_bf[fi][0][:], X[0][:], [X[1]], [F_bf[fi][1]])
            T.op("pe", lambda e: e.transpose(out=pTr[0], in_=X[0][:], identity=ident_f[:]),
                 R=[X[1], r_ident_f], W=[pTr[1]])
            cp("act", FT_bf[fi][0][:], pTr[0], [pTr[1]], [FT_bf[fi][1]])
            T.op("dve", lambda e: e.tensor_add(out=X[0][:], in0=X[0][:], in1=ident_f[:]), R=[X[1], r_ident_f], W=[X[1]])
            cp("act", X_bf[0][:], X[0][:], [X[1]], [X_bf[1]])
            T.op("dve", lambda e: e.tensor_mul(out=DTM[0][:], in0=DT[0][:], in1=masks[:, mi, :]),
                 R=[DT[1], r_masks], W=[DTM[1]])
            T.op("dve", lambda e: e.tensor_mul(out=qkm[0][:], in0=qk_sb[0][:], in1=DTM[0][:]),
                 R=[qk_sb[1], DTM[1]], W=[qkm[1]])
            T.op("dve", lambda e: e.tensor_scalar(out=kg[0][:], in0=ktm[:, tt, :], scalar1=egc[:, col:col + 1],
                                                  scalar2=None, op0=ALU.mult), R=[r_ktm, r_egc], W=[kg[1]])
            T.op("dve", lambda e: e.tensor_scalar(out=kd[0][:], in0=ktm[:, tt, :], scalar1=ekd[:, col:col + 1],
                                                  scalar2=None, op0=ALU.mult), R=[r_ktm, r_ekd], W=[kd[1]])
            for k in range(5):
                fo = 1 - fi
                T.op("pe", lambda e: e.matmul(pB[0][0], lhsT=FT_bf[fi][0][:], rhs=F_bf[fi][0][:], start=True, stop=True),
                     R=[FT_bf[fi][1], F_bf[fi][1]], W=[pB[0][1]])
                T.op("pe", lambda e: e.matmul(pB[1][0], lhsT=F_bf[fi][0][:], rhs=FT_bf[fi][0][:], start=True, stop=True),
                     R=[FT_bf[fi][1], F_bf[fi][1]], W=[pB[1][1]])
                cp("act", F_bf[fo][0][:], pB[0][0], [pB[0][1]], [F_bf[fo][1]])
                cp("dve", FT_bf[fo][0][:], pB[1][0], [pB[1][1]], [FT_bf[fo][1]])
                T.op("pe", lambda e: e.matmul(pB[2][0], lhsT=FT_bf[fo][0][:], rhs=X_bf[0][:], start=True, stop=True),
                     R=[FT_bf[fo][1], X_bf[1]], W=[pB[2][1]])
                cp("act", xt_[0][:], pB[2][0], [pB[2][1]], [xt_[1]])
                T.op("dve", lambda e: e.tensor_add(out=X[0][:], in0=X[0][:], in1=xt_[0][:]), R=[X[1], xt_[1]], W=[X[1]])
                cp("act", X_bf[0][:], X[0][:], [X[1]], [X_bf[1]])
                fi = fo
            T.op("pe", lambda e: e.matmul(pA[0][0], lhsT=kg[0][:], rhs=X_bf[0][:], start=True, stop=True),
                 R=[kg[1], X_bf[1]], W=[pA[0][1]])
            cp("act", nwT[0][:], pA[0][0], [pA[0][1]], [nwT[1]], scale=-1.0)
            for ch in ((0, 1) if d == 0 else (1, 0)):
                ps_ = slice(ch * 64, ch * 64 + 64)
                csl = slice(tt * 128 + ch * 64, tt * 128 + ch * 64 + 64)
                glc = (glA if ch == 0 else glB)[:, col:col + 1]
                r_gl = r_glA if ch == 0 else r_glB
                T.op("pe", lambda e: e.matmul(pA[1][0][0:64, :], lhsT=X_bf[0][ps_, ps_], rhs=vtm[ps_, tt, :],
                                              start=True, stop=False), R=[X_bf[1], r_vtm], W=[pA[1][1]])
                T.op("pe", lambda e: e.matmul(pA[1][0][0:64, :], lhsT=nwT[0][:, ps_], rhs=Sb[:],
                                              start=False, stop=True), R=[nwT[1], r_Sb], W=[pA[1][1]])
                T.op("dve", lambda e: e.tensor_scalar(out=vn[0][ps_, :], in0=pA[1][0][0:64, :],
                                                      scalar1=beta[ps_, col:col + 1], scalar2=None, op0=ALU.mult),
                     R=[pA[1][1], r_beta], W=[vn[1]])
                T.op("pe", lambda e: e.matmul(pA[2][0][0:64, :], lhsT=qT[:, csl], rhs=Sb[:], start=True, stop=True),
                     R=[r_qT, r_Sb], W=[pA[2][1]])
                T.op("pe", lambda e: e.matmul(pB[0][0][0:64, :], lhsT=qkm[0][ps_, ps_], rhs=vn[0][ps_, :],
                                              start=True, stop=True), R=[qkm[1], vn[1]], W=[pB[0][1]])
                T.op("pe", lambda e: e.matmul(pB[1][0], lhsT=kd[0][ps_, :], rhs=vn[0][ps_, :], start=True, stop=True),
                     R=[kd[1], vn[1]], W=[pB[1][1]])
                cp("act", ta[0][ps_, :], pA[2][0][0:64, :], [pA[2][1]], [ta[1]])
                cp("act", tb_[0][ps_, :], pB[0][0][0:64, :], [pB[0][1]], [tb_[1]])
                T.op("dve", lambda e: e.scalar_tensor_tensor(out=obuf[ps_, tt, :], in0=ta[0][ps_, :],
                                                             scalar=egc[ps_, col:col + 1], in1=tb_[0][ps_, :],
                                                             op0=ALU.mult, op1=ALU.add),
                     R=[ta[1], r_egc, tb_[1]], W=[r_obuf])
                cp("act", ts[0][:], pB[1][0], [pB[1][1]], [ts[1]])
                T.op("dve", lambda e: e.scalar_tensor_tensor(out=Sf[:], in0=Sf[:], scalar=glc, in1=ts[0][:],
                                                             op0=ALU.mult, op1=ALU.add),
                     R=[r_Sf, r_gl, ts[1]], W=[r_Sf])
                cp("act", Sb[:], Sf[:], [r_Sf], [r_Sb])

        for h in heads:
            T.dma("sp", qT[:], P.dram["gqT"][h], R=[P.dres["gqT"]], W=[r_qT])
            T.dma("sp", kT[:], P.dram["gkT"][h], R=[P.dres["gkT"]], W=[r_kT])
            T.dma("sp", ktm[:], P.dram["gk_tm"][h].rearrange("(t p) d -> p t d", p=128), R=[P.dres["gk_tm"]], W=[r_ktm])
            T.dma("sp", vtm[:], P.dram["gv_tm"][h].rearrange("(t p) d -> p t d", p=128), R=[P.dres["gv_tm"]], W=[r_vtm])
            T.dma("sp", zt[:], P.dram["siluz"][:, h * 128:(h + 1) * 128].rearrange("(t p) d -> p t d", p=128),
                  R=[P.dres["siluz"]], W=[r_zt])
            for d in range(2):
                T.op("dve", lambda e: e.memset(BUF[d]["Sf"][0][:], 0.0), W=[BUF[d]["Sf"][1]])
                T.op("dve", lambda e: e.memset(BUF[d]["Sb"][0][:], 0.0), W=[BUF[d]["Sb"][1]])
            for step in range(tiles):
                tile_step(h, 0, step, BUF[0])
                tile_step(h, 1, tiles - 1 - step, BUF[1])
            ob0, r_ob0 = BUF[0]["obuf"]
            ob1, r_ob1 = BUF[1]["obuf"]
            for tt in range(tiles):
                T.op("dve", lambda e: e.tensor_add(out=ob0[:, tt, :], in0=ob0[:, tt, :], in1=ob1[:, tt, :]), R=[r_ob0, r_ob1], W=[r_ob0])
                T.op("dve", lambda e: e.memset(ssq[:], 0.0), W=[r_ssq])
                T.op("act", lambda e: e.activation(out=junk[:], in_=ob0[:, tt, :], func=AF.Square, accum_out=ssq[:]),
                     R=[r_ob0], W=[r_junk, r_ssq])
                T.op("act", lambda e: e.activation(out=ssq[:], in_=ssq[:], func=AF.Sqrt, bias=epsc[:], scale=1.0 / 128),
                     R=[r_ssq, r_epsc], W=[r_ssq])
                T.op("dve", lambda e: e.reciprocal(out=ssq[:], in_=ssq[:]), R=[r_ssq], W=[r_ssq])
                T.op("dve", lambda e: e.scalar_tensor_tensor(out=on_[:], in0=ob0[:, tt, :], scalar=ssq[:], in1=gnw[:],
                                                             op0=ALU.mult, op1=ALU.mult), R=[r_ob0, r_ssq, r_gnw], W=[r_on])
                ob_t, ob_r = onb[tt % 2]
                T.op("dve", lambda e: e.tensor_mul(out=ob_t[:], in0=on_[:], in1=zt[:, tt, :]), R=[r_on, r_zt], W=[ob_r])
                T.dma("sp", mh[tt * 128:(tt + 1) * 128, h * 128:(h + 1) * 128], ob_t[:], R=[ob_r], W=[r_mh])


PAIRS = [[0, 1], [2, 3], [4, 5], [6, 7]]


def pair_allgather(P, src, dst, nrows, rows_per_call):
    T = P.T
    sd, rs = P.dram[src], P.dres[src]
    dd, rd = P.dram[dst], P.dres[dst]
    ncall = nrows // rows_per_call
    for c in range(ncall):
        T._deps("pool", [rs], [rd], nowaw=True)
        inst = T.eng["pool"].collective_compute(
            "AllGather", ALU.bypass, replica_groups=PAIRS,
            ins=[sd[c * rows_per_call:(c + 1) * rows_per_call, :].opt()],
            outs=[dd[c].rearrange("r t w -> (r t) w").opt()])
        T._dma_done(inst, [rs], [rd], inc=1)


def phase3(P, C, l, xsrc):
    T = P.T
    ident_bf, r_ident_bf = C["ident_bf"]
    ident_f, r_ident_f = C["ident_f"]
    epsc, r_epsc = C["epsc"]
    pair_allgather(P, "mixed_half", "mg", S, 1024)
    mg, r_mg = P.dram["mg"], P.dres["mg"]
    xd, xr = P.dram[xsrc], P.dres[xsrc]
    x1d, x1r = P.dram["x1"], P.dres["x1"]
    x2d, x2r = P.dram["x2"], P.dres["x2"]
    wo = P.dram["w_out%d" % l].rearrange("(kc p) n -> p kc n", p=128)
    with Pool_(P) as pl:
        wob, r_wob = pl.sb("wob", [128, KC, D], BF16)
        for q4 in range(4):
            T.dma("pool", wob[:, q4 * 4:(q4 + 1) * 4, :], wo[:, q4 * 4:(q4 + 1) * 4, :], R=[P.dres["w_out%d" % l]], W=[r_wob])
        wr_f, r_wrf = pl.sb("wr_f", [128, KC, 16], F32)
        T.dma("sp", wr_f[:], P.dram["w_r%d" % l].rearrange("(kc p) n -> p kc n", p=128), R=[P.dres["w_r%d" % l]], W=[r_wrf])
        n2b, r_n2b = pl.sb("n2b", [128, D], F32)
        T.dma("sp", n2b[:], P.dram["n2w%d" % l].partition_broadcast(128), R=[P.dres["n2w%d" % l]], W=[r_n2b])
        aff, r_aff = pl.sb("aff", [128, NT, 16], F32)
        mt = [pl.sb("mt%d" % i, [128, D], BF16) for i in range(2)]
        mT = [pl.sb("mT%d" % i, [128, KC, 128], BF16) for i in range(2)]
        xt = [pl.sb("p3xt%d" % i, [128, D], F32) for i in range(2)]
        x1t = [pl.sb("x1t%d" % i, [128, D], F32) for i in range(2)]
        x2f, r_x2f = pl.sb("x2f", [128, D], F32)
        x2b = [pl.sb("x2b%d" % i, [128, D], BF16) for i in range(2)]
        x2T, r_x2T = pl.sb("x2T", [128, KC, 128], F32)
        junk, r_junk = pl.sb("p3junk", [128, D], BF16)
        ss, r_ss = pl.sb("p3ss", [128, 1], F32)
        mx, r_mx = pl.sb("p3mx", [128, 1], F32)
        lg, r_lg = pl.sb("p3lg", [128, 16], F32)
        ptr = [pl.ps("p3ptr%d" % i, [128, 8, 128], BF16) for i in range(2)]
        pacc = [pl.ps("p3acc%d" % i, [128, 512], F32) for i in range(2)]
        ptf = [pl.ps("p3ptf%d" % i, [128, 4, 128], F32) for i in range(2)]
        plg = pl.ps("p3lg", [128, 16], F32)
        for tt in range(NT):
            i = tt % 2
            m_t, m_r = mt[i]
            cidx, r0 = (tt * 128) // 1024, (tt * 128) % 1024
            for rk in range(2):
                T.dma("sp", m_t[:, rk * 1024:(rk + 1) * 1024], mg[cidx, rk, r0:r0 + 128, :], R=[r_mg], W=[m_r])
            x_t, x_r = xt[i]
            T.dma("sp", x_t[:], xd[tt * 128:(tt + 1) * 128, :], R=[xr], W=[x_r])
            mT_t, mT_r = mT[i]
            for hh in range(2):
                p_t, p_r = ptr[hh]
                for k in range(8):
                    kc = hh * 8 + k
                    T.op("pe", lambda e: e.transpose(out=p_t[:, k, :], in_=m_t[:, kc * 128:(kc + 1) * 128], identity=ident_bf[:]),
                         R=[m_r, r_ident_bf], W=[p_r])
                T.op("act", lambda e: e.copy(out=mT_t[:, hh * 8:(hh + 1) * 8, :], in_=p_t[:]), R=[p_r], W=[mT_r])
            x1_t, x1_r = x1t[i]
            for cb in range(4):
                pa_t, pa_r = pacc[cb % 2]
                for kc in range(KC):
                    T.op("pe", lambda e: e.matmul(pa_t[:], lhsT=mT_t[:, kc, :], rhs=wob[:, kc, cb * 512:(cb + 1) * 512],
                                                  start=(kc == 0), stop=(kc == KC - 1)), R=[mT_r, r_wob], W=[pa_r])
                T.op("act", lambda e: e.copy(out=x1_t[:, cb * 512:(cb + 1) * 512], in_=pa_t[:]), R=[pa_r], W=[x1_r])
            T.op("dve", lambda e: e.tensor_add(out=x1_t[:], in0=x1_t[:], in1=x_t[:]), R=[x1_r, x_r], W=[x1_r])
            T.dma("sp", x1d[tt * 128:(tt + 1) * 128, :], x1_t[:], R=[x1_r], W=[x1r])
            T.op("dve", lambda e: e.memset(ss[:], 0.0), W=[r_ss])
            T.op("act", lambda e: e.activation(out=junk[:], in_=x1_t[:], func=AF.Square, accum_out=ss[:]), R=[x1_r], W=[r_junk, r_ss])
            T.op("act", lambda e: e.activation(out=ss[:], in_=ss[:], func=AF.Sqrt, bias=epsc[:], scale=1.0 / D), R=[r_ss, r_epsc], W=[r_ss])
            T.op("dve", lambda e: e.reciprocal(out=ss[:], in_=ss[:]), R=[r_ss], W=[r_ss])
            T.op("dve", lambda e: e.scalar_tensor_tensor(out=x2f[:], in0=x1_t[:], scalar=ss[:], in1=n2b[:], op0=ALU.mult, op1=ALU.mult),
                 R=[x1_r, r_ss, r_n2b], W=[r_x2f])
            xb_t, xb_r = x2b[i]
            T.op("act", lambda e: e.copy(out=xb_t[:], in_=x2f[:]), R=[r_x2f], W=[xb_r])
            T.dma("sp", x2d[tt * 128:(tt + 1) * 128, :], xb_t[:], R=[xb_r], W=[x2r])
            for g4 in range(4):
                pf_t, pf_r = ptf[g4 % 2]
                for k in range(4):
                    kc = g4 * 4 + k
                    T.op("pe", lambda e: e.transpose(out=pf_t[:, k, :], in_=x2f[:, kc * 128:(kc + 1) * 128], identity=ident_f[:]),
                         R=[r_x2f, r_ident_f], W=[pf_r])
                T.op("act", lambda e: e.copy(out=x2T[:, g4 * 4:(g4 + 1) * 4, :], in_=pf_t[:]), R=[pf_r], W=[r_x2T])
            for kc in range(KC):
                T.op("pe", lambda e: e.matmul(plg[0][:], lhsT=x2T[:, kc, :], rhs=wr_f[:, kc, :], start=(kc == 0), stop=(kc == KC - 1)),
                     R=[r_x2T, r_wrf], W=[plg[1]])
            T.op("act", lambda e: e.copy(out=lg[:], in_=plg[0][:]), R=[plg[1]], W=[r_lg])
            T.op("dve", lambda e: e.reduce_max(out=mx[:], in_=lg[:], axis=AX.X), R=[r_lg], W=[r_mx])
            T.op("dve", lambda e: e.tensor_scalar(out=mx[:], in0=mx[:], scalar1=-1.0, scalar2=None, op0=ALU.mult), R=[r_mx], W=[r_mx])
            T.op("dve", lambda e: e.memset(ss[:], 0.0), W=[r_ss])
            T.op("act", lambda e: e.activation(out=lg[:], in_=lg[:], func=AF.Exp, bias=mx[:], accum_out=ss[:]), R=[r_lg, r_mx], W=[r_lg, r_ss])
            T.op("dve", lambda e: e.reciprocal(out=ss[:], in_=ss[:]), R=[r_ss], W=[r_ss])
            T.op("dve", lambda e: e.tensor_scalar(out=aff[:, tt, :], in0=lg[:], scalar1=ss[:], scalar2=None, op0=ALU.mult),
                 R=[r_lg, r_ss], W=[r_aff])
        T.dma("sp", P.dram["affd"], aff[:].rearrange("p t e -> p (t e)"), R=[r_aff], W=[P.dres["affd"]])


def phase4(P, C, l, xdst):
    T = P.T
    ident_bf, r_ident_bf = C["ident_bf"]
    masks, r_masks = C["masks"]
    x2d, x2r = P.dram["x2"], P.dres["x2"]
    wg = P.dram["w_g%d" % l]
    wu = P.dram["w_u%d" % l]
    wdn = P.dram["w_d%d" % l]
    with Pool_(P) as pl:
        aff, r_aff = pl.sb("aff4", [128, NT, 16], F32)
        T.dma("sp", aff[:].rearrange("p t e -> p (t e)"), P.dram["affd"], R=[P.dres["affd"]], W=[r_aff])
        ones_f, r_ones_f = pl.sb("ones4", [128, 128], F32)
        T.op("dve", lambda e: e.memset(ones_f[:], 1.0), W=[r_ones_f])
        iota, r_iota = pl.sb("iota", [128, 512], F32)
        T.dma("sp", iota[:], P.dram["iota"], R=[P.dres["iota"]], W=[r_iota])
        msk, r_msk = pl.sb("msk", [128, NT, NE], F32)
        gat, r_gat = pl.sb("gat", [128, NT, NE], F32)
        pos, r_pos = pl.sb("pos", [128, NT, NE], F32)
        prs = Pool_(P)
        prs.__enter__()
        lo, r_lo = prs.sb("lo", [128, NE], F32)
        hi, r_hi = prs.sb("hi", [128, NE], F32)
        mid, r_mid = prs.sb("mid", [128, NE], F32)
        cnt, r_cnt = prs.sb("cnt", [128, NE], F32)
        ge, r_ge = prs.sb("ge", [128, NE], F32)
        dl_, r_dl = prs.sb("dl_", [128, NE], F32)
        cmp, r_cmp = prs.sb("cmp", [128, NE, NT], F32)
        tot, r_tot = prs.sb("tot", [128, NT, NE], F32)
        offs, r_offs = prs.sb("offs", [128, NT, NE], F32)
        pcn = prs.ps("pcn", [128, NE], F32)
        ppos = prs.ps("ppos", [128, NT * NE], F32)
        T.op("dve", lambda e: e.memset(lo[:], 0.0), W=[r_lo])
        T.op("dve", lambda e: e.memset(hi[:], 1.0), W=[r_hi])
        for it in range(26):
            T.op("dve", lambda e: e.tensor_add(out=mid[:], in0=lo[:], in1=hi[:]), R=[r_lo, r_hi], W=[r_mid])
            T.op("dve", lambda e: e.tensor_scalar(out=mid[:], in0=mid[:], scalar1=0.5, scalar2=None, op0=ALU.mult), R=[r_mid], W=[r_mid])
            for ex in range(NE):
                T.op("dve", lambda e: e.tensor_scalar(out=cmp[:, ex, :], in0=aff[:, :, ex], scalar1=mid[:, ex:ex + 1], scalar2=None,
                                                      op0=ALU.is_ge), R=[r_aff, r_mid], W=[r_cmp])
            T.op("dve", lambda e: e.reduce_sum(out=cnt[:], in_=cmp[:], axis=AX.X), R=[r_cmp], W=[r_cnt])
            T.op("pe", lambda e: e.matmul(pcn[0][:], lhsT=ones_f[:], rhs=cnt[:], start=True, stop=True), R=[r_ones_f, r_cnt], W=[pcn[1]])
            T.op("dve", lambda e: e.tensor_scalar(out=ge[:], in0=pcn[0][:], scalar1=float(CAP), scalar2=None, op0=ALU.is_ge),
                 R=[pcn[1]], W=[r_ge])
            T.op("dve", lambda e: e.tensor_sub(out=dl_[:], in0=mid[:], in1=lo[:]), R=[r_mid, r_lo], W=[r_dl])
            T.op("dve", lambda e: e.tensor_mul(out=dl_[:], in0=dl_[:], in1=ge[:]), R=[r_dl, r_ge], W=[r_dl])
            T.op("dve", lambda e: e.tensor_add(out=lo[:], in0=lo[:], in1=dl_[:]), R=[r_lo, r_dl], W=[r_lo])
            T.op("dve", lambda e: e.tensor_sub(out=dl_[:], in0=hi[:], in1=mid[:]), R=[r_mid, r_hi], W=[r_dl])
            T.op("dve", lambda e: e.tensor_mul(out=dl_[:], in0=dl_[:], in1=ge[:]), R=[r_dl, r_ge], W=[r_dl])
            T.op("dve", lambda e: e.tensor_add(out=hi[:], in0=mid[:], in1=dl_[:]), R=[r_mid, r_dl], W=[r_hi])
        for ex in range(NE):
            T.op("dve", lambda e: e.tensor_scalar(out=msk[:, :, ex], in0=aff[:, :, ex], scalar1=lo[:, ex:ex + 1], scalar2=None,
                                                  op0=ALU.is_ge), R=[r_aff, r_lo], W=[r_msk])
        T.op("dve", lambda e: e.tensor_mul(out=gat[:], in0=msk[:], in1=aff[:, :, 0:NE]), R=[r_msk, r_aff], W=[r_gat])
        mflat = msk[:].rearrange("p t e -> p (t e)")
        T.op("pe", lambda e: e.matmul(ppos[0][:], lhsT=masks[:, 8, :], rhs=mflat, start=True, stop=True), R=[r_masks, r_msk], W=[ppos[1]])
        T.op("act", lambda e: e.copy(out=pos[:].rearrange("p t e -> p (t e)"), in_=ppos[0][:]), R=[ppos[1]], W=[r_pos])
        T.op("pe", lambda e: e.matmul(ppos[0][:], lhsT=ones_f[:], rhs=mflat, start=True, stop=True), R=[r_ones_f, r_msk], W=[ppos[1]])
        T.op("act", lambda e: e.copy(out=tot[:].rearrange("p t e -> p (t e)"), in_=ppos[0][:]), R=[ppos[1]], W=[r_tot])
        T.op("dve", lambda e: e.memset(offs[:, 0, :], 0.0), W=[r_offs])
        for tt in range(1, NT):
            T.op("dve", lambda e: e.tensor_add(out=offs[:, tt, :], in0=offs[:, tt - 1, :], in1=tot[:, tt - 1, :]), R=[r_offs, r_tot], W=[r_offs])
        T.op("dve", lambda e: e.tensor_add(out=pos[:], in0=pos[:], in1=offs[:]), R=[r_pos, r_offs], W=[r_pos])
        prs.__exit__(None, None, None)
        ye = [pl.sb("ye%d" % ex, [128, 4, D], BF16) for ex in range(NE)]
        with Pool_(P) as pa:
            x2t = [pa.sb("x2t%d" % i, [128, D], BF16) for i in range(1)] * 2
            sel = [pa.sb("sel%d" % i, [128, 512], BF16) for i in range(2)]
            xsT, r_xsT = pa.sb("xsT", [128, KC, 512], BF16)
            hT, r_hT = pa.sb("hT", [128, KC, 512], BF16)
            wgb = [pa.sb("wgb%d" % i, [128, KC, 128], BF16) for i in range(2)]
            wub = [pa.sb("wub%d" % i, [128, KC, 128], BF16) for i in range(2)]
            wdb = [pa.sb("wdb%d" % i, [128, KC, 128], BF16) for i in range(2)]
            sg, r_sg = pa.sb("sg", [128, 512], F32)
            su, r_su = pa.sb("su", [128, 512], F32)
            psel = [pa.ps("psel%d" % i, [128, 512], F32) for i in range(8)]
            for ex in range(NE):
                wge = wg[ex].rearrange("(kc p) n -> p kc n", p=128)
                wue = wu[ex].rearrange("(kc p) n -> p kc n", p=128)
                wde = wdn[ex].rearrange("(kc p) n -> p kc n", p=128)
                for half in range(2):
                    for tt in range(NT):
                        i = tt % 2
                        xt_t, xt_r = x2t[i]
                        s_t, s_r = sel[i]
                        T.dma("sp", xt_t[:], x2d[tt * 128:(tt + 1) * 128, :], R=[x2r], W=[xt_r])
                        T.op("dve", lambda e: e.tensor_scalar(out=s_t[:], in0=iota[:], scalar1=pos[:, tt, ex:ex + 1],
                                                              scalar2=msk[:, tt, ex:ex + 1], op0=ALU.is_equal, op1=ALU.mult),
                             R=[r_iota, r_pos, r_msk], W=[s_r])
                        for k in range(8):
                            kc = half * 8 + k
                            T.op("pe", lambda e: e.matmul(psel[k][0][:], lhsT=xt_t[:, kc * 128:(kc + 1) * 128], rhs=s_t[:],
                                                          start=(tt == 0), stop=(tt == NT - 1)), R=[xt_r, s_r], W=[psel[k][1]])
                    for k in range(8):
                        kc = half * 8 + k
                        T.op("act", lambda e: e.copy(out=xsT[:, kc, :], in_=psel[k][0][:]), R=[psel[k][1]], W=[r_xsT])
                for f in range(KC):
                    i = f % 2
                    g_t, g_r = wgb[i]
                    u_t, u_r = wub[i]
                    T.dma("pool", g_t[:], wge[:, :, f * 128:(f + 1) * 128], R=[P.dres["w_g%d" % l]], W=[g_r])
                    T.dma("pool", u_t[:], wue[:, :, f * 128:(f + 1) * 128], R=[P.dres["w_u%d" % l]], W=[u_r])
                    pg_t, pg_r = psel[(2 * f) % 8]
                    pu_t, pu_r = psel[(2 * f + 1) % 8]
                    for kc in range(KC):
                        T.op("pe", lambda e: e.matmul(pg_t[:], lhsT=g_t[:, kc, :], rhs=xsT[:, kc, :], start=(kc == 0), stop=(kc == KC - 1)),
                             R=[g_r, r_xsT], W=[pg_r])
                    for kc in range(KC):
                        T.op("pe", lambda e: e.matmul(pu_t[:], lhsT=u_t[:, kc, :], rhs=xsT[:, kc, :], start=(kc == 0), stop=(kc == KC - 1)),
                             R=[u_r, r_xsT], W=[pu_r])
                    T.op("act", lambda e: e.activation(out=sg[:], in_=pg_t[:], func=AF.Silu), R=[pg_r], W=[r_sg])
                    T.op("act", lambda e: e.copy(out=su[:], in_=pu_t[:]), R=[pu_r], W=[r_su])
                    T.op("dve", lambda e: e.tensor_mul(out=hT[:, f, :], in0=sg[:], in1=su[:]), R=[r_sg, r_su], W=[r_hT])
                for cb in range(16):
                    d_t, d_r = wdb[cb % 2]
                    T.dma("pool", d_t[:], wde[:, :, cb * 128:(cb + 1) * 128], R=[P.dres["w_d%d" % l]], W=[d_r])
                    for cs in range(4):
                        py_t, py_r = psel[(cb * 4 + cs) % 8]
                        for f in range(KC):
                            T.op("pe", lambda e: e.matmul(py_t[:, 0:128], lhsT=hT[:, f, cs * 128:(cs + 1) * 128], rhs=d_t[:, f, :],
                                                          start=(f == 0), stop=(f == KC - 1)), R=[r_hT, d_r], W=[py_r])
                        T.op("act", lambda e: e.copy(out=ye[ex][0][:, cs, cb * 128:(cb + 1) * 128], in_=py_t[:, 0:128]), R=[py_r], W=[ye[ex][1]])
        with Pool_(P) as pb:
            selg = [pb.sb("selg%d" % i, [128, 512], BF16) for i in range(2)]
            selT = [pb.sb("selT%d" % i, [128, 4, 128], BF16) for i in range(2)]
            yt = [pb.sb("yt%d" % i, [128, D], BF16) for i in range(2)]
            pT4 = [pb.ps("pT4_%d" % i, [128, 4, 128], BF16) for i in range(2)]
            py = [pb.ps("py%d" % i, [128, 512], F32) for i in range(4)]
            n = 0
            for tt in range(NT):
                for ex in range(NE):
                    i = n % 2
                    n += 1
                    s_t, s_r = selg[i]
                    T.op("dve", lambda e: e.tensor_scalar(out=s_t[:], in0=iota[:], scalar1=pos[:, tt, ex:ex + 1],
                                                          scalar2=gat[:, tt, ex:ex + 1], op0=ALU.is_equal, op1=ALU.mult),
                         R=[r_iota, r_pos, r_gat], W=[s_r])
                    p_t, p_r = pT4[i]
                    for cs in range(4):
                        T.op("pe", lambda e: e.transpose(out=p_t[:, cs, :], in_=s_t[:, cs * 128:(cs + 1) * 128], identity=ident_bf[:]),
                             R=[s_r, r_ident_bf], W=[p_r])
                    st_t, st_r = selT[i]
                    T.op("act", lambda e: e.copy(out=st_t[:], in_=p_t[:]), R=[p_r], W=[st_r])
                    for cb in range(4):
                        for cs in range(4):
                            T.op("pe", lambda e: e.matmul(py[cb][0][:], lhsT=st_t[:, cs, :], rhs=ye[ex][0][:, cs, cb * 512:(cb + 1) * 512],
                                                          start=(ex == 0 and cs == 0), stop=(ex == NE - 1 and cs == 3)),
                                 R=[st_r, ye[ex][1]], W=[py[cb][1]])
                y_t, y_r = yt[tt % 2]
                for cb in range(4):
                    T.op("act", lambda e: e.copy(out=y_t[:, cb * 512:(cb + 1) * 512], in_=py[cb][0][:]), R=[py[cb][1]], W=[y_r])
                T.dma("sp", P.dram["ypart"][tt * 128:(tt + 1) * 128, :], y_t[:], R=[y_r], W=[P.dres["ypart"]])
    pair_allgather(P, "ypart", "yg", S, 512)
    with Pool_(P) as pl:
        xt = [pl.sb("p5x%d" % i, [128, D], F32) for i in range(2)]
        ya = [pl.sb("p5a%d" % i, [128, 2, D], BF16) for i in range(2)]
        for tt in range(NT):
            i = tt % 2
            x_t, x_r = xt[i]
            a_t, a_r = ya[i]
            cidx, r0 = (tt * 128) // 512, (tt * 128) % 512
            T.dma("sp", x_t[:], P.dram["x1"][tt * 128:(tt + 1) * 128, :], R=[P.dres["x1"]], W=[x_r])
            for rk in range(2):
                T.dma("sp", a_t[:, rk, :], P.dram["yg"][cidx, rk, r0:r0 + 128, :], R=[P.dres["yg"]], W=[a_r])
            T.op("dve", lambda e: e.tensor_add(out=x_t[:], in0=x_t[:], in1=a_t[:, 0, :]), R=[x_r, a_r], W=[x_r])
            T.op("dve", lambda e: e.tensor_add(out=x_t[:], in0=x_t[:], in1=a_t[:, 1, :]), R=[x_r, a_r], W=[x_r])
            T.dma("sp", P.dram[xdst][tt * 128:(tt + 1) * 128, :], x_t[:], R=[x_r], W=[P.dres[xdst]])


def phase_final(P, C, xsrc):
    T = P.T
    epsc, r_epsc = C["epsc"]
    with Pool_(P) as pl:
        fw, r_fw = pl.sb("fw", [128, D], F32)
        T.dma("sp", fw[:], P.dram["fnw"].partition_broadcast(128), R=[P.dres["fnw"]], W=[r_fw])
        xt = [pl.sb("p6x%d" % i, [128, D], F32) for i in range(2)]
        junk, r_junk = pl.sb("p6j", [128, D], BF16)
        ss, r_ss = pl.sb("p6s", [128, 1], F32)
        for tt in range(NT):
            x_t, x_r = xt[tt % 2]
            T.dma("sp", x_t[:], P.dram[xsrc][tt * 128:(tt + 1) * 128, :], R=[P.dres[xsrc]], W=[x_r])
            T.op("dve", lambda e: e.memset(ss[:], 0.0), W=[r_ss])
            T.op("act", lambda e: e.activation(out=junk[:], in_=x_t[:], func=AF.Square, accum_out=ss[:]), R=[x_r], W=[r_junk, r_ss])
            T.op("act", lambda e: e.activation(out=ss[:], in_=ss[:], func=AF.Sqrt, bias=epsc[:], scale=1.0 / D), R=[r_ss, r_epsc], W=[r_ss])
            T.op("dve", lambda e: e.reciprocal(out=ss[:], in_=ss[:]), R=[r_ss], W=[r_ss])
            T.op("dve", lambda e: e.scalar_tensor_tensor(out=x_t[:], in0=x_t[:], scalar=ss[:], in1=fw[:], op0=ALU.mult, op1=ALU.mult),
                 R=[x_r, r_ss, r_fw], W=[x_r])
            T.dma("sp", P.dram["out"][tt * 128:(tt + 1) * 128, :], x_t[:], R=[x_r], W=[P.dres["out"]])


def build_program(layers=range(DEPTH), final=True, test_outputs=()):
    P = Prog(test_outputs=test_outputs)
    with P.es:
        declare_io(P, layers=layers)
        declare_scratch(P)
        with Pool_(P) as pc:
            C = load_consts(P, pc)
            src = "x"
            for l in layers:
                dst = "xa" if src != "xa" else "xb"
                P.marks = getattr(P, "marks", [])
                phase1(P, C, l, src)
                P.marks.append(("p1_%d" % l, dict(P.T.cnt)))
                phase_attn(P, C, l)
                P.marks.append(("attn_%d" % l, dict(P.T.cnt)))
                phase_gdn(P, C, l)
                P.marks.append(("gdn_%d" % l, dict(P.T.cnt)))
                phase3(P, C, l, src)
                P.marks.append(("p3_%d" % l, dict(P.T.cnt)))
                phase4(P, C, l, dst)
                P.marks.append(("p4_%d" % l, dict(P.T.cnt)))
                src = dst
            if final:
                phase_final(P, C, src)
        P.T.barrier()
    return P


_CACHE = {}


def kernel(**inputs):
    inputs = {k: np.asarray(v) for k, v in inputs.items()}
    if "P" not in _CACHE:
        _CACHE["P"] = build_program()
    P = _CACHE["P"]
    consts = host_consts()
    need = set(k for k in P.dram.keys())
    in_maps = []
    for c in range(8):
        m = core_inputs(inputs, c // 2, c % 2, consts)
        in_maps.append({k: v for k, v in m.items() if k in need})
    res = run_bass_kernel_spmd(P.nc, in_maps, core_ids=list(range(8)))
    out = np.stack([np.asarray(res.results[2 * b]["out"]) for b in range(4)], 0)
    return out.astype(np.float32)
```

```python
import contextlib
import math
import numpy as np
import ml_dtypes
import concourse.bass as bass
import concourse.mybir as mybir
from concourse.bass_utils import run_bass_kernel_spmd

F32 = mybir.dt.float32
BF16 = mybir.dt.bfloat16
I32 = mybir.dt.int32
AF = mybir.ActivationFunctionType
ALU = mybir.AluOpType
AX = mybir.AxisListType

S = 4096
D = 2048
KC = 16
NT = 32
DEPTH = 2
EPS = 1e-6
NCOL = 3600
GH = 4
DH = 2
NE = 8
CAP = 512


class Res:
    __slots__ = ("name", "w", "r", "dsem", "dname", "dcnt")

    def __init__(self, name):
        self.name = name
        self.w = None
        self.r = {}
        self.dsem = None
        self.dname = None
        self.dcnt = 0


class Tracker:
    def __init__(self, nc, es):
        self.nc = nc
        self.es = es
        self.eng = {"pe": nc.tensor, "dve": nc.vector, "act": nc.scalar, "pool": nc.gpsimd, "sp": nc.sync}
        self.sems = {}
        self.cnt = {}
        for k in ("pe", "dve", "act", "pool"):
            self.sems[k] = es.enter_context(nc.semaphore("e_" + k))
            self.cnt[k] = 0
        self.waited = {k: {} for k in self.eng}
        self.dcount = {}
        self.dfree = []
        self.ninst = 0

    def res(self, name):
        return Res(name)

    def _need(self, need, en, tk, raw):
        sname, val, owner = tk
        if owner == en and (en == "pe" or not raw):
            return
        if self.waited[en].get(sname, 0) >= val:
            return
        if need.get(sname, 0) < val:
            need[sname] = val

    def _emit(self, en, need):
        for sname, val in need.items():
            self.waited[en][sname] = val
            self.eng[en].wait_ge(self.sems[sname], val)
            self.ninst += 1

    def _wait(self, en, tk, raw):
        need = {}
        self._need(need, en, tk, raw)
        self._emit(en, need)

    def _deps(self, en, R, W, nowaw=False):
        need = {}
        for r in R:
            if r.w is not None:
                self._need(need, en, r.w, True)
        for w in W:
            if w.w is not None and not (nowaw and w.w[2] is None):
                self._need(need, en, w.w, False)
            for tk in w.r.values():
                self._need(need, en, tk, False)
        self._emit(en, need)

    def op(self, en, fn, R=(), W=()):
        self._deps(en, R, W)
        inst = fn(self.eng[en])
        self.cnt[en] += 1
        inst.then_inc(self.sems[en], 1)
        self.ninst += 1
        tk = (en, self.cnt[en], en)
        for r in R:
            r.r[en] = tk
        for w in W:
            w.w = tk
            w.r = {}
        return tk

    def dma(self, q, out, in_, R=(), W=(), nowaw=True, **kw):
        self._deps(q, R, W, nowaw=nowaw)
        inst = self.eng[q].dma_start(out=out, in_=in_, **kw)
        self._dma_done(inst, R, W)

    def _dsem(self, w):
        if w.dsem is None:
            if self.dfree:
                w.dname = self.dfree.pop()
            else:
                w.dname = "d_%d" % len(self.dcount)
                self.sems[w.dname] = self.es.enter_context(self.nc.semaphore(w.dname))
                self.dcount[w.dname] = 0
            w.dsem = self.sems[w.dname]

    def release(self, w):
        if w.dsem is not None:
            self.dfree.append(w.dname)
            w.dsem = None
            w.dname = None

    def _dma_done(self, inst, R, W, inc=16):
        w = W[0]
        self._dsem(w)
        self.dcount[w.dname] += inc
        if inc == 16:
            inst.then_inc(w.dsem, 16)
        else:
            inst.then_inc(w.dsem)
        self.ninst += 1
        tk = (w.dname, self.dcount[w.dname], None)
        for r in R:
            r.r[w.dname] = tk
        for ww in W:
            ww.w = tk
            ww.r = {}

    def barrier(self):
        for en in self.eng:
            need = {}
            for k in ("pe", "dve", "act", "pool"):
                if k != en and self.cnt[k] > 0:
                    self._need(need, en, (k, self.cnt[k], k), True)
            for nm, c in self.dcount.items():
                if c > 0:
                    self._need(need, en, (nm, c, None), True)
            self._emit(en, need)


def bf(a):
    return np.ascontiguousarray(a.astype(ml_dtypes.bfloat16))


def host_consts():
    c = {}
    c["ident_bf"] = bf(np.eye(128, dtype=np.float32))
    c["ident_f"] = np.eye(128, dtype=np.float32)
    rot = np.zeros((128, 128), np.float32)
    for m in range(64):
        rot[m + 64, m] = -1.0
        rot[m, m + 64] = 1.0
    c["rotT"] = bf(rot)
    c["ones_bf"] = bf(np.ones((128, 128), np.float32))
    half = 64
    inv_freq = (np.float32(10000.0) ** (-(np.arange(half, dtype=np.float32) / np.float32(half)))).astype(np.float32)
    ang = (np.arange(S, dtype=np.float32)[None, :] * inv_freq[:, None]).astype(np.float32)
    c["cos2"] = np.ascontiguousarray(np.concatenate([np.cos(ang), np.cos(ang)], 0).astype(np.float32))
    c["sin2"] = np.ascontiguousarray(np.concatenate([np.sin(ang), np.sin(ang)], 0).astype(np.float32))
    s = np.arange(128)[:, None]
    cc = np.arange(128)[None, :]
    same = (s // 64) == (cc // 64)
    m = np.zeros((9, 128, 128), np.float32)
    m[0] = same & (s < cc)
    m[1] = same & (s <= cc)
    m[2] = same & (s > cc)
    m[3] = same & (s >= cc)
    m[4] = same
    m[5] = (s < 64) & (cc >= 0)
    m[6] = (s >= 64) & (cc >= 0)
    m[7] = 1.0
    m[8] = (s < cc)
    c["masks"] = np.ascontiguousarray(m.transpose(1, 0, 2))
    c["iota"] = np.ascontiguousarray(np.tile(np.arange(512, dtype=np.float32)[None, :], (128, 1)))
    return c


def core_inputs(inputs, b, hf, consts):
    d = dict(consts)
    d["x"] = np.ascontiguousarray(inputs["x"][b])
    g0 = 512 * hf
    for l in range(DEPTH):
        w = inputs["w_in"][l]
        base = [0, 1024, 2048, 3072]
        cols = []
        cols += list(range(0 + g0, 0 + g0 + 512))
        cols += list(range(1024 + g0, 1024 + g0 + 512))
        cols += list(range(2048 + g0, 2048 + g0 + 512))
        dq0 = 4096 + 32
        cols += list(range(dq0 + g0, dq0 + g0 + 512))
        cols += list(range(dq0 + 1024 + g0, dq0 + 1024 + g0 + 512))
        cols += list(range(3072 + g0, 3072 + g0 + 512))
        cols += list(range(dq0 + 2048 + g0, dq0 + 2048 + g0 + 512))
        hb = [4096 + dd * 8 + 4 * hf + h for dd in range(2) for h in range(4)]
        ha = [4096 + 16 + dd * 8 + 4 * hf + h for dd in range(2) for h in range(4)]
        cols += hb + ha
        d["w_in%d" % l] = np.ascontiguousarray(w[:, cols])
        cw = inputs["conv_w"][l]
        ccols = []
        for grp in range(3):
            ccols += list(range(grp * 1024 + g0, grp * 1024 + g0 + 512))
        cws = cw[:, ccols]
        d["conv%d" % l] = np.ascontiguousarray(cws.reshape(5, 12, 128).transpose(2, 1, 0))
        al = inputs["a_log"][l][:, 4 * hf:4 * hf + 4].reshape(8, 1)
        dtb = inputs["dt_bias"][l][:, 4 * hf:4 * hf + 4].reshape(8, 1)
        d["alog%d" % l] = np.ascontiguousarray(al)
        d["dtb%d" % l] = np.ascontiguousarray(dtb)
        d["n1w%d" % l] = np.ascontiguousarray(inputs["norm1_w"][l].reshape(1, D))
        d["dlam%d" % l] = np.ascontiguousarray(inputs["diff_lambda"][l].reshape(1, 512))
        d["subw%d" % l] = np.ascontiguousarray(inputs["diff_subln_w"][l].reshape(1, 256))
        d["gnw%d" % l] = np.ascontiguousarray(inputs["gdn_norm_w"][l].reshape(1, 128))
        rows = list(range(0, 512)) + list(range(1024, 1536)) + list(range(512, 1024)) + list(range(1536, 2048))
        d["w_out%d" % l] = np.ascontiguousarray(inputs["w_out"][l][rows, :])
        ecols = list(range(8 * hf, 8 * hf + 8)) + list(range(8 * (1 - hf), 8 * (1 - hf) + 8))
        d["w_r%d" % l] = np.ascontiguousarray(inputs["w_router"][l][:, ecols])
        d["n2w%d" % l] = np.ascontiguousarray(inputs["norm2_w"][l].reshape(1, D))
        d["w_g%d" % l] = np.ascontiguousarray(inputs["w_gate"][l][8 * hf:8 * hf + 8])
        d["w_u%d" % l] = np.ascontiguousarray(inputs["w_up"][l][8 * hf:8 * hf + 8])
        d["w_d%d" % l] = np.ascontiguousarray(inputs["w_down"][l][8 * hf:8 * hf + 8])
    d["fnw"] = np.ascontiguousarray(inputs["final_norm_w"].reshape(1, D))
    return d


class Prog:
    def __init__(self, test_outputs=(), test_inputs=()):
        self.nc = bass.Bass("TRN2", target_bir_lowering=False)
        self.es = contextlib.ExitStack()
        self.T = Tracker(self.nc, self.es)
        self.test_outputs = set(test_outputs)
        self.test_inputs = set(test_inputs)
        self.dram = {}
        self.dres = {}

    def din(self, name, shape, dt):
        t = self.nc.dram_tensor(name, list(shape), dt, kind="ExternalInput").ap()
        self.dram[name] = t
        self.dres[name] = Res(name)
        return t

    def dscr(self, name, shape, dt, out=False):
        kind = "ExternalOutput" if (out or name in self.test_outputs) else "Internal"
        if name in self.test_inputs:
            kind = "ExternalInput"
        t = self.nc.dram_tensor(name, list(shape), dt, kind=kind).ap()
        self.dram[name] = t
        self.dres[name] = Res(name)
        return t


class Pool_:
    def __init__(self, P):
        self.P = P
        self.es = contextlib.ExitStack()
        self.created = []

    def __enter__(self):
        self.es.__enter__()
        return self

    def __exit__(self, *a):
        if a[0] is None:
            self.P.T.barrier()
            for r in self.created:
                self.P.T.release(r)
        return self.es.__exit__(*a)

    _uid = [0]

    def sb(self, name, shape, dt):
        Pool_._uid[0] += 1
        name = "%s_u%d" % (name, Pool_._uid[0])
        t = self.es.enter_context(self.P.nc.sbuf_tensor(name, list(shape), dt))
        r = Res(name)
        self.created.append(r)
        return t, r

    def ps(self, name, shape, dt):
        Pool_._uid[0] += 1
        name = "%s_u%d" % (name, Pool_._uid[0])
        t = self.es.enter_context(self.P.nc.psum_tensor(name, list(shape), dt))
        return t, Res(name)


def declare_io(P, layers=range(DEPTH)):
    P.din("x", [S, D], F32)
    P.din("ident_bf", [128, 128], BF16)
    P.din("ident_f", [128, 128], F32)
    P.din("rotT", [128, 128], BF16)
    P.din("ones_bf", [128, 128], BF16)
    P.din("cos2", [128, S], F32)
    P.din("sin2", [128, S], F32)
    P.din("masks", [128, 9, 128], F32)
    P.din("iota", [128, 512], F32)
    P.din("fnw", [1, D], F32)
    for l in layers:
        P.din("w_in%d" % l, [D, NCOL], F32)
        P.din("conv%d" % l, [128, 12, 5], F32)
        P.din("alog%d" % l, [8, 1], F32)
        P.din("dtb%d" % l, [8, 1], F32)
        P.din("n1w%d" % l, [1, D], F32)
        P.din("dlam%d" % l, [1, 512], F32)
        P.din("subw%d" % l, [1, 256], F32)
        P.din("gnw%d" % l, [1, 128], F32)
        P.din("w_out%d" % l, [D, D], F32)
        P.din("w_r%d" % l, [D, 16], F32)
        P.din("n2w%d" % l, [1, D], F32)
        P.din("w_g%d" % l, [NE, D, D], F32)
        P.din("w_u%d" % l, [NE, D, D], F32)
        P.din("w_d%d" % l, [NE, D, D], F32)


def declare_scratch(P):
    P.dscr("gqT", [GH, 128, S], BF16)
    P.dscr("gkT", [GH, 128, S], BF16)
    P.dscr("gk_tm", [GH, S, 128], BF16)
    P.dscr("gv_tm", [GH, S, 128], BF16)
    P.dscr("dqT", [4, 128, S], BF16)
    P.dscr("dkT", [4, 128, S], BF16)
    P.dscr("siluz", [S, 512], BF16)
    P.dscr("dv", [S, 512], BF16)
    P.dscr("gsc", [6, 128, NT * 8], F32)
    P.dscr("mixed_half", [S, 1024], BF16)
    P.dscr("mg", [4, 2, 1024, 1024], BF16)
    P.dscr("x1", [S, D], F32)
    P.dscr("x2", [S, D], BF16)
    P.dscr("affd", [128, NT * 16], F32)
    P.dscr("ypart", [S, D], BF16)
    P.dscr("yg", [8, 2, 512, D], BF16)
    P.dscr("xa", [S, D], F32)
    P.dscr("xb", [S, D], F32)
    P.dscr("out", [S, D], F32, out=True)


def load_consts(P, pool):
    T = P.T
    C = {}
    for nm, shape, dt in (("ident_bf", [128, 128], BF16), ("ident_f", [128, 128], F32), ("rotT", [128, 128], BF16),
                          ("ones_bf", [128, 128], BF16), ("masks", [128, 9, 128], F32)):
        t, r = pool.sb("c_" + nm, shape, dt)
        T.dma("sp", t[:], P.dram[nm], R=[P.dres[nm]], W=[r])
        C[nm] = (t, r)
    for nm, val in (("epsc", EPS), ("onec", 1.0)):
        t, r = pool.sb("c_" + nm, [128, 1], F32)
        T.op("dve", lambda e: e.memset(t[:], val), W=[r])
        C[nm] = (t, r)
    return C


def phase1(P, C, l, xsrc, upto=None, sections=("gdn", "rope", "tm", "bd"), dbg=None):
    T = P.T
    nc = P.nc
    ident_bf, r_ident_bf = C["ident_bf"]
    ident_f, r_ident_f = C["ident_f"]
    rotT, r_rotT = C["rotT"]
    ones_bf, r_ones = C["ones_bf"]
    masks, r_masks = C["masks"]
    epsc, r_epsc = C["epsc"]
    onec, r_onec = C["onec"]
    xd = P.dram[xsrc]
    xr = P.dres[xsrc]
    wd = P.dram["w_in%d" % l].rearrange("(kc p) n -> p kc n", p=128)
    wr = P.dres["w_in%d" % l]
    with Pool_(P) as pl:
        nT, r_nT = pl.sb("nT", [128, KC, S], BF16)
        with Pool_(P) as pa:
            w1b, r_w1b = pa.sb("w1b", [128, D], F32)
            T.dma("sp", w1b[:], P.dram["n1w%d" % l].partition_broadcast(128), R=[P.dres["n1w%d" % l]], W=[r_w1b])
            xt = [pa.sb("xt%d" % i, [128, D], F32) for i in range(2)]
            nt = [pa.sb("nt%d" % i, [128, D], BF16) for i in range(2)]
            junk, r_junk = pa.sb("junk", [128, D], BF16)
            ss = [pa.sb("ss%d" % i, [128, 1], F32) for i in range(2)]
            rstd = [pa.sb("rstd%d" % i, [128, 1], F32) for i in range(2)]
            ptr = [pa.ps("ptr%d" % i, [128, 8, 128], BF16) for i in range(2)]
            for tt in range(NT):
                i = tt % 2
                x_t, x_r = xt[i]
                n_t, n_r = nt[i]
                T.dma("sp", x_t[:], xd[tt * 128:(tt + 1) * 128, :], R=[xr], W=[x_r])
                T.op("dve", lambda e: e.memset(ss[i][0][:], 0.0), W=[ss[i][1]])
                T.op("act", lambda e: e.activation(out=junk[:], in_=x_t[:], func=AF.Square, accum_out=ss[i][0][:]),
                     R=[x_r], W=[r_junk, ss[i][1]])
                T.op("act", lambda e: e.activation(out=rstd[i][0][:], in_=ss[i][0][:], func=AF.Sqrt, bias=epsc[:],
                                                   scale=1.0 / D), R=[ss[i][1], r_epsc], W=[rstd[i][1]])
                T.op("dve", lambda e: e.reciprocal(out=rstd[i][0][:], in_=rstd[i][0][:]), R=[rstd[i][1]], W=[rstd[i][1]])
                T.op("dve", lambda e: e.scalar_tensor_tensor(out=n_t[:], in0=x_t[:], scalar=rstd[i][0][:], in1=w1b[:],
                                                             op0=ALU.mult, op1=ALU.mult),
                     R=[x_r, rstd[i][1], r_w1b], W=[n_r])
                for hh in range(2):
                    p_t, p_r = ptr[hh]
                    for k in range(8):
                        kc = hh * 8 + k
                        T.op("pe", lambda e: e.transpose(out=p_t[:, k, :], in_=n_t[:, kc * 128:(kc + 1) * 128],
                                                         identity=ident_bf[:]),
                             R=[n_r, r_ident_bf], W=[p_r])
                    en = "act" if hh == 0 else "dve"
                    if en == "act":
                        T.op("act", lambda e: e.copy(out=nT[:, hh * 8:(hh + 1) * 8, tt * 128:(tt + 1) * 128], in_=p_t[:]),
                             R=[p_r], W=[r_nT])
                    else:
                        T.op("dve", lambda e: e.tensor_copy(out=nT[:, hh * 8:(hh + 1) * 8, tt * 128:(tt + 1) * 128],
                                                            in_=p_t[:]), R=[p_r], W=[r_nT])
        if upto == "1a":
            if "dbg_nT" in P.dram:
                T.dma("sp", P.dram["dbg_nT"], nT[:], R=[r_nT], W=[P.dres["dbg_nT"]])
            return
        with Pool_(P) as pb:
            wblk = [pb.sb("wblk%d" % i, [128, KC, 256], BF16) for i in range(2)]
            raw = [pb.sb("raw%d" % i, [128, S + 4], BF16) for i in range(2)]
            acc = [pb.sb("acc%d" % i, [128, 1024], F32) for i in range(2)]
            sl = [pb.sb("sl%d" % i, [128, 1024], F32) for i in range(2)]
            sq = [pb.sb("sq%d" % i, [128, 1024], BF16) for i in range(1)] * 2
            rn = [pb.sb("rn%d" % i, [128, 1024], F32) for i in range(1)] * 2
            ob = [pb.sb("ob%d" % i, [128, 1024], BF16) for i in range(2)]
            tm = [pb.sb("tm%d" % i, [128, 8, 128], BF16) for i in range(1)] * 2
            cst = [pb.sb("cst%d" % i, [128, 512], F32) for i in range(1)] * 2
            snt = [pb.sb("snt%d" % i, [128, 512], F32) for i in range(1)] * 2
            tbf = [pb.sb("tbf%d" % i, [128, 512], BF16) for i in range(1)] * 2
            ra = [pb.sb("ra%d" % i, [128, 512], F32) for i in range(1)] * 2
            rb = [pb.sb("rb%d" % i, [128, 512], F32) for i in range(1)] * 2
            convw, r_convw = pb.sb("convw", [128, 12, 5], F32)
            T.dma("sp", convw[:], P.dram["conv%d" % l], R=[P.dres["conv%d" % l]], W=[r_convw])
            pacc = [pb.ps("pacc%d" % i, [128, 512], F32) for i in range(2)]
            prot = [pb.ps("prot%d" % i, [128, 512], F32) for i in range(1)]
            pss = [pb.ps("pss%d" % i, [128, 1024], F32) for i in range(1)]
            ptm = [pb.ps("ptm%d" % i, [128, 8, 128], BF16) for i in range(2)]
            for i in range(2):
                T.op("dve", lambda e: e.memset(raw[i][0][:], 0.0), W=[raw[i][1]])
            nblk = [0]

            def load_w(c0, ncols):
                i = nblk[0] % 2
                nblk[0] += 1
                w_t, w_r = wblk[i]
                for q4 in range(4):
                    T.dma("pool", w_t[:, q4 * 4:(q4 + 1) * 4, 0:ncols], wd[:, q4 * 4:(q4 + 1) * 4, c0:c0 + ncols],
                          R=[wr], W=[w_r])
                return w_t, w_r

            cnt = {"pacc": 0, "ptm": 0, "st": 0, "rp": 0}

            def proj_fm(w_t, w_r, cw, tb, m=128):
                i = cnt["pacc"] % 2
                cnt["pacc"] += 1
                p_t, p_r = pacc[i]
                for kc in range(KC):
                    T.op("pe", lambda e: e.matmul(p_t[0:m, :], lhsT=w_t[:, kc, cw:cw + m],
                                                  rhs=nT[:, kc, tb * 512:(tb + 1) * 512],
                                                  start=(kc == 0), stop=(kc == KC - 1)),
                         R=[w_r, r_nT], W=[p_r])
                return p_t, p_r

            for j in (range(12) if "gdn" in sections else ()):
                grp, h = j // 4, j % 4
                if j % 2 == 0:
                    w_t, w_r = load_w(j * 128, 256)
                cw = (j % 2) * 128
                r_t, r_r = raw[j % 2]
                for tb in range(8):
                    p_t, p_r = proj_fm(w_t, w_r, cw, tb)
                    T.op("act", lambda e: e.copy(out=r_t[:, 2 + tb * 512:2 + (tb + 1) * 512], in_=p_t[:]),
                         R=[p_r], W=[r_r])
                if j == 0 and "dbg_raw" in P.dram:
                    T.dma("sp", P.dram["dbg_raw"], r_t[:], R=[r_r], W=[P.dres["dbg_raw"]])
                    return
                for blk in range(4):
                    si = cnt["st"] % 2
                    cnt["st"] += 1
                    a_t, a_r = acc[si]
                    s_t, s_r = sl[si]
                    o_t, o_r = ob[si]
                    t0 = blk * 1024
                    T.op("dve", lambda e: e.tensor_scalar(out=a_t[:], in0=r_t[:, t0:t0 + 1024], scalar1=convw[:, j, 0:1],
                                                          scalar2=None, op0=ALU.mult), R=[r_r, r_convw], W=[a_r])
                    for k in range(1, 5):
                        T.op("dve", lambda e: e.scalar_tensor_tensor(out=a_t[:], in0=r_t[:, t0 + k:t0 + k + 1024],
                                                                     scalar=convw[:, j, k:k + 1], in1=a_t[:],
                                                                     op0=ALU.mult, op1=ALU.add),
                             R=[r_r, r_convw, a_r], W=[a_r])
                    if grp == 2:
                        T.op("act", lambda e: e.activation(out=o_t[:], in_=a_t[:], func=AF.Silu), R=[a_r], W=[o_r])
                    else:
                        q_t, q_r = sq[si]
                        n_t, n_r = rn[si]
                        T.op("act", lambda e: e.activation(out=s_t[:], in_=a_t[:], func=AF.Silu), R=[a_r], W=[s_r])
                        T.op("act", lambda e: e.activation(out=q_t[:], in_=s_t[:], func=AF.Square), R=[s_r], W=[q_r])
                        ps_t, ps_r = pss[0]
                        for hh in range(2):
                            T.op("pe", lambda e: e.matmul(ps_t[:, hh * 512:(hh + 1) * 512], lhsT=ones_bf[:],
                                                          rhs=q_t[:, hh * 512:(hh + 1) * 512], start=True, stop=True),
                                 R=[r_ones, q_r], W=[ps_r])
                        T.op("act", lambda e: e.activation(out=n_t[:], in_=ps_t[:], func=AF.Sqrt, bias=epsc[:]),
                             R=[ps_r, r_epsc], W=[n_r])
                        T.op("dve", lambda e: e.reciprocal(out=n_t[:], in_=n_t[:]), R=[n_r], W=[n_r])
                        qs = (128.0 ** -0.5) if grp == 0 else 1.0
                        T.op("dve", lambda e: e.scalar_tensor_tensor(out=o_t[:], in0=s_t[:], scalar=qs, in1=n_t[:],
                                                                     op0=ALU.mult, op1=ALU.mult),
                             R=[s_r, n_r], W=[o_r])
                    if grp < 2:
                        dst = P.dram["gqT" if grp == 0 else "gkT"]
                        T.dma("sp", dst[h, :, t0:t0 + 1024], o_t[:], R=[o_r],
                              W=[P.dres["gqT" if grp == 0 else "gkT"]])
                    if grp >= 1:
                        pi = cnt["ptm"] % 2
                        cnt["ptm"] += 1
                        pt_t, pt_r = ptm[pi]
                        tm_t, tm_r = tm[pi]
                        for k in range(8):
                            T.op("pe", lambda e: e.transpose(out=pt_t[:, k, :], in_=o_t[:, k * 128:(k + 1) * 128],
                                                             identity=ident_bf[:]), R=[o_r, r_ident_bf], W=[pt_r])
                        T.op("act", lambda e: e.copy(out=tm_t[:], in_=pt_t[:]), R=[pt_r], W=[tm_r])
                        nm = "gk_tm" if grp == 1 else "gv_tm"
                        T.dma("sp", P.dram[nm][h, t0:t0 + 1024, :].rearrange("(t p) d -> p t d", p=128), tm_t[:],
                              R=[tm_r], W=[P.dres[nm]])
            if upto == "gdn":
                return
            for j in (dbg["rope_j"] if dbg else (range(12, 20) if "rope" in sections else ())):
                jj = j - 12
                isq, cj = (jj < 4), jj % 4
                if j % 2 == 0:
                    w_t, w_r = load_w(j * 128, 256)
                cw = (j % 2) * 128
                nm = "dqT" if isq else "dkT"
                for tb in (dbg["rope_tb"] if dbg else range(8)):
                    i = cnt["rp"] % 2
                    cnt["rp"] += 1
                    c_t, c_r = cst[i]
                    s_t, s_r = snt[i]
                    sk = dbg.get("skip", "") if dbg else ""
                    if "c" in sk:
                        T.op("dve", lambda e: e.memset(c_t[:], 1.0), W=[c_r])
                        T.op("dve", lambda e: e.memset(s_t[:], 0.0), W=[s_r])
                    else:
                        T.dma("sp", c_t[:], P.dram["cos2"][:, tb * 512:(tb + 1) * 512], R=[P.dres["cos2"]], W=[c_r])
                        T.dma("sp", s_t[:], P.dram["sin2"][:, tb * 512:(tb + 1) * 512], R=[P.dres["sin2"]], W=[s_r])
                    p_t, p_r = proj_fm(w_t, w_r, cw, tb)
                    b_t, b_r = tbf[i]
                    T.op("act", lambda e: e.copy(out=b_t[:], in_=p_t[:]), R=[p_r], W=[b_r])
                    pr_t, pr_r = prot[0]
                    if "r" in sk:
                        T.op("pe", lambda e: e.matmul(pr_t[:], lhsT=ones_bf[:], rhs=b_t[:], start=True, stop=True),
                             R=[r_ones, b_r], W=[pr_r])
                    else:
                        T.op("pe", lambda e: e.matmul(pr_t[:], lhsT=rotT[:], rhs=b_t[:], start=True, stop=True),
                             R=[r_rotT, b_r], W=[pr_r])
                    a_t, a_r = ra[i]
                    bb_t, bb_r = rb[i]
                    o_t, o_r = ob[i]
                    if "m" in sk:
                        T.op("act", lambda e: e.copy(out=o_t[:, 0:512], in_=pr_t[:]), R=[pr_r], W=[o_r])
                    elif "1" in sk:
                        T.op("dve", lambda e: e.tensor_mul(out=a_t[:], in0=p_t[:], in1=c_t[:]), R=[p_r, c_r], W=[a_r])
                        T.op("act", lambda e: e.copy(out=o_t[:, 0:512], in_=a_t[:]), R=[a_r], W=[o_r])
                    elif "3" in sk:
                        T.op("dve", lambda e: e.tensor_mul(out=a_t[:], in0=c_t[:], in1=c_t[:]), R=[c_r], W=[a_r])
                        T.op("act", lambda e: e.copy(out=o_t[:, 0:512], in_=a_t[:]), R=[a_r], W=[o_r])
                    elif "2" in sk:
                        T.op("dve", lambda e: e.tensor_mul(out=a_t[:], in0=p_t[:], in1=c_t[:]), R=[p_r, c_r], W=[a_r])
                        T.op("dve", lambda e: e.tensor_mul(out=bb_t[:], in0=pr_t[:], in1=s_t[:]), R=[pr_r, s_r], W=[bb_r])
                        T.op("act", lambda e: e.copy(out=o_t[:, 0:512], in_=bb_t[:]), R=[a_r, bb_r], W=[o_r])
                    else:
                        T.op("act", lambda e: e.copy(out=a_t[:], in_=p_t[:]), R=[p_r], W=[a_r])
                        T.op("act", lambda e: e.copy(out=bb_t[:], in_=pr_t[:]), R=[pr_r], W=[bb_r])
                        T.op("dve", lambda e: e.tensor_mul(out=a_t[:], in0=a_t[:], in1=c_t[:]),
                             R=[a_r, c_r], W=[a_r])
                        T.op("dve", lambda e: e.tensor_mul(out=bb_t[:], in0=bb_t[:], in1=s_t[:]),
                             R=[bb_r, s_r], W=[bb_r])
                        T.op("dve", lambda e: e.tensor_add(out=o_t[:, 0:512], in0=a_t[:], in1=bb_t[:]),
                             R=[a_r, bb_r], W=[o_r])
                    T.dma("sp", P.dram[nm][cj, :, tb * 512:(tb + 1) * 512], o_t[:, 0:512], R=[o_r], W=[P.dres[nm]])
            if upto == "rope":
                return
            for g in range(2):
                nm = "siluz" if g == 0 else "dv"
                for cb in range(2):
                    c0 = 2560 + g * 512 + cb * 256
                    w_t, w_r = load_w(c0, 256)
                    for tt in range(NT):
                        i = cnt["pacc"] % 2
                        cnt["pacc"] += 1
                        p_t, p_r = pacc[i]
                        for kc in range(KC):
                            T.op("pe", lambda e: e.matmul(p_t[:, 0:256], lhsT=nT[:, kc, tt * 128:(tt + 1) * 128],
                                                          rhs=w_t[:, kc, 0:256], start=(kc == 0), stop=(kc == KC - 1)),
                                 R=[w_r, r_nT], W=[p_r])
                        si = cnt["st"] % 2
                        cnt["st"] += 1
                        o_t, o_r = ob[si]
                        T.op("act", lambda e: e.activation(out=o_t[:, 0:256], in_=p_t[:, 0:256],
                                                           func=(AF.Silu if g == 0 else AF.Copy)), R=[p_r], W=[o_r])
                        T.dma("sp", P.dram[nm][tt * 128:(tt + 1) * 128, cb * 256:(cb + 1) * 256], o_t[:, 0:256],
                              R=[o_r], W=[P.dres[nm]])
        if upto == "tm":
            return
        with Pool_(P) as pg:
            w_t, w_r = pg.sb("wsm", [128, KC, 16], BF16)
            T.dma("pool", w_t[:], wd[:, :, 3584:3600], R=[wr], W=[w_r])
            pacc2 = [pg.ps("pacc2_%d" % i, [128, 512], F32) for i in range(2)]
            psm = [pg.ps("psm0", [128, 512], F32)]
            pcnt = [0]

            def proj_fm(w_t, w_r, cw, tb, m=128):
                i = pcnt[0] % 2
                pcnt[0] += 1
                p_t, p_r = pacc2[i]
                for kc in range(KC):
                    T.op("pe", lambda e: e.matmul(p_t[0:m, :], lhsT=w_t[:, kc, cw:cw + m],
                                                  rhs=nT[:, kc, tb * 512:(tb + 1) * 512],
                                                  start=(kc == 0), stop=(kc == KC - 1)),
                         R=[w_r, r_nT], W=[p_r])
                return p_t, p_r
            bfm, r_bfm = pg.sb("bfm", [8, S], F32)
            gfm, r_gfm = pg.sb("gfm", [8, S], F32)
            alog, r_alog = pg.sb("alog", [8, 1], F32)
            dtb, r_dtb = pg.sb("dtb", [8, 1], F32)
            nega, r_nega = pg.sb("nega", [8, 1], F32)
            T.dma("sp", alog[:], P.dram["alog%d" % l], R=[P.dres["alog%d" % l]], W=[r_alog])
            T.dma("sp", dtb[:], P.dram["dtb%d" % l], R=[P.dres["dtb%d" % l]], W=[r_dtb])
            T.op("act", lambda e: e.activation(out=nega[:], in_=alog[:], func=AF.Exp), R=[r_alog], W=[r_nega])
            T.op("dve", lambda e: e.tensor_scalar(out=nega[:], in0=nega[:], scalar1=-1.0, scalar2=None, op0=ALU.mult),
                 R=[r_nega], W=[r_nega])
            for tb in range(8):
                p_t, p_r = proj_fm(w_t, w_r, 0, tb, m=8)
                T.op("act", lambda e: e.activation(out=bfm[:, tb * 512:(tb + 1) * 512], in_=p_t[0:8, :],
                                                   func=AF.Sigmoid), R=[p_r], W=[r_bfm])
            for tb in range(8):
                p_t, p_r = proj_fm(w_t, w_r, 8, tb, m=8)
                T.op("act", lambda e: e.activation(out=gfm[:, tb * 512:(tb + 1) * 512], in_=p_t[0:8, :],
                                                   func=AF.Exp, bias=dtb[:]), R=[p_r, r_dtb], W=[r_gfm])
            T.op("act", lambda e: e.activation(out=gfm[:], in_=gfm[:], func=AF.Ln, bias=onec[0:8, :]), R=[r_gfm, r_onec], W=[r_gfm])
            T.op("dve", lambda e: e.tensor_scalar(out=gfm[:], in0=gfm[:], scalar1=nega[:], scalar2=None, op0=ALU.mult),
                 R=[r_gfm, r_nega], W=[r_gfm])
            btm, r_btm = pg.sb("btm", [128, NT, 8], F32)
            gtm, r_gtm = pg.sb("gtm", [128, NT, 8], F32)
            gcs, r_gcs = pg.sb("gcs", [128, NT, 8], F32)
            gto, r_gto = pg.sb("gto", [128, NT, 8], F32)
            ex1, r_ex1 = pg.sb("ex1", [128, NT, 8], F32)
            ex2, r_ex2 = pg.sb("ex2", [128, NT, 8], F32)
            ex3, r_ex3 = pg.sb("ex3", [128, 2, NT, 8], F32)
            ps_t, ps_r = psm[0]
            psv = ps_t[:, 0:256].rearrange("p (t h) -> p t h", h=8)
            for src, r_src, dst, r_dst in ((bfm, r_bfm, btm, r_btm), (gfm, r_gfm, gtm, r_gtm)):
                for tt in range(NT):
                    T.op("pe", lambda e: e.transpose(out=psv[:, tt, :], in_=src[:, tt * 128:(tt + 1) * 128],
                                                     identity=ident_f[0:8, 0:8]), R=[r_src, r_ident_f], W=[ps_r])
                T.op("dve", lambda e: e.tensor_copy(out=dst[:], in_=psv), R=[ps_r], W=[r_dst])
            T.op("pe", lambda e: e.matmul(psv[:, :, 0:4], lhsT=masks[:, 1, :], rhs=gtm[:, :, 0:4], start=True, stop=True),
                 R=[r_masks, r_gtm], W=[ps_r])
            T.op("pe", lambda e: e.matmul(psv[:, :, 4:8], lhsT=masks[:, 3, :], rhs=gtm[:, :, 4:8], start=True, stop=True),
                 R=[r_masks, r_gtm], W=[ps_r])
            T.op("dve", lambda e: e.tensor_copy(out=gcs[:], in_=psv), R=[ps_r], W=[r_gcs])
            T.op("pe", lambda e: e.matmul(ps_t[:, 0:256], lhsT=masks[:, 4, :], rhs=gtm[:].rearrange("p t h -> p (t h)"),
                                          start=True, stop=True), R=[r_masks, r_gtm], W=[ps_r])
            T.op("dve", lambda e: e.tensor_copy(out=gto[:], in_=psv), R=[ps_r], W=[r_gto])
            T.op("act", lambda e: e.activation(out=ex1[:], in_=gcs[:], func=AF.Exp), R=[r_gcs], W=[r_ex1])
            T.op("dve", lambda e: e.tensor_tensor(out=ex2[:], in0=gto[:], in1=gcs[:], op=ALU.subtract),
                 R=[r_gto, r_gcs], W=[r_ex2])
            T.op("act", lambda e: e.activation(out=ex2[:], in_=ex2[:], func=AF.Exp), R=[r_ex2], W=[r_ex2])
            for ab in range(2):
                T.op("pe", lambda e: e.matmul(ps_t[:, 0:256], lhsT=masks[:, 5 + ab, :],
                                              rhs=gtm[:].rearrange("p t h -> p (t h)"), start=True, stop=True),
                     R=[r_masks, r_gtm], W=[ps_r])
                T.op("act", lambda e: e.activation(out=ex3[:, ab], in_=psv, func=AF.Exp), R=[ps_r], W=[r_ex3])
            gsc = P.dram["gsc"]
            rg = P.dres["gsc"]
            for k, (src, r_src) in enumerate(((btm, r_btm), (gcs, r_gcs), (ex1, r_ex1), (ex2, r_ex2))):
                T.dma("sp", gsc[k], src[:].rearrange("p t h -> p (t h)"), R=[r_src], W=[rg])
            for ab in range(2):
                T.dma("sp", gsc[4 + ab], ex3[:, ab].rearrange("p t h -> p (t h)"), R=[r_ex3], W=[rg])
    T.barrier()


def phase_attn(P, C, l):
    T = P.T
    lam_init = 0.8 - 0.6 * math.exp(-0.3 * l)
    scale = 128.0 ** -0.5
    epsc, r_epsc = C["epsc"]
    mh = P.dram["mixed_half"]
    r_mh = P.dres["mixed_half"]
    with Pool_(P) as pl:
        dlb, r_dlb = pl.sb("dlb", [128, 512], F32)
        prod, r_prod = pl.sb("prod", [128, 256], F32)
        s12, r_s12 = pl.sb("s12", [128, 2], F32)
        nlam, r_nlam = pl.sb("nlam", [128, 1], F32)
        subw, r_subw = pl.sb("subw", [128, 256], F32)
        T.dma("sp", dlb[:], P.dram["dlam%d" % l].partition_broadcast(128), R=[P.dres["dlam%d" % l]], W=[r_dlb])
        T.dma("sp", subw[:], P.dram["subw%d" % l].partition_broadcast(128), R=[P.dres["subw%d" % l]], W=[r_subw])
        T.op("dve", lambda e: e.tensor_mul(out=prod[:, 0:128], in0=dlb[:, 0:128], in1=dlb[:, 128:256]), R=[r_dlb], W=[r_prod])
        T.op("dve", lambda e: e.tensor_mul(out=prod[:, 128:256], in0=dlb[:, 256:384], in1=dlb[:, 384:512]), R=[r_dlb], W=[r_prod])
        T.op("dve", lambda e: e.reduce_sum(out=s12[:, 0:1], in_=prod[:, 0:128], axis=AX.X), R=[r_prod], W=[r_s12])
        T.op("dve", lambda e: e.reduce_sum(out=s12[:, 1:2], in_=prod[:, 128:256], axis=AX.X), R=[r_prod], W=[r_s12])
        T.op("act", lambda e: e.activation(out=s12[:], in_=s12[:], func=AF.Exp), R=[r_s12], W=[r_s12])
        T.op("dve", lambda e: e.tensor_sub(out=nlam[:], in0=s12[:, 1:2], in1=s12[:, 0:1]), R=[r_s12], W=[r_nlam])
        T.op("dve", lambda e: e.tensor_scalar(out=nlam[:], in0=nlam[:], scalar1=-lam_init, scalar2=None, op0=ALU.add),
             R=[r_nlam], W=[r_nlam])
        T.op("dve", lambda e: e.tensor_scalar(out=subw[:], in0=subw[:], scalar1=1.0 - lam_init, scalar2=None, op0=ALU.mult),
             R=[r_subw], W=[r_subw])
        vp, r_vp = pl.sb("vp", [128, NT, 257], BF16)
        qT2, r_qT2 = pl.sb("qT2", [128, 2, S], BF16)
        kT2, r_kT2 = pl.sb("kT2", [128, 2, S], BF16)
        pT = [pl.sb("pT%d" % i, [128, 512], BF16) for i in range(2)]
        ev = [pl.sb("ev%d" % i, [128, 257], F32) for i in range(2)]
        o0 = [pl.sb("o0_%d" % i, [128, 256], F32) for i in range(4)]
        oc, r_oc = pl.sb("oc", [128, 256], F32)
        junk, r_junk = pl.sb("junk2", [128, 256], F32)
        rc, r_rc = pl.sb("rc", [128, 1], F32)
        ssq, r_ssq = pl.sb("ssq", [128, 1], F32)
        onb = [pl.sb("onb%d" % i, [128, 256], BF16) for i in range(2)]
        pss = [pl.ps("pss%d" % i, [128, 512], F32) for i in range(2)]
        po = [pl.ps("po%d" % i, [128, 512], F32) for i in range(4)]
        n_on = 0
        for h in range(2):
            T.op("dve", lambda e: e.memset(vp[:, :, 256:257], 1.0), W=[r_vp])
            T.dma("sp", vp[:, :, 0:256], P.dram["dv"][:, h * 256:(h + 1) * 256].rearrange("(t p) d -> p t d", p=128),
                  R=[P.dres["dv"]], W=[r_vp], nowaw=False)
            for half in range(2):
                T.dma("sp", qT2[:, half, :], P.dram["dqT"][2 * h + half], R=[P.dres["dqT"]], W=[r_qT2])
                T.dma("sp", kT2[:, half, :], P.dram["dkT"][2 * h + half], R=[P.dres["dkT"]], W=[r_kT2])
            for qb in range(8):
                for half in range(2):
                    def s_mm(kt_):
                        ps_t_, ps_r_ = pss[kt_ % 2]
                        T.op("pe", lambda e: e.matmul(ps_t_[:], lhsT=kT2[:, half, kt_ * 128:(kt_ + 1) * 128],
                                                      rhs=qT2[:, half, qb * 512:(qb + 1) * 512], start=True, stop=True),
                             R=[r_kT2, r_qT2], W=[ps_r_])
                    s_mm(0)
                    for kt in range(NT):
                        ps_t, ps_r = pss[kt % 2]
                        p_t, p_r = pT[kt % 2]
                        if kt + 1 < NT:
                            s_mm(kt + 1)
                        T.op("act", lambda e: e.activation(out=p_t[:], in_=ps_t[:], func=AF.Exp, scale=scale),
                             R=[ps_r], W=[p_r])
                        for qs in range(4):
                            T.op("pe", lambda e: e.matmul(po[qs][0][:, 0:257], lhsT=p_t[:, qs * 128:(qs + 1) * 128],
                                                          rhs=vp[:, kt, :], start=(kt == 0), stop=(kt == NT - 1)),
                                 R=[p_r, r_vp], W=[po[qs][1]])
                    for qs in range(4):
                        e_t, e_r = ev[qs % 2]
                        T.op("act", lambda e: e.copy(out=e_t[:], in_=po[qs][0][:, 0:257]), R=[po[qs][1]], W=[e_r])
                        T.op("dve", lambda e: e.reciprocal(out=rc[:], in_=e_t[:, 256:257]), R=[e_r], W=[r_rc])
                        if half == 0:
                            T.op("dve", lambda e: e.tensor_scalar(out=o0[qs][0][:], in0=e_t[:, 0:256], scalar1=rc[:],
                                                                  scalar2=None, op0=ALU.mult), R=[e_r, r_rc], W=[o0[qs][1]])
                        else:
                            T.op("dve", lambda e: e.tensor_mul(out=rc[:], in0=rc[:], in1=nlam[:]), R=[r_rc, r_nlam], W=[r_rc])
                            T.op("dve", lambda e: e.scalar_tensor_tensor(out=oc[:], in0=e_t[:, 0:256], scalar=rc[:],
                                                                         in1=o0[qs][0][:], op0=ALU.mult, op1=ALU.add),
                                 R=[e_r, r_rc, o0[qs][1]], W=[r_oc])
                            T.op("dve", lambda e: e.memset(ssq[:], 0.0), W=[r_ssq])
                            T.op("act", lambda e: e.activation(out=junk[:], in_=oc[:], func=AF.Square, accum_out=ssq[:]),
                                 R=[r_oc], W=[r_junk, r_ssq])
                            T.op("act", lambda e: e.activation(out=ssq[:], in_=ssq[:], func=AF.Sqrt, bias=epsc[:],
                                                               scale=1.0 / 256), R=[r_ssq, r_epsc], W=[r_ssq])
                            T.op("dve", lambda e: e.reciprocal(out=ssq[:], in_=ssq[:]), R=[r_ssq], W=[r_ssq])
                            ob_t, ob_r = onb[n_on % 2]
                            n_on += 1
                            T.op("dve", lambda e: e.scalar_tensor_tensor(out=ob_t[:], in0=oc[:], scalar=ssq[:], in1=subw[:],
                                                                         op0=ALU.mult, op1=ALU.mult),
                                 R=[r_oc, r_ssq, r_subw], W=[ob_r])
                            t0 = (qb * 4 + qs) * 128
                            T.dma("sp", mh[t0:t0 + 128, 512 + h * 256:512 + (h + 1) * 256], ob_t[:], R=[ob_r], W=[r_mh])


def phase_gdn(P, C, l, heads=range(GH), tiles=NT):
    T = P.T
    ident_bf, r_ident_bf = C["ident_bf"]
    ident_f, r_ident_f = C["ident_f"]
    masks, r_masks = C["masks"]
    epsc, r_epsc = C["epsc"]
    mh = P.dram["mixed_half"]
    r_mh = P.dres["mixed_half"]
    with Pool_(P) as pl:
        gs = []
        for k in range(6):
            t, r = pl.sb("gs%d" % k, [128, NT * 8], F32)
            T.dma("sp", t[:], P.dram["gsc"][k], R=[P.dres["gsc"]], W=[r])
            gs.append((t, r))
        (beta, r_beta), (gc, r_gc), (egc, r_egc), (ekd, r_ekd), (glA, r_glA), (glB, r_glB) = gs
        nbeta, r_nbeta = pl.sb("nbeta", [128, NT * 8], F32)
        T.op("dve", lambda e: e.tensor_scalar(out=nbeta[:], in0=beta[:], scalar1=-1.0, scalar2=None, op0=ALU.mult),
             R=[r_beta], W=[r_nbeta])
        gnw, r_gnw = pl.sb("gnw", [128, 128], F32)
        T.dma("sp", gnw[:], P.dram["gnw%d" % l].partition_broadcast(128), R=[P.dres["gnw%d" % l]], W=[r_gnw])
        ones_f, r_ones_f = pl.sb("ones_f", [128, 128], F32)
        T.op("dve", lambda e: e.memset(ones_f[:], 1.0), W=[r_ones_f])
        qT, r_qT = pl.sb("g_qT", [128, S], BF16)
        kT, r_kT = pl.sb("g_kT", [128, S], BF16)
        ktm, r_ktm = pl.sb("g_ktm", [128, NT, 128], BF16)
        vtm, r_vtm = pl.sb("g_vtm", [128, NT, 128], BF16)
        BUF = []
        for d_ in range(2):
            B = {}
            B["obuf"] = pl.sb("g_obuf%d" % d_, [128, NT, 128], F32)
            B["Sf"] = pl.sb("g_S%d" % d_, [128, 128], F32)
            B["Sb"] = pl.sb("g_Sb%d" % d_, [128, 128], BF16)
            for nm in ("Ig", "tmp", "DT", "DTM", "kk", "qk", "X", "xtmp", "ta", "tb", "ts"):
                B[nm] = pl.sb("g_%s%d" % (nm, d_), [128, 128], F32)
            for nm in ("F0", "F1", "FT0", "FT1", "Xb", "qkm", "kg", "kd", "nwT", "vn"):
                B[nm] = pl.sb("g_%s%d" % (nm, d_), [128, 128], BF16)
            pq = []
            for k in range(4):
                t_, r_ = pl.ps("g_pq%d_%d" % (d_, k), [128, 128], F32)
                pq.append((t_[:], r_))
            B["pA"] = [pq[0], pq[1], pq[2]]
            B["pB"] = [pq[0], pq[3], pq[2]]
            B["pTr"] = pq[3]
            BUF.append(B)
        zt, r_zt = pl.sb("g_zt", [128, NT, 128], BF16)
        ssq, r_ssq = pl.sb("g_ssq", [128, 1], F32)
        junk, r_junk = pl.sb("g_junk", [128, 128], F32)
        on_, r_on = pl.sb("g_on", [128, 128], F32)
        onb = [pl.sb("g_onb%d" % i, [128, 128], BF16) for i in range(2)]

        def cp(en, out, in_, R, W, scale=None):
            if en == "act":
                if scale is None:
                    T.op("act", lambda e: e.copy(out=out, in_=in_), R=R, W=W)
                else:
                    T.op("act", lambda e: e.activation(out=out, in_=in_, func=AF.Copy, scale=scale), R=R, W=W)
            else:
                T.op("dve", lambda e: e.tensor_copy(out=out, in_=in_), R=R, W=W)

        def tile_step(h, d, tt, B):
            ms, mi = (0, 1) if d == 0 else (2, 3)
            col = tt * 8 + d * 4 + h
            tsl = slice(tt * 128, (tt + 1) * 128)
            gcc = gc[:, col:col + 1]
            pA, pB, pTr = B["pA"], B["pB"], B["pTr"]
            Ig, tmp, DT, DTM, kk_sb, qk_sb, X, xt_ = (B[k] for k in ("Ig", "tmp", "DT", "DTM", "kk", "qk", "X", "xtmp"))
            F_bf = [B["F0"], B["F1"]]
            FT_bf = [B["FT0"], B["FT1"]]
            X_bf, qkm, kg, kd, nwT, vn, ta, tb_, ts = (B[k] for k in ("Xb", "qkm", "kg", "kd", "nwT", "vn", "ta", "tb", "ts"))
            obuf, r_obuf = B["obuf"]
            Sf, r_Sf = B["Sf"]
            Sb, r_Sb = B["Sb"]
            T.op("pe", lambda e: e.matmul(pA[0][0], lhsT=kT[:, tsl], rhs=kT[:, tsl], start=True, stop=True),
                 R=[r_kT], W=[pA[0][1]]); yield
            T.op("pe", lambda e: e.matmul(pA[1][0], lhsT=kT[:, tsl], rhs=qT[:, tsl], start=True, stop=True),
                 R=[r_kT, r_qT], W=[pA[1][1]]); yield
            cp("act", kk_sb[0][:], pA[0][0], [pA[0][1]], [kk_sb[1]]); yield
            cp("act", qk_sb[0][:], pA[1][0], [pA[1][1]], [qk_sb[1]]); yield
            T.op("dve", lambda e: e.tensor_scalar(out=Ig[0][:], in0=ident_f[:], scalar1=gcc, scalar2=None, op0=ALU.mult),
                 R=[r_ident_f, r_gc], W=[Ig[1]]); yield
            T.op("pe", lambda e: e.matmul(pA[2][0], lhsT=ones_f[:], rhs=Ig[0][:], start=True, stop=True),
                 R=[r_ones_f, Ig[1]], W=[pA[2][1]]); yield
            T.op("dve", lambda e: e.tensor_scalar(out=tmp[0][:], in0=pA[2][0], scalar1=gcc, scalar2=0.0,
                                                  op0=ALU.subtract, op1=ALU.min), R=[pA[2][1], r_gc], W=[tmp[1]]); yield
            T.op("act", lambda e: e.activation(out=DT[0][:], in_=tmp[0][:], func=AF.Exp), R=[tmp[1]], W=[DT[1]]); yield
            T.op("dve", lambda e: e.tensor_mul(out=DTM[0][:], in0=DT[0][:], in1=masks[:, ms, :]),
                 R=[DT[1], r_masks], W=[DTM[1]]); yield
            T.op("dve", lambda e: e.scalar_tensor_tensor(out=X[0][:], in0=kk_sb[0][:], scalar=nbeta[:, col:col + 1],
                                                         in1=DTM[0][:], op0=ALU.mult, op1=ALU.mult),
                 R=[kk_sb[1], r_nbeta, DTM[1]], W=[X[1]]); yield
            fi = 0
            cp("act", F_bf[fi][0][:], X[0][:], [X[1]], [F_bf[fi][1]]); yield
            T.op("pe", lambda e: e.transpose(out=pTr[0], in_=X[0][:], identity=ident_f[:]),
                 R=[X[1], r_ident_f], W=[pTr[1]]); yield
            cp("act", FT_bf[fi][0][:], pTr[0], [pTr[1]], [FT_bf[fi][1]]); yield
            T.op("dve", lambda e: e.tensor_add(out=X[0][:], in0=X[0][:], in1=ident_f[:]), R=[X[1], r_ident_f], W=[X[1]]); yield
            cp("act", X_bf[0][:], X[0][:], [X[1]], [X_bf[1]]); yield
            T.op("dve", lambda e: e.tensor_mul(out=DTM[0][:], in0=DT[0][:], in1=masks[:, mi, :]),
                 R=[DT[1], r_masks], W=[DTM[1]]); yield
            T.op("dve", lambda e: e.tensor_mul(out=qkm[0][:], in0=qk_sb[0][:], in1=DTM[0][:]),
                 R=[qk_sb[1], DTM[1]], W=[qkm[1]]); yield
            T.op("dve", lambda e: e.tensor_scalar(out=kg[0][:], in0=ktm[:, tt, :], scalar1=egc[:, col:col + 1],
                                                  scalar2=None, op0=ALU.mult), R=[r_ktm, r_egc], W=[kg[1]]); yield
            T.op("dve", lambda e: e.tensor_scalar(out=kd[0][:], in0=ktm[:, tt, :], scalar1=ekd[:, col:col + 1],
                                                  scalar2=None, op0=ALU.mult), R=[r_ktm, r_ekd], W=[kd[1]]); yield
            for k in range(5):
                fo = 1 - fi
                T.op("pe", lambda e: e.matmul(pB[0][0], lhsT=FT_bf[fi][0][:], rhs=F_bf[fi][0][:], start=True, stop=True),
                     R=[FT_bf[fi][1], F_bf[fi][1]], W=[pB[0][1]]); yield
                T.op("pe", lambda e: e.matmul(pB[1][0], lhsT=F_bf[fi][0][:], rhs=FT_bf[fi][0][:], start=True, stop=True),
                     R=[FT_bf[fi][1], F_bf[fi][1]], W=[pB[1][1]]); yield
                cp("act", F_bf[fo][0][:], pB[0][0], [pB[0][1]], [F_bf[fo][1]]); yield
                cp("dve", FT_bf[fo][0][:], pB[1][0], [pB[1][1]], [FT_bf[fo][1]]); yield
                T.op("pe", lambda e: e.matmul(pB[2][0], lhsT=FT_bf[fo][0][:], rhs=X_bf[0][:], start=True, stop=True),
                     R=[FT_bf[fo][1], X_bf[1]], W=[pB[2][1]]); yield
                cp("act", xt_[0][:], pB[2][0], [pB[2][1]], [xt_[1]]); yield
                T.op("dve", lambda e: e.tensor_add(out=X[0][:], in0=X[0][:], in1=xt_[0][:]), R=[X[1], xt_[1]], W=[X[1]]); yield
                cp("act", X_bf[0][:], X[0][:], [X[1]], [X_bf[1]]); yield
                fi = fo
            T.op("pe", lambda e: e.matmul(pA[0][0], lhsT=kg[0][:], rhs=X_bf[0][:], start=True, stop=True),
                 R=[kg[1], X_bf[1]], W=[pA[0][1]]); yield
            cp("act", nwT[0][:], pA[0][0], [pA[0][1]], [nwT[1]], scale=-1.0); yield
            for ch in ((0, 1) if d == 0 else (1, 0)):
                ps_ = slice(ch * 64, ch * 64 + 64)
                csl = slice(tt * 128 + ch * 64, tt * 128 + ch * 64 + 64)
                glc = (glA if ch == 0 else glB)[:, col:col + 1]
                r_gl = r_glA if ch == 0 else r_glB
                T.op("pe", lambda e: e.matmul(pA[1][0][0:64, :], lhsT=X_bf[0][ps_, ps_], rhs=vtm[ps_, tt, :],
                                              start=True, stop=False), R=[X_bf[1], r_vtm], W=[pA[1][1]]); yield
                T.op("pe", lambda e: e.matmul(pA[1][0][0:64, :], lhsT=nwT[0][:, ps_], rhs=Sb[:],
                                              start=False, stop=True), R=[nwT[1], r_Sb], W=[pA[1][1]]); yield
                T.op("dve", lambda e: e.tensor_scalar(out=vn[0][ps_, :], in0=pA[1][0][0:64, :],
                                                      scalar1=beta[ps_, col:col + 1], scalar2=None, op0=ALU.mult),
                     R=[pA[1][1], r_beta], W=[vn[1]]); yield
                T.op("pe", lambda e: e.matmul(pA[2][0][0:64, :], lhsT=qT[:, csl], rhs=Sb[:], start=True, stop=True),
                     R=[r_qT, r_Sb], W=[pA[2][1]]); yield
                T.op("pe", lambda e: e.matmul(pB[0][0][0:64, :], lhsT=qkm[0][ps_, ps_], rhs=vn[0][ps_, :],
                                              start=True, stop=True), R=[qkm[1], vn[1]], W=[pB[0][1]]); yield
                T.op("pe", lambda e: e.matmul(pB[1][0], lhsT=kd[0][ps_, :], rhs=vn[0][ps_, :], start=True, stop=True),
                     R=[kd[1], vn[1]], W=[pB[1][1]]); yield
                cp("act", ta[0][ps_, :], pA[2][0][0:64, :], [pA[2][1]], [ta[1]]); yield
                cp("act", tb_[0][ps_, :], pB[0][0][0:64, :], [pB[0][1]], [tb_[1]]); yield
                T.op("dve", lambda e: e.scalar_tensor_tensor(out=obuf[ps_, tt, :], in0=ta[0][ps_, :],
                                                             scalar=egc[ps_, col:col + 1], in1=tb_[0][ps_, :],
                                                             op0=ALU.mult, op1=ALU.add),
                     R=[ta[1], r_egc, tb_[1]], W=[r_obuf]); yield
                cp("act", ts[0][:], pB[1][0], [pB[1][1]], [ts[1]]); yield
                T.op("dve", lambda e: e.scalar_tensor_tensor(out=Sf[:], in0=Sf[:], scalar=glc, in1=ts[0][:],
                                                             op0=ALU.mult, op1=ALU.add),
                     R=[r_Sf, r_gl, ts[1]], W=[r_Sf]); yield
                cp("act", Sb[:], Sf[:], [r_Sf], [r_Sb]); yield

        for h in heads:
            T.dma("sp", qT[:], P.dram["gqT"][h], R=[P.dres["gqT"]], W=[r_qT])
            T.dma("sp", kT[:], P.dram["gkT"][h], R=[P.dres["gkT"]], W=[r_kT])
            T.dma("sp", ktm[:], P.dram["gk_tm"][h].rearrange("(t p) d -> p t d", p=128), R=[P.dres["gk_tm"]], W=[r_ktm])
            T.dma("sp", vtm[:], P.dram["gv_tm"][h].rearrange("(t p) d -> p t d", p=128), R=[P.dres["gv_tm"]], W=[r_vtm])
            T.dma("sp", zt[:], P.dram["siluz"][:, h * 128:(h + 1) * 128].rearrange("(t p) d -> p t d", p=128),
                  R=[P.dres["siluz"]], W=[r_zt])
            for d in range(2):
                T.op("dve", lambda e: e.memset(BUF[d]["Sf"][0][:], 0.0), W=[BUF[d]["Sf"][1]])
                T.op("dve", lambda e: e.memset(BUF[d]["Sb"][0][:], 0.0), W=[BUF[d]["Sb"][1]])
            for step in range(tiles):
                gens = [tile_step(h, 0, step, BUF[0]), tile_step(h, 1, tiles - 1 - step, BUF[1])]
                while gens:
                    for g in list(gens):
                        try:
                            next(g)
                        except StopIteration:
                            gens.remove(g)
            ob0, r_ob0 = BUF[0]["obuf"]
            ob1, r_ob1 = BUF[1]["obuf"]
            for tt in range(tiles):
                T.op("dve", lambda e: e.tensor_add(out=ob0[:, tt, :], in0=ob0[:, tt, :], in1=ob1[:, tt, :]), R=[r_ob0, r_ob1], W=[r_ob0])
                T.op("dve", lambda e: e.memset(ssq[:], 0.0), W=[r_ssq])
                T.op("act", lambda e: e.activation(out=junk[:], in_=ob0[:, tt, :], func=AF.Square, accum_out=ssq[:]),
                     R=[r_ob0], W=[r_junk, r_ssq])
                T.op("act", lambda e: e.activation(out=ssq[:], in_=ssq[:], func=AF.Sqrt, bias=epsc[:], scale=1.0 / 128),
                     R=[r_ssq, r_epsc], W=[r_ssq])
                T.op("dve", lambda e: e.reciprocal(out=ssq[:], in_=ssq[:]), R=[r_ssq], W=[r_ssq])
                T.op("dve", lambda e: e.scalar_tensor_tensor(out=on_[:], in0=ob0[:, tt, :], scalar=ssq[:], in1=gnw[:],
                                                             op0=ALU.mult, op1=ALU.mult), R=[r_ob0, r_ssq, r_gnw], W=[r_on])
                ob_t, ob_r = onb[tt % 2]
                T.op("dve", lambda e: e.tensor_mul(out=ob_t[:], in0=on_[:], in1=zt[:, tt, :]), R=[r_on, r_zt], W=[ob_r])
                T.dma("sp", mh[tt * 128:(tt + 1) * 128, h * 128:(h + 1) * 128], ob_t[:], R=[ob_r], W=[r_mh])


PAIRS = [[0, 1], [2, 3], [4, 5], [6, 7]]


def pair_allgather(P, src, dst, nrows, rows_per_call):
    T = P.T
    sd, rs = P.dram[src], P.dres[src]
    dd, rd = P.dram[dst], P.dres[dst]
    ncall = nrows // rows_per_call
    for c in range(ncall):
        T._deps("pool", [rs], [rd], nowaw=True)
        inst = T.eng["pool"].collective_compute(
            "AllGather", ALU.bypass, replica_groups=PAIRS,
            ins=[sd[c * rows_per_call:(c + 1) * rows_per_call, :].opt()],
            outs=[dd[c].rearrange("r t w -> (r t) w").opt()])
        T._dma_done(inst, [rs], [rd], inc=1)


def phase3(P, C, l, xsrc):
    T = P.T
    ident_bf, r_ident_bf = C["ident_bf"]
    ident_f, r_ident_f = C["ident_f"]
    epsc, r_epsc = C["epsc"]
    pair_allgather(P, "mixed_half", "mg", S, 1024)
    mg, r_mg = P.dram["mg"], P.dres["mg"]
    xd, xr = P.dram[xsrc], P.dres[xsrc]
    x1d, x1r = P.dram["x1"], P.dres["x1"]
    x2d, x2r = P.dram["x2"], P.dres["x2"]
    wo = P.dram["w_out%d" % l].rearrange("(kc p) n -> p kc n", p=128)
    with Pool_(P) as pl:
        wob, r_wob = pl.sb("wob", [128, KC, D], BF16)
        for q4 in range(4):
            T.dma("pool", wob[:, q4 * 4:(q4 + 1) * 4, :], wo[:, q4 * 4:(q4 + 1) * 4, :], R=[P.dres["w_out%d" % l]], W=[r_wob])
        wr_f, r_wrf = pl.sb("wr_f", [128, KC, 16], F32)
        T.dma("sp", wr_f[:], P.dram["w_r%d" % l].rearrange("(kc p) n -> p kc n", p=128), R=[P.dres["w_r%d" % l]], W=[r_wrf])
        n2b, r_n2b = pl.sb("n2b", [128, D], F32)
        T.dma("sp", n2b[:], P.dram["n2w%d" % l].partition_broadcast(128), R=[P.dres["n2w%d" % l]], W=[r_n2b])
        aff, r_aff = pl.sb("aff", [128, NT, 16], F32)
        mt = [pl.sb("mt%d" % i, [128, D], BF16) for i in range(2)]
        mT = [pl.sb("mT%d" % i, [128, KC, 128], BF16) for i in range(2)]
        xt = [pl.sb("p3xt%d" % i, [128, D], F32) for i in range(2)]
        x1t = [pl.sb("x1t%d" % i, [128, D], F32) for i in range(2)]
        x2f, r_x2f = pl.sb("x2f", [128, D], F32)
        x2b = [pl.sb("x2b%d" % i, [128, D], BF16) for i in range(2)]
        x2T, r_x2T = pl.sb("x2T", [128, KC, 128], F32)
        junk, r_junk = pl.sb("p3junk", [128, D], BF16)
        ss, r_ss = pl.sb("p3ss", [128, 1], F32)
        mx, r_mx = pl.sb("p3mx", [128, 1], F32)
        lg, r_lg = pl.sb("p3lg", [128, 16], F32)
        ptr = [pl.ps("p3ptr%d" % i, [128, 8, 128], BF16) for i in range(2)]
        pacc = [pl.ps("p3acc%d" % i, [128, 512], F32) for i in range(2)]
        ptf = [pl.ps("p3ptf%d" % i, [128, 4, 128], F32) for i in range(2)]
        plg = pl.ps("p3lg", [128, 16], F32)
        for tt in range(NT):
            i = tt % 2
            m_t, m_r = mt[i]
            cidx, r0 = (tt * 128) // 1024, (tt * 128) % 1024
            for rk in range(2):
                T.dma("sp", m_t[:, rk * 1024:(rk + 1) * 1024], mg[cidx, rk, r0:r0 + 128, :], R=[r_mg], W=[m_r])
            x_t, x_r = xt[i]
            T.dma("sp", x_t[:], xd[tt * 128:(tt + 1) * 128, :], R=[xr], W=[x_r])
            mT_t, mT_r = mT[i]
            for hh in range(2):
                p_t, p_r = ptr[hh]
                for k in range(8):
                    kc = hh * 8 + k
                    T.op("pe", lambda e: e.transpose(out=p_t[:, k, :], in_=m_t[:, kc * 128:(kc + 1) * 128], identity=ident_bf[:]),
                         R=[m_r, r_ident_bf], W=[p_r])
                T.op("act", lambda e: e.copy(out=mT_t[:, hh * 8:(hh + 1) * 8, :], in_=p_t[:]), R=[p_r], W=[mT_r])
            x1_t, x1_r = x1t[i]
            for cb in range(4):
                pa_t, pa_r = pacc[cb % 2]
                for kc in range(KC):
                    T.op("pe", lambda e: e.matmul(pa_t[:], lhsT=mT_t[:, kc, :], rhs=wob[:, kc, cb * 512:(cb + 1) * 512],
                                                  start=(kc == 0), stop=(kc == KC - 1)), R=[mT_r, r_wob], W=[pa_r])
                T.op("act", lambda e: e.copy(out=x1_t[:, cb * 512:(cb + 1) * 512], in_=pa_t[:]), R=[pa_r], W=[x1_r])
            T.op("dve", lambda e: e.tensor_add(out=x1_t[:], in0=x1_t[:], in1=x_t[:]), R=[x1_r, x_r], W=[x1_r])
            T.dma("sp", x1d[tt * 128:(tt + 1) * 128, :], x1_t[:], R=[x1_r], W=[x1r])
            T.op("dve", lambda e: e.memset(ss[:], 0.0), W=[r_ss])
            T.op("act", lambda e: e.activation(out=junk[:], in_=x1_t[:], func=AF.Square, accum_out=ss[:]), R=[x1_r], W=[r_junk, r_ss])
            T.op("act", lambda e: e.activation(out=ss[:], in_=ss[:], func=AF.Sqrt, bias=epsc[:], scale=1.0 / D), R=[r_ss, r_epsc], W=[r_ss])
            T.op("dve", lambda e: e.reciprocal(out=ss[:], in_=ss[:]), R=[r_ss], W=[r_ss])
            T.op("dve", lambda e: e.scalar_tensor_tensor(out=x2f[:], in0=x1_t[:], scalar=ss[:], in1=n2b[:], op0=ALU.mult, op1=ALU.mult),
                 R=[x1_r, r_ss, r_n2b], W=[r_x2f])
            xb_t, xb_r = x2b[i]
            T.op("act", lambda e: e.copy(out=xb_t[:], in_=x2f[:]), R=[r_x2f], W=[xb_r])
            T.dma("sp", x2d[tt * 128:(tt + 1) * 128, :], xb_t[:], R=[xb_r], W=[x2r])
            for g4 in range(4):
                pf_t, pf_r = ptf[g4 % 2]
                for k in range(4):
                    kc = g4 * 4 + k
                    T.op("pe", lambda e: e.transpose(out=pf_t[:, k, :], in_=x2f[:, kc * 128:(kc + 1) * 128], identity=ident_f[:]),
                         R=[r_x2f, r_ident_f], W=[pf_r])
                T.op("act", lambda e: e.copy(out=x2T[:, g4 * 4:(g4 + 1) * 4, :], in_=pf_t[:]), R=[pf_r], W=[r_x2T])
            for kc in range(KC):
                T.op("pe", lambda e: e.matmul(plg[0][:], lhsT=x2T[:, kc, :], rhs=wr_f[:, kc, :], start=(kc == 0), stop=(kc == KC - 1)),
                     R=[r_x2T, r_wrf], W=[plg[1]])
            T.op("act", lambda e: e.copy(out=lg[:], in_=plg[0][:]), R=[plg[1]], W=[r_lg])
            T.op("dve", lambda e: e.reduce_max(out=mx[:], in_=lg[:], axis=AX.X), R=[r_lg], W=[r_mx])
            T.op("dve", lambda e: e.tensor_scalar(out=mx[:], in0=mx[:], scalar1=-1.0, scalar2=None, op0=ALU.mult), R=[r_mx], W=[r_mx])
            T.op("dve", lambda e: e.memset(ss[:], 0.0), W=[r_ss])
            T.op("act", lambda e: e.activation(out=lg[:], in_=lg[:], func=AF.Exp, bias=mx[:], accum_out=ss[:]), R=[r_lg, r_mx], W=[r_lg, r_ss])
            T.op("dve", lambda e: e.reciprocal(out=ss[:], in_=ss[:]), R=[r_ss], W=[r_ss])
            T.op("dve", lambda e: e.tensor_scalar(out=aff[:, tt, :], in0=lg[:], scalar1=ss[:], scalar2=None, op0=ALU.mult),
                 R=[r_lg, r_ss], W=[r_aff])
        T.dma("sp", P.dram["affd"], aff[:].rearrange("p t e -> p (t e)"), R=[r_aff], W=[P.dres["affd"]])


def phase4(P, C, l, xdst):
    T = P.T
    ident_bf, r_ident_bf = C["ident_bf"]
    masks, r_masks = C["masks"]
    x2d, x2r = P.dram["x2"], P.dres["x2"]
    wg = P.dram["w_g%d" % l]
    wu = P.dram["w_u%d" % l]
    wdn = P.dram["w_d%d" % l]
    with Pool_(P) as pl:
        aff, r_aff = pl.sb("aff4", [128, NT, 16], F32)
        T.dma("sp", aff[:].rearrange("p t e -> p (t e)"), P.dram["affd"], R=[P.dres["affd"]], W=[r_aff])
        ones_f, r_ones_f = pl.sb("ones4", [128, 128], F32)
        T.op("dve", lambda e: e.memset(ones_f[:], 1.0), W=[r_ones_f])
        iota, r_iota = pl.sb("iota", [128, 512], F32)
        T.dma("sp", iota[:], P.dram["iota"], R=[P.dres["iota"]], W=[r_iota])
        msk, r_msk = pl.sb("msk", [128, NT, NE], F32)
        gat, r_gat = pl.sb("gat", [128, NT, NE], F32)
        pos, r_pos = pl.sb("pos", [128, NT, NE], F32)
        prs = Pool_(P)
        prs.__enter__()
        lo, r_lo = prs.sb("lo", [128, NE], F32)
        hi, r_hi = prs.sb("hi", [128, NE], F32)
        mid, r_mid = prs.sb("mid", [128, NE], F32)
        cnt, r_cnt = prs.sb("cnt", [128, NE], F32)
        ge, r_ge = prs.sb("ge", [128, NE], F32)
        dl_, r_dl = prs.sb("dl_", [128, NE], F32)
        cmp, r_cmp = prs.sb("cmp", [128, NE, NT], F32)
        tot, r_tot = prs.sb("tot", [128, NT, NE], F32)
        offs, r_offs = prs.sb("offs", [128, NT, NE], F32)
        pcn = prs.ps("pcn", [128, NE], F32)
        ppos = prs.ps("ppos", [128, NT * NE], F32)
        T.op("dve", lambda e: e.memset(lo[:], 0.0), W=[r_lo])
        T.op("dve", lambda e: e.memset(hi[:], 1.0), W=[r_hi])
        for it in range(26):
            T.op("dve", lambda e: e.tensor_add(out=mid[:], in0=lo[:], in1=hi[:]), R=[r_lo, r_hi], W=[r_mid])
            T.op("dve", lambda e: e.tensor_scalar(out=mid[:], in0=mid[:], scalar1=0.5, scalar2=None, op0=ALU.mult), R=[r_mid], W=[r_mid])
            for ex in range(NE):
                T.op("dve", lambda e: e.tensor_scalar(out=cmp[:, ex, :], in0=aff[:, :, ex], scalar1=mid[:, ex:ex + 1], scalar2=None,
                                                      op0=ALU.is_ge), R=[r_aff, r_mid], W=[r_cmp])
            T.op("dve", lambda e: e.reduce_sum(out=cnt[:], in_=cmp[:], axis=AX.X), R=[r_cmp], W=[r_cnt])
            T.op("pe", lambda e: e.matmul(pcn[0][:], lhsT=ones_f[:], rhs=cnt[:], start=True, stop=True), R=[r_ones_f, r_cnt], W=[pcn[1]])
            T.op("dve", lambda e: e.tensor_scalar(out=ge[:], in0=pcn[0][:], scalar1=float(CAP), scalar2=None, op0=ALU.is_ge),
                 R=[pcn[1]], W=[r_ge])
            T.op("dve", lambda e: e.tensor_sub(out=dl_[:], in0=mid[:], in1=lo[:]), R=[r_mid, r_lo], W=[r_dl])
            T.op("dve", lambda e: e.tensor_mul(out=dl_[:], in0=dl_[:], in1=ge[:]), R=[r_dl, r_ge], W=[r_dl])
            T.op("dve", lambda e: e.tensor_add(out=lo[:], in0=lo[:], in1=dl_[:]), R=[r_lo, r_dl], W=[r_lo])
            T.op("dve", lambda e: e.tensor_sub(out=dl_[:], in0=hi[:], in1=mid[:]), R=[r_mid, r_hi], W=[r_dl])
            T.op("dve", lambda e: e.tensor_mul(out=dl_[:], in0=dl_[:], in1=ge[:]), R=[r_dl, r_ge], W=[r_dl])
            T.op("dve", lambda e: e.tensor_add(out=hi[:], in0=mid[:], in1=dl_[:]), R=[r_mid, r_dl], W=[r_hi])
        for ex in range(NE):
            T.op("dve", lambda e: e.tensor_scalar(out=msk[:, :, ex], in0=aff[:, :, ex], scalar1=lo[:, ex:ex + 1], scalar2=None,
                                                  op0=ALU.is_ge), R=[r_aff, r_lo], W=[r_msk])
        T.op("dve", lambda e: e.tensor_mul(out=gat[:], in0=msk[:], in1=aff[:, :, 0:NE]), R=[r_msk, r_aff], W=[r_gat])
        mflat = msk[:].rearrange("p t e -> p (t e)")
        T.op("pe", lambda e: e.matmul(ppos[0][:], lhsT=masks[:, 8, :], rhs=mflat, start=True, stop=True), R=[r_masks, r_msk], W=[ppos[1]])
        T.op("act", lambda e: e.copy(out=pos[:].rearrange("p t e -> p (t e)"), in_=ppos[0][:]), R=[ppos[1]], W=[r_pos])
        T.op("pe", lambda e: e.matmul(ppos[0][:], lhsT=ones_f[:], rhs=mflat, start=True, stop=True), R=[r_ones_f, r_msk], W=[ppos[1]])
        T.op("act", lambda e: e.copy(out=tot[:].rearrange("p t e -> p (t e)"), in_=ppos[0][:]), R=[ppos[1]], W=[r_tot])
        T.op("dve", lambda e: e.memset(offs[:, 0, :], 0.0), W=[r_offs])
        for tt in range(1, NT):
            T.op("dve", lambda e: e.tensor_add(out=offs[:, tt, :], in0=offs[:, tt - 1, :], in1=tot[:, tt - 1, :]), R=[r_offs, r_tot], W=[r_offs])
        T.op("dve", lambda e: e.tensor_add(out=pos[:], in0=pos[:], in1=offs[:]), R=[r_pos, r_offs], W=[r_pos])
        prs.__exit__(None, None, None)
        ye = [pl.sb("ye%d" % ex, [128, 4, D], BF16) for ex in range(NE)]
        with Pool_(P) as pa:
            x2t = [pa.sb("x2t%d" % i, [128, D], BF16) for i in range(1)] * 2
            sel = [pa.sb("sel%d" % i, [128, 512], BF16) for i in range(2)]
            xsT, r_xsT = pa.sb("xsT", [128, KC, 512], BF16)
            hT, r_hT = pa.sb("hT", [128, KC, 512], BF16)
            wgb = [pa.sb("wgb%d" % i, [128, KC, 128], BF16) for i in range(2)]
            wub = [pa.sb("wub%d" % i, [128, KC, 128], BF16) for i in range(2)]
            wdb = [pa.sb("wdb%d" % i, [128, KC, 128], BF16) for i in range(2)]
            sg, r_sg = pa.sb("sg", [128, 512], F32)
            su, r_su = pa.sb("su", [128, 512], F32)
            psel = [pa.ps("psel%d" % i, [128, 512], F32) for i in range(8)]
            for ex in range(NE):
                wge = wg[ex].rearrange("(kc p) n -> p kc n", p=128)
                wue = wu[ex].rearrange("(kc p) n -> p kc n", p=128)
                wde = wdn[ex].rearrange("(kc p) n -> p kc n", p=128)
                for half in range(2):
                    for tt in range(NT):
                        i = tt % 2
                        xt_t, xt_r = x2t[i]
                        s_t, s_r = sel[i]
                        T.dma("sp", xt_t[:], x2d[tt * 128:(tt + 1) * 128, :], R=[x2r], W=[xt_r])
                        T.op("dve", lambda e: e.tensor_scalar(out=s_t[:], in0=iota[:], scalar1=pos[:, tt, ex:ex + 1],
                                                              scalar2=msk[:, tt, ex:ex + 1], op0=ALU.is_equal, op1=ALU.mult),
                             R=[r_iota, r_pos, r_msk], W=[s_r])
                        for k in range(8):
                            kc = half * 8 + k
                            T.op("pe", lambda e: e.matmul(psel[k][0][:], lhsT=xt_t[:, kc * 128:(kc + 1) * 128], rhs=s_t[:],
                                                          start=(tt == 0), stop=(tt == NT - 1)), R=[xt_r, s_r], W=[psel[k][1]])
                    for k in range(8):
                        kc = half * 8 + k
                        T.op("act", lambda e: e.copy(out=xsT[:, kc, :], in_=psel[k][0][:]), R=[psel[k][1]], W=[r_xsT])
                for f in range(KC):
                    i = f % 2
                    g_t, g_r = wgb[i]
                    u_t, u_r = wub[i]
                    T.dma("pool", g_t[:], wge[:, :, f * 128:(f + 1) * 128], R=[P.dres["w_g%d" % l]], W=[g_r])
                    T.dma("pool", u_t[:], wue[:, :, f * 128:(f + 1) * 128], R=[P.dres["w_u%d" % l]], W=[u_r])
                    pg_t, pg_r = psel[(2 * f) % 8]
                    pu_t, pu_r = psel[(2 * f + 1) % 8]
                    for kc in range(KC):
                        T.op("pe", lambda e: e.matmul(pg_t[:], lhsT=g_t[:, kc, :], rhs=xsT[:, kc, :], start=(kc == 0), stop=(kc == KC - 1)),
                             R=[g_r, r_xsT], W=[pg_r])
                    for kc in range(KC):
                        T.op("pe", lambda e: e.matmul(pu_t[:], lhsT=u_t[:, kc, :], rhs=xsT[:, kc, :], start=(kc == 0), stop=(kc == KC - 1)),
                             R=[u_r, r_xsT], W=[pu_r])
                    T.op("act", lambda e: e.activation(out=sg[:], in_=pg_t[:], func=AF.Silu), R=[pg_r], W=[r_sg])
                    T.op("act", lambda e: e.copy(out=su[:], in_=pu_t[:]), R=[pu_r], W=[r_su])
                    T.op("dve", lambda e: e.tensor_mul(out=hT[:, f, :], in0=sg[:], in1=su[:]), R=[r_sg, r_su], W=[r_hT])
                for cb in range(16):
                    d_t, d_r = wdb[cb % 2]
                    T.dma("pool", d_t[:], wde[:, :, cb * 128:(cb + 1) * 128], R=[P.dres["w_d%d" % l]], W=[d_r])
                    for cs in range(4):
                        py_t, py_r = psel[(cb * 4 + cs) % 8]
                        for f in range(KC):
                            T.op("pe", lambda e: e.matmul(py_t[:, 0:128], lhsT=hT[:, f, cs * 128:(cs + 1) * 128], rhs=d_t[:, f, :],
                                                          start=(f == 0), stop=(f == KC - 1)), R=[r_hT, d_r], W=[py_r])
                        T.op("act", lambda e: e.copy(out=ye[ex][0][:, cs, cb * 128:(cb + 1) * 128], in_=py_t[:, 0:128]), R=[py_r], W=[ye[ex][1]])
        with Pool_(P) as pb:
            selg = [pb.sb("selg%d" % i, [128, 512], BF16) for i in range(2)]
            selT = [pb.sb("selT%d" % i, [128, 4, 128], BF16) for i in range(2)]
            yt = [pb.sb("yt%d" % i, [128, D], BF16) for i in range(2)]
            pT4 = [pb.ps("pT4_%d" % i, [128, 4, 128], BF16) for i in range(2)]
            py = [pb.ps("py%d" % i, [128, 512], F32) for i in range(4)]
            n = 0
            for tt in range(NT):
                for ex in range(NE):
                    i = n % 2
                    n += 1
                    s_t, s_r = selg[i]
                    T.op("dve", lambda e: e.tensor_scalar(out=s_t[:], in0=iota[:], scalar1=pos[:, tt, ex:ex + 1],
                                                          scalar2=gat[:, tt, ex:ex + 1], op0=ALU.is_equal, op1=ALU.mult),
                         R=[r_iota, r_pos, r_gat], W=[s_r])
                    p_t, p_r = pT4[i]
                    for cs in range(4):
                        T.op("pe", lambda e: e.transpose(out=p_t[:, cs, :], in_=s_t[:, cs * 128:(cs + 1) * 128], identity=ident_bf[:]),
                             R=[s_r, r_ident_bf], W=[p_r])
                    st_t, st_r = selT[i]
                    T.op("act", lambda e: e.copy(out=st_t[:], in_=p_t[:]), R=[p_r], W=[st_r])
                    for cb in range(4):
                        for cs in range(4):
                            T.op("pe", lambda e: e.matmul(py[cb][0][:], lhsT=st_t[:, cs, :], rhs=ye[ex][0][:, cs, cb * 512:(cb + 1) * 512],
                                                          start=(ex == 0 and cs == 0), stop=(ex == NE - 1 and cs == 3)),
                                 R=[st_r, ye[ex][1]], W=[py[cb][1]])
                y_t, y_r = yt[tt % 2]
                for cb in range(4):
                    T.op("act", lambda e: e.copy(out=y_t[:, cb * 512:(cb + 1) * 512], in_=py[cb][0][:]), R=[py[cb][1]], W=[y_r])
                T.dma("sp", P.dram["ypart"][tt * 128:(tt + 1) * 128, :], y_t[:], R=[y_r], W=[P.dres["ypart"]])
    pair_allgather(P, "ypart", "yg", S, 512)
    with Pool_(P) as pl:
        xt = [pl.sb("p5x%d" % i, [128, D], F32) for i in range(2)]
        ya = [pl.sb("p5a%d" % i, [128, 2, D], BF16) for i in range(2)]
        for tt in range(NT):
            i = tt % 2
            x_t, x_r = xt[i]
            a_t, a_r = ya[i]
            cidx, r0 = (tt * 128) // 512, (tt * 128) % 512
            T.dma("sp", x_t[:], P.dram["x1"][tt * 128:(tt + 1) * 128, :], R=[P.dres["x1"]], W=[x_r])
            for rk in range(2):
                T.dma("sp", a_t[:, rk, :], P.dram["yg"][cidx, rk, r0:r0 + 128, :], R=[P.dres["yg"]], W=[a_r])
            T.op("dve", lambda e: e.tensor_add(out=x_t[:], in0=x_t[:], in1=a_t[:, 0, :]), R=[x_r, a_r], W=[x_r])
            T.op("dve", lambda e: e.tensor_add(out=x_t[:], in0=x_t[:], in1=a_t[:, 1, :]), R=[x_r, a_r], W=[x_r])
            T.dma("sp", P.dram[xdst][tt * 128:(tt + 1) * 128, :], x_t[:], R=[x_r], W=[P.dres[xdst]])


def phase_final(P, C, xsrc):
    T = P.T
    epsc, r_epsc = C["epsc"]
    with Pool_(P) as pl:
        fw, r_fw = pl.sb("fw", [128, D], F32)
        T.dma("sp", fw[:], P.dram["fnw"].partition_broadcast(128), R=[P.dres["fnw"]], W=[r_fw])
        xt = [pl.sb("p6x%d" % i, [128, D], F32) for i in range(2)]
        junk, r_junk = pl.sb("p6j", [128, D], BF16)
        ss, r_ss = pl.sb("p6s", [128, 1], F32)
        for tt in range(NT):
            x_t, x_r = xt[tt % 2]
            T.dma("sp", x_t[:], P.dram[xsrc][tt * 128:(tt + 1) * 128, :], R=[P.dres[xsrc]], W=[x_r])
            T.op("dve", lambda e: e.memset(ss[:], 0.0), W=[r_ss])
            T.op("act", lambda e: e.activation(out=junk[:], in_=x_t[:], func=AF.Square, accum_out=ss[:]), R=[x_r], W=[r_junk, r_ss])
            T.op("act", lambda e: e.activation(out=ss[:], in_=ss[:], func=AF.Sqrt, bias=epsc[:], scale=1.0 / D), R=[r_ss, r_epsc], W=[r_ss])
            T.op("dve", lambda e: e.reciprocal(out=ss[:], in_=ss[:]), R=[r_ss], W=[r_ss])
            T.op("dve", lambda e: e.scalar_tensor_tensor(out=x_t[:], in0=x_t[:], scalar=ss[:], in1=fw[:], op0=ALU.mult, op1=ALU.mult),
                 R=[x_r, r_ss, r_fw], W=[x_r])
            T.dma("sp", P.dram["out"][tt * 128:(tt + 1) * 128, :], x_t[:], R=[x_r], W=[P.dres["out"]])


def build_program(layers=range(DEPTH), final=True, test_outputs=()):
    P = Prog(test_outputs=test_outputs)
    with P.es:
        declare_io(P, layers=layers)
        declare_scratch(P)
        with Pool_(P) as pc:
            C = load_consts(P, pc)
            src = "x"
            for l in layers:
                dst = "xa" if src != "xa" else "xb"
                P.marks = getattr(P, "marks", [])
                phase1(P, C, l, src)
                P.marks.append(("p1_%d" % l, dict(P.T.cnt)))
                phase_attn(P, C, l)
                P.marks.append(("attn_%d" % l, dict(P.T.cnt)))
                phase_gdn(P, C, l)
                P.marks.append(("gdn_%d" % l, dict(P.T.cnt)))
                phase3(P, C, l, src)
                P.marks.append(("p3_%d" % l, dict(P.T.cnt)))
                phase4(P, C, l, dst)
                P.marks.append(("p4_%d" % l, dict(P.T.cnt)))
                src = dst
            if final:
                phase_final(P, C, src)
        P.T.barrier()
    return P


_CACHE = {}


def kernel(**inputs):
    inputs = {k: np.asarray(v) for k, v in inputs.items()}
    if "P" not in _CACHE:
        _CACHE["P"] = build_program()
    P = _CACHE["P"]
    consts = host_consts()
    need = set(k for k in P.dram.keys())
    in_maps = []
    for c in range(8):
        m = core_inputs(inputs, c // 2, c % 2, consts)
        in_maps.append({k: v for k, v in m.items() if k in need})
    res = run_bass_kernel_spmd(P.nc, in_maps, core_ids=list(range(8)))
    out = np.stack([np.asarray(res.results[2 * b]["out"]) for b in range(4)], 0)
    return out.astype(np.float32)
```

```python
import contextlib
import math
import numpy as np
import ml_dtypes
import concourse.bass as bass
import concourse.mybir as mybir
from concourse.bass_utils import run_bass_kernel_spmd

F32 = mybir.dt.float32
BF16 = mybir.dt.bfloat16
I32 = mybir.dt.int32
AF = mybir.ActivationFunctionType
ALU = mybir.AluOpType
AX = mybir.AxisListType

S = 4096
D = 2048
KC = 16
NT = 32
DEPTH = 2
EPS = 1e-6
NCOL = 3600
GH = 4
DH = 2
NE = 8
CAP = 512


class Res:
    __slots__ = ("name", "w", "r", "dsem", "dname", "dcnt")

    def __init__(self, name):
        self.name = name
        self.w = None
        self.r = {}
        self.dsem = None
        self.dname = None
        self.dcnt = 0


class Tracker:
    def __init__(self, nc, es):
        self.nc = nc
        self.es = es
        self.eng = {"pe": nc.tensor, "dve": nc.vector, "act": nc.scalar, "pool": nc.gpsimd, "sp": nc.sync}
        self.sems = {}
        self.cnt = {}
        for k in ("pe", "dve", "act", "pool"):
            self.sems[k] = es.enter_context(nc.semaphore("e_" + k))
            self.cnt[k] = 0
        self.waited = {k: {} for k in self.eng}
        self.dcount = {}
        self.dfree = []
        self.ninst = 0

    def res(self, name):
        return Res(name)

    def _need(self, need, en, tk, raw):
        sname, val, owner = tk
        if owner == en and (en == "pe" or not raw):
            return
        if self.waited[en].get(sname, 0) >= val:
            return
        if need.get(sname, 0) < val:
            need[sname] = val

    def _emit(self, en, need):
        for sname, val in need.items():
            self.waited[en][sname] = val
            self.eng[en].wait_ge(self.sems[sname], val)
            self.ninst += 1

    def _wait(self, en, tk, raw):
        need = {}
        self._need(need, en, tk, raw)
        self._emit(en, need)

    def _deps(self, en, R, W, nowaw=False):
        need = {}
        for r in R:
            if r.w is not None:
                self._need(need, en, r.w, True)
        for w in W:
            if w.w is not None and not (nowaw and w.w[2] is None):
                self._need(need, en, w.w, False)
            for tk in w.r.values():
                self._need(need, en, tk, False)
        self._emit(en, need)

    def op(self, en, fn, R=(), W=()):
        self._deps(en, R, W)
        inst = fn(self.eng[en])
        self.cnt[en] += 1
        inst.then_inc(self.sems[en], 1)
        self.ninst += 1
        tk = (en, self.cnt[en], en)
        for r in R:
            r.r[en] = tk
        for w in W:
            w.w = tk
            w.r = {}
        return tk

    def dma(self, q, out, in_, R=(), W=(), nowaw=True, **kw):
        self._deps(q, R, W, nowaw=nowaw)
        inst = self.eng[q].dma_start(out=out, in_=in_, **kw)
        self._dma_done(inst, R, W)

    def _dsem(self, w):
        if w.dsem is None:
            if self.dfree:
                w.dname = self.dfree.pop()
            else:
                w.dname = "d_%d" % len(self.dcount)
                self.sems[w.dname] = self.es.enter_context(self.nc.semaphore(w.dname))
                self.dcount[w.dname] = 0
            w.dsem = self.sems[w.dname]

    def release(self, w):
        if w.dsem is not None:
            self.dfree.append(w.dname)
            w.dsem = None
            w.dname = None

    def _dma_done(self, inst, R, W, inc=16):
        w = W[0]
        self._dsem(w)
        self.dcount[w.dname] += inc
        if inc == 16:
            inst.then_inc(w.dsem, 16)
        else:
            inst.then_inc(w.dsem)
        self.ninst += 1
        tk = (w.dname, self.dcount[w.dname], None)
        for r in R:
            r.r[w.dname] = tk
        for ww in W:
            ww.w = tk
            ww.r = {}

    def barrier(self):
        for en in self.eng:
            need = {}
            for k in ("pe", "dve", "act", "pool"):
                if k != en and self.cnt[k] > 0:
                    self._need(need, en, (k, self.cnt[k], k), True)
            for nm, c in self.dcount.items():
                if c > 0:
                    self._need(need, en, (nm, c, None), True)
            self._emit(en, need)


def bf(a):
    return np.ascontiguousarray(a.astype(ml_dtypes.bfloat16))


def host_consts():
    c = {}
    c["ident_bf"] = bf(np.eye(128, dtype=np.float32))
    c["ident_f"] = np.eye(128, dtype=np.float32)
    rot = np.zeros((128, 128), np.float32)
    for m in range(64):
        rot[m + 64, m] = -1.0
        rot[m, m + 64] = 1.0
    c["rotT"] = bf(rot)
    c["ones_bf"] = bf(np.ones((128, 128), np.float32))
    half = 64
    inv_freq = (np.float32(10000.0) ** (-(np.arange(half, dtype=np.float32) / np.float32(half)))).astype(np.float32)
    ang = (np.arange(S, dtype=np.float32)[None, :] * inv_freq[:, None]).astype(np.float32)
    c["cos2"] = np.ascontiguousarray(np.concatenate([np.cos(ang), np.cos(ang)], 0).astype(np.float32))
    c["sin2"] = np.ascontiguousarray(np.concatenate([np.sin(ang), np.sin(ang)], 0).astype(np.float32))
    s = np.arange(128)[:, None]
    cc = np.arange(128)[None, :]
    same = (s // 64) == (cc // 64)
    m = np.zeros((9, 128, 128), np.float32)
    m[0] = same & (s < cc)
    m[1] = same & (s <= cc)
    m[2] = same & (s > cc)
    m[3] = same & (s >= cc)
    m[4] = same
    m[5] = (s < 64) & (cc >= 0)
    m[6] = (s >= 64) & (cc >= 0)
    m[7] = 1.0
    m[8] = (s < cc)
    c["masks"] = np.ascontiguousarray(m.transpose(1, 0, 2))
    c["iota"] = np.ascontiguousarray(np.tile(np.arange(512, dtype=np.float32)[None, :], (128, 1)))
    return c


def core_inputs(inputs, b, hf, consts):
    d = dict(consts)
    d["x"] = np.ascontiguousarray(inputs["x"][b])
    g0 = 512 * hf
    for l in range(DEPTH):
        w = inputs["w_in"][l]
        base = [0, 1024, 2048, 3072]
        cols = []
        cols += list(range(0 + g0, 0 + g0 + 512))
        cols += list(range(1024 + g0, 1024 + g0 + 512))
        cols += list(range(2048 + g0, 2048 + g0 + 512))
        dq0 = 4096 + 32
        cols += list(range(dq0 + g0, dq0 + g0 + 512))
        cols += list(range(dq0 + 1024 + g0, dq0 + 1024 + g0 + 512))
        cols += list(range(3072 + g0, 3072 + g0 + 512))
        cols += list(range(dq0 + 2048 + g0, dq0 + 2048 + g0 + 512))
        hb = [4096 + dd * 8 + 4 * hf + h for dd in range(2) for h in range(4)]
        ha = [4096 + 16 + dd * 8 + 4 * hf + h for dd in range(2) for h in range(4)]
        cols += hb + ha
        d["w_in%d" % l] = np.ascontiguousarray(w[:, cols])
        cw = inputs["conv_w"][l]
        ccols = []
        for grp in range(3):
            ccols += list(range(grp * 1024 + g0, grp * 1024 + g0 + 512))
        cws = cw[:, ccols]
        d["conv%d" % l] = np.ascontiguousarray(cws.reshape(5, 12, 128).transpose(2, 1, 0))
        al = inputs["a_log"][l][:, 4 * hf:4 * hf + 4].reshape(8, 1)
        dtb = inputs["dt_bias"][l][:, 4 * hf:4 * hf + 4].reshape(8, 1)
        d["alog%d" % l] = np.ascontiguousarray(al)
        d["dtb%d" % l] = np.ascontiguousarray(dtb)
        d["n1w%d" % l] = np.ascontiguousarray(inputs["norm1_w"][l].reshape(1, D))
        d["dlam%d" % l] = np.ascontiguousarray(inputs["diff_lambda"][l].reshape(1, 512))
        d["subw%d" % l] = np.ascontiguousarray(inputs["diff_subln_w"][l].reshape(1, 256))
        d["gnw%d" % l] = np.ascontiguousarray(inputs["gdn_norm_w"][l].reshape(1, 128))
        rows = list(range(0, 512)) + list(range(1024, 1536)) + list(range(512, 1024)) + list(range(1536, 2048))
        d["w_out%d" % l] = np.ascontiguousarray(inputs["w_out"][l][rows, :])
        ecols = list(range(8 * hf, 8 * hf + 8)) + list(range(8 * (1 - hf), 8 * (1 - hf) + 8))
        d["w_r%d" % l] = np.ascontiguousarray(inputs["w_router"][l][:, ecols])
        d["n2w%d" % l] = np.ascontiguousarray(inputs["norm2_w"][l].reshape(1, D))
        d["w_g%d" % l] = np.ascontiguousarray(inputs["w_gate"][l][8 * hf:8 * hf + 8])
        d["w_u%d" % l] = np.ascontiguousarray(inputs["w_up"][l][8 * hf:8 * hf + 8])
        d["w_d%d" % l] = np.ascontiguousarray(inputs["w_down"][l][8 * hf:8 * hf + 8])
    d["fnw"] = np.ascontiguousarray(inputs["final_norm_w"].reshape(1, D))
    return d


class Prog:
    def __init__(self, test_outputs=(), test_inputs=()):
        self.nc = bass.Bass("TRN2", target_bir_lowering=False)
        self.es = contextlib.ExitStack()
        self.T = Tracker(self.nc, self.es)
        self.test_outputs = set(test_outputs)
        self.test_inputs = set(test_inputs)
        self.dram = {}
        self.dres = {}

    def din(self, name, shape, dt):
        t = self.nc.dram_tensor(name, list(shape), dt, kind="ExternalInput").ap()
        self.dram[name] = t
        self.dres[name] = Res(name)
        return t

    def dscr(self, name, shape, dt, out=False):
        kind = "ExternalOutput" if (out or name in self.test_outputs) else "Internal"
        if name in self.test_inputs:
            kind = "ExternalInput"
        t = self.nc.dram_tensor(name, list(shape), dt, kind=kind).ap()
        self.dram[name] = t
        self.dres[name] = Res(name)
        return t


class Pool_:
    def __init__(self, P):
        self.P = P
        self.es = contextlib.ExitStack()
        self.created = []

    def __enter__(self):
        self.es.__enter__()
        return self

    def __exit__(self, *a):
        if a[0] is None:
            self.P.T.barrier()
            for r in self.created:
                self.P.T.release(r)
        return self.es.__exit__(*a)

    _uid = [0]

    def sb(self, name, shape, dt):
        Pool_._uid[0] += 1
        name = "%s_u%d" % (name, Pool_._uid[0])
        t = self.es.enter_context(self.P.nc.sbuf_tensor(name, list(shape), dt))
        r = Res(name)
        self.created.append(r)
        return t, r

    def ps(self, name, shape, dt):
        Pool_._uid[0] += 1
        name = "%s_u%d" % (name, Pool_._uid[0])
        t = self.es.enter_context(self.P.nc.psum_tensor(name, list(shape), dt))
        return t, Res(name)


def declare_io(P, layers=range(DEPTH)):
    P.din("x", [S, D], F32)
    P.din("ident_bf", [128, 128], BF16)
    P.din("ident_f", [128, 128], F32)
    P.din("rotT", [128, 128], BF16)
    P.din("ones_bf", [128, 128], BF16)
    P.din("cos2", [128, S], F32)
    P.din("sin2", [128, S], F32)
    P.din("masks", [128, 9, 128], F32)
    P.din("iota", [128, 512], F32)
    P.din("fnw", [1, D], F32)
    for l in layers:
        P.din("w_in%d" % l, [D, NCOL], F32)
        P.din("conv%d" % l, [128, 12, 5], F32)
        P.din("alog%d" % l, [8, 1], F32)
        P.din("dtb%d" % l, [8, 1], F32)
        P.din("n1w%d" % l, [1, D], F32)
        P.din("dlam%d" % l, [1, 512], F32)
        P.din("subw%d" % l, [1, 256], F32)
        P.din("gnw%d" % l, [1, 128], F32)
        P.din("w_out%d" % l, [D, D], F32)
        P.din("w_r%d" % l, [D, 16], F32)
        P.din("n2w%d" % l, [1, D], F32)
        P.din("w_g%d" % l, [NE, D, D], F32)
        P.din("w_u%d" % l, [NE, D, D], F32)
        P.din("w_d%d" % l, [NE, D, D], F32)


def declare_scratch(P):
    P.dscr("gqT", [GH, 128, S], BF16)
    P.dscr("gkT", [GH, 128, S], BF16)
    P.dscr("gk_tm", [GH, S, 128], BF16)
    P.dscr("gv_tm", [GH, S, 128], BF16)
    P.dscr("dqT", [4, 128, S], BF16)
    P.dscr("dkT", [4, 128, S], BF16)
    P.dscr("siluz", [S, 512], BF16)
    P.dscr("dv", [S, 512], BF16)
    P.dscr("gsc", [6, 128, NT * 8], F32)
    P.dscr("mixed_half", [S, 1024], BF16)
    P.dscr("mg", [4, 2, 1024, 1024], BF16)
    P.dscr("x1", [S, D], F32)
    P.dscr("x2", [S, D], BF16)
    P.dscr("affd", [128, NT * 16], F32)
    P.dscr("ypart", [S, D], BF16)
    P.dscr("yg", [8, 2, 512, D], BF16)
    P.dscr("xa", [S, D], F32)
    P.dscr("xb", [S, D], F32)
    P.dscr("out", [S, D], F32, out=True)


def load_consts(P, pool):
    T = P.T
    C = {}
    for nm, shape, dt in (("ident_bf", [128, 128], BF16), ("ident_f", [128, 128], F32), ("rotT", [128, 128], BF16),
                          ("ones_bf", [128, 128], BF16), ("masks", [128, 9, 128], F32)):
        t, r = pool.sb("c_" + nm, shape, dt)
        T.dma("sp", t[:], P.dram[nm], R=[P.dres[nm]], W=[r])
        C[nm] = (t, r)
    for nm, val in (("epsc", EPS), ("onec", 1.0)):
        t, r = pool.sb("c_" + nm, [128, 1], F32)
        T.op("dve", lambda e: e.memset(t[:], val), W=[r])
        C[nm] = (t, r)
    return C


def phase1(P, C, l, xsrc, upto=None, sections=("gdn", "rope", "tm", "bd"), dbg=None):
    T = P.T
    nc = P.nc
    ident_bf, r_ident_bf = C["ident_bf"]
    ident_f, r_ident_f = C["ident_f"]
    rotT, r_rotT = C["rotT"]
    ones_bf, r_ones = C["ones_bf"]
    masks, r_masks = C["masks"]
    epsc, r_epsc = C["epsc"]
    onec, r_onec = C["onec"]
    xd = P.dram[xsrc]
    xr = P.dres[xsrc]
    wd = P.dram["w_in%d" % l].rearrange("(kc p) n -> p kc n", p=128)
    wr = P.dres["w_in%d" % l]
    with Pool_(P) as pl:
        nT, r_nT = pl.sb("nT", [128, KC, S], BF16)
        with Pool_(P) as pa:
            w1b, r_w1b = pa.sb("w1b", [128, D], F32)
            T.dma("sp", w1b[:], P.dram["n1w%d" % l].partition_broadcast(128), R=[P.dres["n1w%d" % l]], W=[r_w1b])
            xt = [pa.sb("xt%d" % i, [128, D], F32) for i in range(2)]
            nt = [pa.sb("nt%d" % i, [128, D], BF16) for i in range(2)]
            junk, r_junk = pa.sb("junk", [128, D], BF16)
            ss = [pa.sb("ss%d" % i, [128, 1], F32) for i in range(2)]
            rstd = [pa.sb("rstd%d" % i, [128, 1], F32) for i in range(2)]
            ptr = [pa.ps("ptr%d" % i, [128, 8, 128], BF16) for i in range(2)]
            for tt in range(NT):
                i = tt % 2
                x_t, x_r = xt[i]
                n_t, n_r = nt[i]
                T.dma("sp", x_t[:], xd[tt * 128:(tt + 1) * 128, :], R=[xr], W=[x_r])
                T.op("dve", lambda e: e.memset(ss[i][0][:], 0.0), W=[ss[i][1]])
                T.op("act", lambda e: e.activation(out=junk[:], in_=x_t[:], func=AF.Square, accum_out=ss[i][0][:]),
                     R=[x_r], W=[r_junk, ss[i][1]])
                T.op("act", lambda e: e.activation(out=rstd[i][0][:], in_=ss[i][0][:], func=AF.Sqrt, bias=epsc[:],
                                                   scale=1.0 / D), R=[ss[i][1], r_epsc], W=[rstd[i][1]])
                T.op("dve", lambda e: e.reciprocal(out=rstd[i][0][:], in_=rstd[i][0][:]), R=[rstd[i][1]], W=[rstd[i][1]])
                T.op("dve", lambda e: e.scalar_tensor_tensor(out=n_t[:], in0=x_t[:], scalar=rstd[i][0][:], in1=w1b[:],
                                                             op0=ALU.mult, op1=ALU.mult),
                     R=[x_r, rstd[i][1], r_w1b], W=[n_r])
                for hh in range(2):
                    p_t, p_r = ptr[hh]
                    for k in range(8):
                        kc = hh * 8 + k
                        T.op("pe", lambda e: e.transpose(out=p_t[:, k, :], in_=n_t[:, kc * 128:(kc + 1) * 128],
                                                         identity=ident_bf[:]),
                             R=[n_r, r_ident_bf], W=[p_r])
                    en = "act" if hh == 0 else "dve"
                    if en == "act":
                        T.op("act", lambda e: e.copy(out=nT[:, hh * 8:(hh + 1) * 8, tt * 128:(tt + 1) * 128], in_=p_t[:]),
                             R=[p_r], W=[r_nT])
                    else:
                        T.op("dve", lambda e: e.tensor_copy(out=nT[:, hh * 8:(hh + 1) * 8, tt * 128:(tt + 1) * 128],
                                                            in_=p_t[:]), R=[p_r], W=[r_nT])
        if upto == "1a":
            if "dbg_nT" in P.dram:
                T.dma("sp", P.dram["dbg_nT"], nT[:], R=[r_nT], W=[P.dres["dbg_nT"]])
            return
        with Pool_(P) as pb:
            wblk = [pb.sb("wblk%d" % i, [128, KC, 256], BF16) for i in range(2)]
            raw = [pb.sb("raw%d" % i, [128, S + 4], BF16) for i in range(2)]
            acc = [pb.sb("acc%d" % i, [128, 1024], F32) for i in range(2)]
            sl = [pb.sb("sl%d" % i, [128, 1024], F32) for i in range(2)]
            sq = [pb.sb("sq%d" % i, [128, 1024], BF16) for i in range(1)] * 2
            rn = [pb.sb("rn%d" % i, [128, 1024], F32) for i in range(1)] * 2
            ob = [pb.sb("ob%d" % i, [128, 1024], BF16) for i in range(2)]
            tm = [pb.sb("tm%d" % i, [128, 8, 128], BF16) for i in range(1)] * 2
            cst = [pb.sb("cst%d" % i, [128, 512], F32) for i in range(1)] * 2
            snt = [pb.sb("snt%d" % i, [128, 512], F32) for i in range(1)] * 2
            tbf = [pb.sb("tbf%d" % i, [128, 512], BF16) for i in range(1)] * 2
            ra = [pb.sb("ra%d" % i, [128, 512], F32) for i in range(1)] * 2
            rb = [pb.sb("rb%d" % i, [128, 512], F32) for i in range(1)] * 2
            convw, r_convw = pb.sb("convw", [128, 12, 5], F32)
            T.dma("sp", convw[:], P.dram["conv%d" % l], R=[P.dres["conv%d" % l]], W=[r_convw])
            pacc = [pb.ps("pacc%d" % i, [128, 512], F32) for i in range(2)]
            prot = [pb.ps("prot%d" % i, [128, 512], F32) for i in range(1)]
            pss = [pb.ps("pss%d" % i, [128, 1024], F32) for i in range(1)]
            ptm = [pb.ps("ptm%d" % i, [128, 8, 128], BF16) for i in range(2)]
            for i in range(2):
                T.op("dve", lambda e: e.memset(raw[i][0][:], 0.0), W=[raw[i][1]])
            nblk = [0]

            def load_w(c0, ncols):
                i = nblk[0] % 2
                nblk[0] += 1
                w_t, w_r = wblk[i]
                for q4 in range(4):
                    T.dma("pool", w_t[:, q4 * 4:(q4 + 1) * 4, 0:ncols], wd[:, q4 * 4:(q4 + 1) * 4, c0:c0 + ncols],
                          R=[wr], W=[w_r])
                return w_t, w_r

            cnt = {"pacc": 0, "ptm": 0, "st": 0, "rp": 0}

            def proj_fm(w_t, w_r, cw, tb, m=128):
                i = cnt["pacc"] % 2
                cnt["pacc"] += 1
                p_t, p_r = pacc[i]
                for kc in range(KC):
                    T.op("pe", lambda e: e.matmul(p_t[0:m, :], lhsT=w_t[:, kc, cw:cw + m],
                                                  rhs=nT[:, kc, tb * 512:(tb + 1) * 512],
                                                  start=(kc == 0), stop=(kc == KC - 1)),
                         R=[w_r, r_nT], W=[p_r])
                return p_t, p_r

            for j in (range(12) if "gdn" in sections else ()):
                grp, h = j // 4, j % 4
                if j % 2 == 0:
                    w_t, w_r = load_w(j * 128, 256)
                cw = (j % 2) * 128
                r_t, r_r = raw[j % 2]
                for tb in range(8):
                    p_t, p_r = proj_fm(w_t, w_r, cw, tb)
                    T.op("act", lambda e: e.copy(out=r_t[:, 2 + tb * 512:2 + (tb + 1) * 512], in_=p_t[:]),
                         R=[p_r], W=[r_r])
                if j == 0 and "dbg_raw" in P.dram:
                    T.dma("sp", P.dram["dbg_raw"], r_t[:], R=[r_r], W=[P.dres["dbg_raw"]])
                    return
                for blk in range(4):
                    si = cnt["st"] % 2
                    cnt["st"] += 1
                    a_t, a_r = acc[si]
                    s_t, s_r = sl[si]
                    o_t, o_r = ob[si]
                    t0 = blk * 1024
                    T.op("dve", lambda e: e.tensor_scalar(out=a_t[:], in0=r_t[:, t0:t0 + 1024], scalar1=convw[:, j, 0:1],
                                                          scalar2=None, op0=ALU.mult), R=[r_r, r_convw], W=[a_r])
                    for k in range(1, 5):
                        T.op("dve", lambda e: e.scalar_tensor_tensor(out=a_t[:], in0=r_t[:, t0 + k:t0 + k + 1024],
                                                                     scalar=convw[:, j, k:k + 1], in1=a_t[:],
                                                                     op0=ALU.mult, op1=ALU.add),
                             R=[r_r, r_convw, a_r], W=[a_r])
                    if grp == 2:
                        T.op("act", lambda e: e.activation(out=o_t[:], in_=a_t[:], func=AF.Silu), R=[a_r], W=[o_r])
                    else:
                        q_t, q_r = sq[si]
                        n_t, n_r = rn[si]
                        T.op("act", lambda e: e.activation(out=s_t[:], in_=a_t[:], func=AF.Silu), R=[a_r], W=[s_r])
                        T.op("act", lambda e: e.activation(out=q_t[:], in_=s_t[:], func=AF.Square), R=[s_r], W=[q_r])
                        ps_t, ps_r = pss[0]
                        for hh in range(2):
                            T.op("pe", lambda e: e.matmul(ps_t[:, hh * 512:(hh + 1) * 512], lhsT=ones_bf[:],
                                                          rhs=q_t[:, hh * 512:(hh + 1) * 512], start=True, stop=True),
                                 R=[r_ones, q_r], W=[ps_r])
                        T.op("act", lambda e: e.activation(out=n_t[:], in_=ps_t[:], func=AF.Sqrt, bias=epsc[:]),
                             R=[ps_r, r_epsc], W=[n_r])
                        T.op("dve", lambda e: e.reciprocal(out=n_t[:], in_=n_t[:]), R=[n_r], W=[n_r])
                        qs = (128.0 ** -0.5) if grp == 0 else 1.0
                        T.op("dve", lambda e: e.scalar_tensor_tensor(out=o_t[:], in0=s_t[:], scalar=qs, in1=n_t[:],
                                                                     op0=ALU.mult, op1=ALU.mult),
                             R=[s_r, n_r], W=[o_r])
                    if grp < 2:
                        dst = P.dram["gqT" if grp == 0 else "gkT"]
                        T.dma("sp", dst[h, :, t0:t0 + 1024], o_t[:], R=[o_r],
                              W=[P.dres["gqT" if grp == 0 else "gkT"]])
                    if grp >= 1:
                        pi = cnt["ptm"] % 2
                        cnt["ptm"] += 1
                        pt_t, pt_r = ptm[pi]
                        tm_t, tm_r = tm[pi]
                        for k in range(8):
                            T.op("pe", lambda e: e.transpose(out=pt_t[:, k, :], in_=o_t[:, k * 128:(k + 1) * 128],
                                                             identity=ident_bf[:]), R=[o_r, r_ident_bf], W=[pt_r])
                        T.op("act", lambda e: e.copy(out=tm_t[:], in_=pt_t[:]), R=[pt_r], W=[tm_r])
                        nm = "gk_tm" if grp == 1 else "gv_tm"
                        T.dma("sp", P.dram[nm][h, t0:t0 + 1024, :].rearrange("(t p) d -> p t d", p=128), tm_t[:],
                              R=[tm_r], W=[P.dres[nm]])
            if upto == "gdn":
                return
            for j in (dbg["rope_j"] if dbg else (range(12, 20) if "rope" in sections else ())):
                jj = j - 12
                isq, cj = (jj < 4), jj % 4
                if j % 2 == 0:
                    w_t, w_r = load_w(j * 128, 256)
                cw = (j % 2) * 128
                nm = "dqT" if isq else "dkT"
                for tb in (dbg["rope_tb"] if dbg else range(8)):
                    i = cnt["rp"] % 2
                    cnt["rp"] += 1
                    c_t, c_r = cst[i]
                    s_t, s_r = snt[i]
                    sk = dbg.get("skip", "") if dbg else ""
                    if "c" in sk:
                        T.op("dve", lambda e: e.memset(c_t[:], 1.0), W=[c_r])
                        T.op("dve", lambda e: e.memset(s_t[:], 0.0), W=[s_r])
                    else:
                        T.dma("sp", c_t[:], P.dram["cos2"][:, tb * 512:(tb + 1) * 512], R=[P.dres["cos2"]], W=[c_r])
                        T.dma("sp", s_t[:], P.dram["sin2"][:, tb * 512:(tb + 1) * 512], R=[P.dres["sin2"]], W=[s_r])
                    p_t, p_r = proj_fm(w_t, w_r, cw, tb)
                    b_t, b_r = tbf[i]
                    T.op("act", lambda e: e.copy(out=b_t[:], in_=p_t[:]), R=[p_r], W=[b_r])
                    pr_t, pr_r = prot[0]
                    if "r" in sk:
                        T.op("pe", lambda e: e.matmul(pr_t[:], lhsT=ones_bf[:], rhs=b_t[:], start=True, stop=True),
                             R=[r_ones, b_r], W=[pr_r])
                    else:
                        T.op("pe", lambda e: e.matmul(pr_t[:], lhsT=rotT[:], rhs=b_t[:], start=True, stop=True),
                             R=[r_rotT, b_r], W=[pr_r])
                    a_t, a_r = ra[i]
                    bb_t, bb_r = rb[i]
                    o_t, o_r = ob[i]
                    if "m" in sk:
                        T.op("act", lambda e: e.copy(out=o_t[:, 0:512], in_=pr_t[:]), R=[pr_r], W=[o_r])
                    elif "1" in sk:
                        T.op("dve", lambda e: e.tensor_mul(out=a_t[:], in0=p_t[:], in1=c_t[:]), R=[p_r, c_r], W=[a_r])
                        T.op("act", lambda e: e.copy(out=o_t[:, 0:512], in_=a_t[:]), R=[a_r], W=[o_r])
                    elif "3" in sk:
                        T.op("dve", lambda e: e.tensor_mul(out=a_t[:], in0=c_t[:], in1=c_t[:]), R=[c_r], W=[a_r])
                        T.op("act", lambda e: e.copy(out=o_t[:, 0:512], in_=a_t[:]), R=[a_r], W=[o_r])
                    elif "2" in sk:
                        T.op("dve", lambda e: e.tensor_mul(out=a_t[:], in0=p_t[:], in1=c_t[:]), R=[p_r, c_r], W=[a_r])
                        T.op("dve", lambda e: e.tensor_mul(out=bb_t[:], in0=pr_t[:], in1=s_t[:]), R=[pr_r, s_r], W=[bb_r])
                        T.op("act", lambda e: e.copy(out=o_t[:, 0:512], in_=bb_t[:]), R=[a_r, bb_r], W=[o_r])
                    else:
                        T.op("act", lambda e: e.copy(out=a_t[:], in_=p_t[:]), R=[p_r], W=[a_r])
                        T.op("act", lambda e: e.copy(out=bb_t[:], in_=pr_t[:]), R=[pr_r], W=[bb_r])
                        T.op("dve", lambda e: e.tensor_mul(out=a_t[:], in0=a_t[:], in1=c_t[:]),
                             R=[a_r, c_r], W=[a_r])
                        T.op("dve", lambda e: e.tensor_mul(out=bb_t[:], in0=bb_t[:], in1=s_t[:]),
                             R=[bb_r, s_r], W=[bb_r])
                        T.op("dve", lambda e: e.tensor_add(out=o_t[:, 0:512], in0=a_t[:], in1=bb_t[:]),
                             R=[a_r, bb_r], W=[o_r])
                    T.dma("sp", P.dram[nm][cj, :, tb * 512:(tb + 1) * 512], o_t[:, 0:512], R=[o_r], W=[P.dres[nm]])
            if upto == "rope":
                return
            for g in range(2):
                nm = "siluz" if g == 0 else "dv"
                for cb in range(2):
                    c0 = 2560 + g * 512 + cb * 256
                    w_t, w_r = load_w(c0, 256)
                    for tt in range(NT):
                        i = cnt["pacc"] % 2
                        cnt["pacc"] += 1
                        p_t, p_r = pacc[i]
                        for kc in range(KC):
                            T.op("pe", lambda e: e.matmul(p_t[:, 0:256], lhsT=nT[:, kc, tt * 128:(tt + 1) * 128],
                                                          rhs=w_t[:, kc, 0:256], start=(kc == 0), stop=(kc == KC - 1)),
                                 R=[w_r, r_nT], W=[p_r])
                        si = cnt["st"] % 2
                        cnt["st"] += 1
                        o_t, o_r = ob[si]
                        T.op("act", lambda e: e.activation(out=o_t[:, 0:256], in_=p_t[:, 0:256],
                                                           func=(AF.Silu if g == 0 else AF.Copy)), R=[p_r], W=[o_r])
                        T.dma("sp", P.dram[nm][tt * 128:(tt + 1) * 128, cb * 256:(cb + 1) * 256], o_t[:, 0:256],
                              R=[o_r], W=[P.dres[nm]])
        if upto == "tm":
            return
        with Pool_(P) as pg:
            w_t, w_r = pg.sb("wsm", [128, KC, 16], BF16)
            T.dma("pool", w_t[:], wd[:, :, 3584:3600], R=[wr], W=[w_r])
            pacc2 = [pg.ps("pacc2_%d" % i, [128, 512], F32) for i in range(2)]
            psm = [pg.ps("psm0", [128, 512], F32)]
            pcnt = [0]

            def proj_fm(w_t, w_r, cw, tb, m=128):
                i = pcnt[0] % 2
                pcnt[0] += 1
                p_t, p_r = pacc2[i]
                for kc in range(KC):
                    T.op("pe", lambda e: e.matmul(p_t[0:m, :], lhsT=w_t[:, kc, cw:cw + m],
                                                  rhs=nT[:, kc, tb * 512:(tb + 1) * 512],
                                                  start=(kc == 0), stop=(kc == KC - 1)),
                         R=[w_r, r_nT], W=[p_r])
                return p_t, p_r
            bfm, r_bfm = pg.sb("bfm", [8, S], F32)
            gfm, r_gfm = pg.sb("gfm", [8, S], F32)
            alog, r_alog = pg.sb("alog", [8, 1], F32)
            dtb, r_dtb = pg.sb("dtb", [8, 1], F32)
            nega, r_nega = pg.sb("nega", [8, 1], F32)
            T.dma("sp", alog[:], P.dram["alog%d" % l], R=[P.dres["alog%d" % l]], W=[r_alog])
            T.dma("sp", dtb[:], P.dram["dtb%d" % l], R=[P.dres["dtb%d" % l]], W=[r_dtb])
            T.op("act", lambda e: e.activation(out=nega[:], in_=alog[:], func=AF.Exp), R=[r_alog], W=[r_nega])
            T.op("dve", lambda e: e.tensor_scalar(out=nega[:], in0=nega[:], scalar1=-1.0, scalar2=None, op0=ALU.mult),
                 R=[r_nega], W=[r_nega])
            for tb in range(8):
                p_t, p_r = proj_fm(w_t, w_r, 0, tb, m=8)
                T.op("act", lambda e: e.activation(out=bfm[:, tb * 512:(tb + 1) * 512], in_=p_t[0:8, :],
                                                   func=AF.Sigmoid), R=[p_r], W=[r_bfm])
            for tb in range(8):
                p_t, p_r = proj_fm(w_t, w_r, 8, tb, m=8)
                T.op("act", lambda e: e.activation(out=gfm[:, tb * 512:(tb + 1) * 512], in_=p_t[0:8, :],
                                                   func=AF.Exp, bias=dtb[:]), R=[p_r, r_dtb], W=[r_gfm])
            T.op("act", lambda e: e.activation(out=gfm[:], in_=gfm[:], func=AF.Ln, bias=onec[0:8, :]), R=[r_gfm, r_onec], W=[r_gfm])
            T.op("dve", lambda e: e.tensor_scalar(out=gfm[:], in0=gfm[:], scalar1=nega[:], scalar2=None, op0=ALU.mult),
                 R=[r_gfm, r_nega], W=[r_gfm])
            btm, r_btm = pg.sb("btm", [128, NT, 8], F32)
            gtm, r_gtm = pg.sb("gtm", [128, NT, 8], F32)
            gcs, r_gcs = pg.sb("gcs", [128, NT, 8], F32)
            gto, r_gto = pg.sb("gto", [128, NT, 8], F32)
            ex1, r_ex1 = pg.sb("ex1", [128, NT, 8], F32)
            ex2, r_ex2 = pg.sb("ex2", [128, NT, 8], F32)
            ex3, r_ex3 = pg.sb("ex3", [128, 2, NT, 8], F32)
            ps_t, ps_r = psm[0]
            psv = ps_t[:, 0:256].rearrange("p (t h) -> p t h", h=8)
            for src, r_src, dst, r_dst in ((bfm, r_bfm, btm, r_btm), (gfm, r_gfm, gtm, r_gtm)):
                for tt in range(NT):
                    T.op("pe", lambda e: e.transpose(out=psv[:, tt, :], in_=src[:, tt * 128:(tt + 1) * 128],
                                                     identity=ident_f[0:8, 0:8]), R=[r_src, r_ident_f], W=[ps_r])
                T.op("dve", lambda e: e.tensor_copy(out=dst[:], in_=psv), R=[ps_r], W=[r_dst])
            T.op("pe", lambda e: e.matmul(psv[:, :, 0:4], lhsT=masks[:, 1, :], rhs=gtm[:, :, 0:4], start=True, stop=True),
                 R=[r_masks, r_gtm], W=[ps_r])
            T.op("pe", lambda e: e.matmul(psv[:, :, 4:8], lhsT=masks[:, 3, :], rhs=gtm[:, :, 4:8], start=True, stop=True),
                 R=[r_masks, r_gtm], W=[ps_r])
            T.op("dve", lambda e: e.tensor_copy(out=gcs[:], in_=psv), R=[ps_r], W=[r_gcs])
            T.op("pe", lambda e: e.matmul(ps_t[:, 0:256], lhsT=masks[:, 4, :], rhs=gtm[:].rearrange("p t h -> p (t h)"),
                                          start=True, stop=True), R=[r_masks, r_gtm], W=[ps_r])
            T.op("dve", lambda e: e.tensor_copy(out=gto[:], in_=psv), R=[ps_r], W=[r_gto])
            T.op("act", lambda e: e.activation(out=ex1[:], in_=gcs[:], func=AF.Exp), R=[r_gcs], W=[r_ex1])
            T.op("dve", lambda e: e.tensor_tensor(out=ex2[:], in0=gto[:], in1=gcs[:], op=ALU.subtract),
                 R=[r_gto, r_gcs], W=[r_ex2])
            T.op("act", lambda e: e.activation(out=ex2[:], in_=ex2[:], func=AF.Exp), R=[r_ex2], W=[r_ex2])
            for ab in range(2):
                T.op("pe", lambda e: e.matmul(ps_t[:, 0:256], lhsT=masks[:, 5 + ab, :],
                                              rhs=gtm[:].rearrange("p t h -> p (t h)"), start=True, stop=True),
                     R=[r_masks, r_gtm], W=[ps_r])
                T.op("act", lambda e: e.activation(out=ex3[:, ab], in_=psv, func=AF.Exp), R=[ps_r], W=[r_ex3])
            gsc = P.dram["gsc"]
            rg = P.dres["gsc"]
            for k, (src, r_src) in enumerate(((btm, r_btm), (gcs, r_gcs), (ex1, r_ex1), (ex2, r_ex2))):
                T.dma("sp", gsc[k], src[:].rearrange("p t h -> p (t h)"), R=[r_src], W=[rg])
            for ab in range(2):
                T.dma("sp", gsc[4 + ab], ex3[:, ab].rearrange("p t h -> p (t h)"), R=[r_ex3], W=[rg])
    T.barrier()


def phase_attn(P, C, l):
    T = P.T
    lam_init = 0.8 - 0.6 * math.exp(-0.3 * l)
    scale = 128.0 ** -0.5
    epsc, r_epsc = C["epsc"]
    mh = P.dram["mixed_half"]
    r_mh = P.dres["mixed_half"]
    with Pool_(P) as pl:
        dlb, r_dlb = pl.sb("dlb", [128, 512], F32)
        prod, r_prod = pl.sb("prod", [128, 256], F32)
        s12, r_s12 = pl.sb("s12", [128, 2], F32)
        nlam, r_nlam = pl.sb("nlam", [128, 1], F32)
        subw, r_subw = pl.sb("subw", [128, 256], F32)
        T.dma("sp", dlb[:], P.dram["dlam%d" % l].partition_broadcast(128), R=[P.dres["dlam%d" % l]], W=[r_dlb])
        T.dma("sp", subw[:], P.dram["subw%d" % l].partition_broadcast(128), R=[P.dres["subw%d" % l]], W=[r_subw])
        T.op("dve", lambda e: e.tensor_mul(out=prod[:, 0:128], in0=dlb[:, 0:128], in1=dlb[:, 128:256]), R=[r_dlb], W=[r_prod])
        T.op("dve", lambda e: e.tensor_mul(out=prod[:, 128:256], in0=dlb[:, 256:384], in1=dlb[:, 384:512]), R=[r_dlb], W=[r_prod])
        T.op("dve", lambda e: e.reduce_sum(out=s12[:, 0:1], in_=prod[:, 0:128], axis=AX.X), R=[r_prod], W=[r_s12])
        T.op("dve", lambda e: e.reduce_sum(out=s12[:, 1:2], in_=prod[:, 128:256], axis=AX.X), R=[r_prod], W=[r_s12])
        T.op("act", lambda e: e.activation(out=s12[:], in_=s12[:], func=AF.Exp), R=[r_s12], W=[r_s12])
        T.op("dve", lambda e: e.tensor_sub(out=nlam[:], in0=s12[:, 1:2], in1=s12[:, 0:1]), R=[r_s12], W=[r_nlam])
        T.op("dve", lambda e: e.tensor_scalar(out=nlam[:], in0=nlam[:], scalar1=-lam_init, scalar2=None, op0=ALU.add),
             R=[r_nlam], W=[r_nlam])
        T.op("dve", lambda e: e.tensor_scalar(out=subw[:], in0=subw[:], scalar1=1.0 - lam_init, scalar2=None, op0=ALU.mult),
             R=[r_subw], W=[r_subw])
        vp, r_vp = pl.sb("vp", [128, NT, 257], BF16)
        qT2, r_qT2 = pl.sb("qT2", [128, 2, S], BF16)
        kT2, r_kT2 = pl.sb("kT2", [128, 2, S], BF16)
        pT = [pl.sb("pT%d" % i, [128, 512], BF16) for i in range(2)]
        ev = [pl.sb("ev%d" % i, [128, 257], F32) for i in range(2)]
        o0 = [pl.sb("o0_%d" % i, [128, 256], F32) for i in range(4)]
        oc, r_oc = pl.sb("oc", [128, 256], F32)
        junk, r_junk = pl.sb("junk2", [128, 256], F32)
        rc, r_rc = pl.sb("rc", [128, 1], F32)
        ssq, r_ssq = pl.sb("ssq", [128, 1], F32)
        onb = [pl.sb("onb%d" % i, [128, 256], BF16) for i in range(2)]
        pss = [pl.ps("pss%d" % i, [128, 512], F32) for i in range(2)]
        po = [pl.ps("po%d" % i, [128, 512], F32) for i in range(4)]
        n_on = 0
        for h in range(2):
            T.op("dve", lambda e: e.memset(vp[:, :, 256:257], 1.0), W=[r_vp])
            T.dma("sp", vp[:, :, 0:256], P.dram["dv"][:, h * 256:(h + 1) * 256].rearrange("(t p) d -> p t d", p=128),
                  R=[P.dres["dv"]], W=[r_vp], nowaw=False)
            for half in range(2):
                T.dma("sp", qT2[:, half, :], P.dram["dqT"][2 * h + half], R=[P.dres["dqT"]], W=[r_qT2])
                T.dma("sp", kT2[:, half, :], P.dram["dkT"][2 * h + half], R=[P.dres["dkT"]], W=[r_kT2])
            for qb in range(8):
                for half in range(2):
                    def s_mm(kt_):
                        ps_t_, ps_r_ = pss[kt_ % 2]
                        T.op("pe", lambda e: e.matmul(ps_t_[:], lhsT=kT2[:, half, kt_ * 128:(kt_ + 1) * 128],
                                                      rhs=qT2[:, half, qb * 512:(qb + 1) * 512], start=True, stop=True),
                             R=[r_kT2, r_qT2], W=[ps_r_])
                    s_mm(0)
                    for kt in range(NT):
                        ps_t, ps_r = pss[kt % 2]
                        p_t, p_r = pT[kt % 2]
                        if kt + 1 < NT:
                            s_mm(kt + 1)
                        T.op("act", lambda e: e.activation(out=p_t[:], in_=ps_t[:], func=AF.Exp, scale=scale),
                             R=[ps_r], W=[p_r])
                        for qs in range(4):
                            T.op("pe", lambda e: e.matmul(po[qs][0][:, 0:257], lhsT=p_t[:, qs * 128:(qs + 1) * 128],
                                                          rhs=vp[:, kt, :], start=(kt == 0), stop=(kt == NT - 1)),
                                 R=[p_r, r_vp], W=[po[qs][1]])
                    for qs in range(4):
                        e_t, e_r = ev[qs % 2]
                        T.op("act", lambda e: e.copy(out=e_t[:], in_=po[qs][0][:, 0:257]), R=[po[qs][1]], W=[e_r])
                        T.op("dve", lambda e: e.reciprocal(out=rc[:], in_=e_t[:, 256:257]), R=[e_r], W=[r_rc])
                        if half == 0:
                            T.op("dve", lambda e: e.tensor_scalar(out=o0[qs][0][:], in0=e_t[:, 0:256], scalar1=rc[:],
                                                                  scalar2=None, op0=ALU.mult), R=[e_r, r_rc], W=[o0[qs][1]])
                        else:
                            T.op("dve", lambda e: e.tensor_mul(out=rc[:], in0=rc[:], in1=nlam[:]), R=[r_rc, r_nlam], W=[r_rc])
                            T.op("dve", lambda e: e.scalar_tensor_tensor(out=oc[:], in0=e_t[:, 0:256], scalar=rc[:],
                                                                         in1=o0[qs][0][:], op0=ALU.mult, op1=ALU.add),
                                 R=[e_r, r_rc, o0[qs][1]], W=[r_oc])
                            T.op("dve", lambda e: e.memset(ssq[:], 0.0), W=[r_ssq])
                            T.op("act", lambda e: e.activation(out=junk[:], in_=oc[:], func=AF.Square, accum_out=ssq[:]),
                                 R=[r_oc], W=[r_junk, r_ssq])
                            T.op("act", lambda e: e.activation(out=ssq[:], in_=ssq[:], func=AF.Sqrt, bias=epsc[:],
                                                               scale=1.0 / 256), R=[r_ssq, r_epsc], W=[r_ssq])
                            T.op("dve", lambda e: e.reciprocal(out=ssq[:], in_=ssq[:]), R=[r_ssq], W=[r_ssq])
                            ob_t, ob_r = onb[n_on % 2]
                            n_on += 1
                            T.op("dve", lambda e: e.scalar_tensor_tensor(out=ob_t[:], in0=oc[:], scalar=ssq[:], in1=subw[:],
                                                                         op0=ALU.mult, op1=ALU.mult),
                                 R=[r_oc, r_ssq, r_subw], W=[ob_r])
                            t0 = (qb * 4 + qs) * 128
                            T.dma("sp", mh[t0:t0 + 128, 512 + h * 256:512 + (h + 1) * 256], ob_t[:], R=[ob_r], W=[r_mh])


def phase_gdn(P, C, l, heads=range(GH), tiles=NT):
    T = P.T
    ident_bf, r_ident_bf = C["ident_bf"]
    ident_f, r_ident_f = C["ident_f"]
    masks, r_masks = C["masks"]
    epsc, r_epsc = C["epsc"]
    mh = P.dram["mixed_half"]
    r_mh = P.dres["mixed_half"]
    with Pool_(P) as pl:
        gs = []
        for k in range(6):
            t, r = pl.sb("gs%d" % k, [128, NT * 8], F32)
            T.dma("sp", t[:], P.dram["gsc"][k], R=[P.dres["gsc"]], W=[r])
            gs.append((t, r))
        (beta, r_beta), (gc, r_gc), (egc, r_egc), (ekd, r_ekd), (glA, r_glA), (glB, r_glB) = gs
        nbeta, r_nbeta = pl.sb("nbeta", [128, NT * 8], F32)
        T.op("dve", lambda e: e.tensor_scalar(out=nbeta[:], in0=beta[:], scalar1=-1.0, scalar2=None, op0=ALU.mult),
             R=[r_beta], W=[r_nbeta])
        gnw, r_gnw = pl.sb("gnw", [128, 128], F32)
        T.dma("sp", gnw[:], P.dram["gnw%d" % l].partition_broadcast(128), R=[P.dres["gnw%d" % l]], W=[r_gnw])
        ones_f, r_ones_f = pl.sb("ones_f", [128, 128], F32)
        T.op("dve", lambda e: e.memset(ones_f[:], 1.0), W=[r_ones_f])
        qT, r_qT = pl.sb("g_qT", [128, S], BF16)
        kT, r_kT = pl.sb("g_kT", [128, S], BF16)
        ktm, r_ktm = pl.sb("g_ktm", [128, NT, 128], BF16)
        vtm, r_vtm = pl.sb("g_vtm", [128, NT, 128], BF16)
        BUF = []
        for d_ in range(2):
            B = {}
            B["obuf"] = pl.sb("g_obuf%d" % d_, [128, NT, 128], F32)
            B["Sf"] = pl.sb("g_S%d" % d_, [128, 128], F32)
            B["Sb"] = pl.sb("g_Sb%d" % d_, [128, 128], BF16)
            for nm in ("Ig", "tmp", "DT", "DTM", "kk", "qk", "X", "xtmp", "ta", "tb", "ts"):
                B[nm] = pl.sb("g_%s%d" % (nm, d_), [128, 128], F32)
            for nm in ("F0", "F1", "FT0", "FT1", "Xb", "qkm", "kg", "kd", "nwT", "vn"):
                B[nm] = pl.sb("g_%s%d" % (nm, d_), [128, 128], BF16)
            pq = []
            for k in range(4):
                t_, r_ = pl.ps("g_pq%d_%d" % (d_, k), [128, 128], F32)
                pq.append((t_[:], r_))
            B["pA"] = [pq[0], pq[1], pq[2]]
            B["pB"] = [pq[0], pq[3], pq[2]]
            B["pTr"] = pq[3]
            BUF.append(B)
        zt, r_zt = pl.sb("g_zt", [128, NT, 128], BF16)
        ssq, r_ssq = pl.sb("g_ssq", [128, 1], F32)
        junk, r_junk = pl.sb("g_junk", [128, 128], F32)
        on_, r_on = pl.sb("g_on", [128, 128], F32)
        onb = [pl.sb("g_onb%d" % i, [128, 128], BF16) for i in range(2)]

        def cp(en, out, in_, R, W, scale=None):
            if en == "act":
                if scale is None:
                    T.op("act", lambda e: e.copy(out=out, in_=in_), R=R, W=W)
                else:
                    T.op("act", lambda e: e.activation(out=out, in_=in_, func=AF.Copy, scale=scale), R=R, W=W)
            else:
                T.op("dve", lambda e: e.tensor_copy(out=out, in_=in_), R=R, W=W)

        def tile_step(h, d, tt, B):
            ms, mi = (0, 1) if d == 0 else (2, 3)
            col = tt * 8 + d * 4 + h
            tsl = slice(tt * 128, (tt + 1) * 128)
            gcc = gc[:, col:col + 1]
            pA, pB, pTr = B["pA"], B["pB"], B["pTr"]
            Ig, tmp, DT, DTM, kk_sb, qk_sb, X, xt_ = (B[k] for k in ("Ig", "tmp", "DT", "DTM", "kk", "qk", "X", "xtmp"))
            F_bf = [B["F0"], B["F1"]]
            FT_bf = [B["FT0"], B["FT1"]]
            X_bf, qkm, kg, kd, nwT, vn, ta, tb_, ts = (B[k] for k in ("Xb", "qkm", "kg", "kd", "nwT", "vn", "ta", "tb", "ts"))
            obuf, r_obuf = B["obuf"]
            Sf, r_Sf = B["Sf"]
            Sb, r_Sb = B["Sb"]
            T.op("pe", lambda e: e.matmul(pA[0][0], lhsT=kT[:, tsl], rhs=kT[:, tsl], start=True, stop=True),
                 R=[r_kT], W=[pA[0][1]]); yield
            T.op("pe", lambda e: e.matmul(pA[1][0], lhsT=kT[:, tsl], rhs=qT[:, tsl], start=True, stop=True),
                 R=[r_kT, r_qT], W=[pA[1][1]]); yield
            cp("act", kk_sb[0][:], pA[0][0], [pA[0][1]], [kk_sb[1]]); yield
            cp("act", qk_sb[0][:], pA[1][0], [pA[1][1]], [qk_sb[1]]); yield
            T.op("dve", lambda e: e.tensor_scalar(out=Ig[0][:], in0=ident_f[:], scalar1=gcc, scalar2=None, op0=ALU.mult),
                 R=[r_ident_f, r_gc], W=[Ig[1]]); yield
            T.op("pe", lambda e: e.matmul(pA[2][0], lhsT=ones_f[:], rhs=Ig[0][:], start=True, stop=True),
                 R=[r_ones_f, Ig[1]], W=[pA[2][1]]); yield
            T.op("dve", lambda e: e.tensor_scalar(out=tmp[0][:], in0=pA[2][0], scalar1=gcc, scalar2=0.0,
                                                  op0=ALU.subtract, op1=ALU.min), R=[pA[2][1], r_gc], W=[tmp[1]]); yield
            T.op("act", lambda e: e.activation(out=DT[0][:], in_=tmp[0][:], func=AF.Exp), R=[tmp[1]], W=[DT[1]]); yield
            T.op("dve", lambda e: e.tensor_mul(out=DTM[0][:], in0=DT[0][:], in1=masks[:, ms, :]),
                 R=[DT[1], r_masks], W=[DTM[1]]); yield
            T.op("dve", lambda e: e.scalar_tensor_tensor(out=X[0][:], in0=kk_sb[0][:], scalar=nbeta[:, col:col + 1],
                                                         in1=DTM[0][:], op0=ALU.mult, op1=ALU.mult),
                 R=[kk_sb[1], r_nbeta, DTM[1]], W=[X[1]]); yield
            fi = 0
            cp("act", F_bf[fi][0][:], X[0][:], [X[1]], [F_bf[fi][1]]); yield
            T.op("pe", lambda e: e.transpose(out=pTr[0], in_=X[0][:], identity=ident_f[:]),
                 R=[X[1], r_ident_f], W=[pTr[1]]); yield
            cp("act", FT_bf[fi][0][:], pTr[0], [pTr[1]], [FT_bf[fi][1]]); yield
            T.op("dve", lambda e: e.tensor_add(out=X[0][:], in0=X[0][:], in1=ident_f[:]), R=[X[1], r_ident_f], W=[X[1]]); yield
            cp("act", X_bf[0][:], X[0][:], [X[1]], [X_bf[1]]); yield
            T.op("dve", lambda e: e.tensor_mul(out=DTM[0][:], in0=DT[0][:], in1=masks[:, mi, :]),
                 R=[DT[1], r_masks], W=[DTM[1]]); yield
            T.op("dve", lambda e: e.tensor_mul(out=qkm[0][:], in0=qk_sb[0][:], in1=DTM[0][:]),
                 R=[qk_sb[1], DTM[1]], W=[qkm[1]]); yield
            T.op("dve", lambda e: e.tensor_scalar(out=kg[0][:], in0=ktm[:, tt, :], scalar1=egc[:, col:col + 1],
                                                  scalar2=None, op0=ALU.mult), R=[r_ktm, r_egc], W=[kg[1]]); yield
            T.op("dve", lambda e: e.tensor_scalar(out=kd[0][:], in0=ktm[:, tt, :], scalar1=ekd[:, col:col + 1],
                                                  scalar2=None, op0=ALU.mult), R=[r_ktm, r_ekd], W=[kd[1]]); yield
            for k in range(5):
                fo = 1 - fi
                T.op("pe", lambda e: e.matmul(pB[0][0], lhsT=FT_bf[fi][0][:], rhs=F_bf[fi][0][:], start=True, stop=True),
                     R=[FT_bf[fi][1], F_bf[fi][1]], W=[pB[0][1]]); yield
                T.op("pe", lambda e: e.matmul(pB[1][0], lhsT=F_bf[fi][0][:], rhs=FT_bf[fi][0][:], start=True, stop=True),
                     R=[FT_bf[fi][1], F_bf[fi][1]], W=[pB[1][1]]); yield
                cp("act", F_bf[fo][0][:], pB[0][0], [pB[0][1]], [F_bf[fo][1]]); yield
                cp("dve", FT_bf[fo][0][:], pB[1][0], [pB[1][1]], [FT_bf[fo][1]]); yield
                T.op("pe", lambda e: e.matmul(pB[2][0], lhsT=FT_bf[fo][0][:], rhs=X_bf[0][:], start=True, stop=True),
                     R=[FT_bf[fo][1], X_bf[1]], W=[pB[2][1]]); yield
                cp("act", xt_[0][:], pB[2][0], [pB[2][1]], [xt_[1]]); yield
                T.op("dve", lambda e: e.tensor_add(out=X[0][:], in0=X[0][:], in1=xt_[0][:]), R=[X[1], xt_[1]], W=[X[1]]); yield
                cp("act", X_bf[0][:], X[0][:], [X[1]], [X_bf[1]]); yield
                fi = fo
            T.op("pe", lambda e: e.matmul(pA[0][0], lhsT=kg[0][:], rhs=X_bf[0][:], start=True, stop=True),
                 R=[kg[1], X_bf[1]], W=[pA[0][1]]); yield
            cp("act", nwT[0][:], pA[0][0], [pA[0][1]], [nwT[1]], scale=-1.0); yield
            for ch in ((0, 1) if d == 0 else (1, 0)):
                ps_ = slice(ch * 64, ch * 64 + 64)
                csl = slice(tt * 128 + ch * 64, tt * 128 + ch * 64 + 64)
                glc = (glA if ch == 0 else glB)[:, col:col + 1]
                r_gl = r_glA if ch == 0 else r_glB
                T.op("pe", lambda e: e.matmul(pA[1][0][0:64, :], lhsT=X_bf[0][ps_, ps_], rhs=vtm[ps_, tt, :],
                                              start=True, stop=False), R=[X_bf[1], r_vtm], W=[pA[1][1]]); yield
                T.op("pe", lambda e: e.matmul(pA[1][0][0:64, :], lhsT=nwT[0][:, ps_], rhs=Sb[:],
                                              start=False, stop=True), R=[nwT[1], r_Sb], W=[pA[1][1]]); yield
                T.op("dve", lambda e: e.tensor_scalar(out=vn[0][ps_, :], in0=pA[1][0][0:64, :],
                                                      scalar1=beta[ps_, col:col + 1], scalar2=None, op0=ALU.mult),
                     R=[pA[1][1], r_beta], W=[vn[1]]); yield
                T.op("pe", lambda e: e.matmul(pA[2][0][0:64, :], lhsT=qT[:, csl], rhs=Sb[:], start=True, stop=True),
                     R=[r_qT, r_Sb], W=[pA[2][1]]); yield
                T.op("pe", lambda e: e.matmul(pB[0][0][0:64, :], lhsT=qkm[0][ps_, ps_], rhs=vn[0][ps_, :],
                                              start=True, stop=True), R=[qkm[1], vn[1]], W=[pB[0][1]]); yield
                T.op("pe", lambda e: e.matmul(pB[1][0], lhsT=kd[0][ps_, :], rhs=vn[0][ps_, :], start=True, stop=True),
                     R=[kd[1], vn[1]], W=[pB[1][1]]); yield
                cp("act", ta[0][ps_, :], pA[2][0][0:64, :], [pA[2][1]], [ta[1]]); yield
                cp("act", tb_[0][ps_, :], pB[0][0][0:64, :], [pB[0][1]], [tb_[1]]); yield
                T.op("dve", lambda e: e.scalar_tensor_tensor(out=obuf[ps_, tt, :], in0=ta[0][ps_, :],
                                                             scalar=egc[ps_, col:col + 1], in1=tb_[0][ps_, :],
                                                             op0=ALU.mult, op1=ALU.add),
                     R=[ta[1], r_egc, tb_[1]], W=[r_obuf]); yield
                cp("act", ts[0][:], pB[1][0], [pB[1][1]], [ts[1]]); yield
                T.op("dve", lambda e: e.scalar_tensor_tensor(out=Sf[:], in0=Sf[:], scalar=glc, in1=ts[0][:],
                                                             op0=ALU.mult, op1=ALU.add),
                     R=[r_Sf, r_gl, ts[1]], W=[r_Sf]); yield
                cp("act", Sb[:], Sf[:], [r_Sf], [r_Sb]); yield

        for h in heads:
            T.dma("sp", qT[:], P.dram["gqT"][h], R=[P.dres["gqT"]], W=[r_qT])
            T.dma("sp", kT[:], P.dram["gkT"][h], R=[P.dres["gkT"]], W=[r_kT])
            T.dma("sp", ktm[:], P.dram["gk_tm"][h].rearrange("(t p) d -> p t d", p=128), R=[P.dres["gk_tm"]], W=[r_ktm])
            T.dma("sp", vtm[:], P.dram["gv_tm"][h].rearrange("(t p) d -> p t d", p=128), R=[P.dres["gv_tm"]], W=[r_vtm])
            T.dma("sp", zt[:], P.dram["siluz"][:, h * 128:(h + 1) * 128].rearrange("(t p) d -> p t d", p=128),
                  R=[P.dres["siluz"]], W=[r_zt])
            for d in range(2):
                T.op("dve", lambda e: e.memset(BUF[d]["Sf"][0][:], 0.0), W=[BUF[d]["Sf"][1]])
                T.op("dve", lambda e: e.memset(BUF[d]["Sb"][0][:], 0.0), W=[BUF[d]["Sb"][1]])
            for step in range(tiles):
                gens = [tile_step(h, 0, step, BUF[0]), tile_step(h, 1, tiles - 1 - step, BUF[1])]
                while gens:
                    for g in list(gens):
                        try:
                            next(g)
                        except StopIteration:
                            gens.remove(g)
            ob0, r_ob0 = BUF[0]["obuf"]
            ob1, r_ob1 = BUF[1]["obuf"]
            for tt in range(tiles):
                T.op("dve", lambda e: e.tensor_add(out=ob0[:, tt, :], in0=ob0[:, tt, :], in1=ob1[:, tt, :]), R=[r_ob0, r_ob1], W=[r_ob0])
                T.op("dve", lambda e: e.memset(ssq[:], 0.0), W=[r_ssq])
                T.op("act", lambda e: e.activation(out=junk[:], in_=ob0[:, tt, :], func=AF.Square, accum_out=ssq[:]),
                     R=[r_ob0], W=[r_junk, r_ssq])
                T.op("act", lambda e: e.activation(out=ssq[:], in_=ssq[:], func=AF.Sqrt, bias=epsc[:], scale=1.0 / 128),
                     R=[r_ssq, r_epsc], W=[r_ssq])
                T.op("dve", lambda e: e.reciprocal(out=ssq[:], in_=ssq[:]), R=[r_ssq], W=[r_ssq])
                T.op("dve", lambda e: e.scalar_tensor_tensor(out=on_[:], in0=ob0[:, tt, :], scalar=ssq[:], in1=gnw[:],
                                                             op0=ALU.mult, op1=ALU.mult), R=[r_ob0, r_ssq, r_gnw], W=[r_on])
                ob_t, ob_r = onb[tt % 2]
                T.op("dve", lambda e: e.tensor_mul(out=ob_t[:], in0=on_[:], in1=zt[:, tt, :]), R=[r_on, r_zt], W=[ob_r])
                T.dma("sp", mh[tt * 128:(tt + 1) * 128, h * 128:(h + 1) * 128], ob_t[:], R=[ob_r], W=[r_mh])


PAIRS = [[0, 1], [2, 3], [4, 5], [6, 7]]


def pair_allgather(P, src, dst, nrows, rows_per_call):
    T = P.T
    sd, rs = P.dram[src], P.dres[src]
    dd, rd = P.dram[dst], P.dres[dst]
    ncall = nrows // rows_per_call
    for c in range(ncall):
        T._deps("pool", [rs], [rd], nowaw=True)
        inst = T.eng["pool"].collective_compute(
            "AllGather", ALU.bypass, replica_groups=PAIRS,
            ins=[sd[c * rows_per_call:(c + 1) * rows_per_call, :].opt()],
            outs=[dd[c].rearrange("r t w -> (r t) w").opt()])
        T._dma_done(inst, [rs], [rd], inc=1)


def phase3(P, C, l, xsrc):
    T = P.T
    ident_bf, r_ident_bf = C["ident_bf"]
    ident_f, r_ident_f = C["ident_f"]
    epsc, r_epsc = C["epsc"]
    pair_allgather(P, "mixed_half", "mg", S, 1024)
    mg, r_mg = P.dram["mg"], P.dres["mg"]
    xd, xr = P.dram[xsrc], P.dres[xsrc]
    x1d, x1r = P.dram["x1"], P.dres["x1"]
    x2d, x2r = P.dram["x2"], P.dres["x2"]
    wo = P.dram["w_out%d" % l].rearrange("(kc p) n -> p kc n", p=128)
    with Pool_(P) as pl:
        wob, r_wob = pl.sb("wob", [128, KC, D], BF16)
        for q4 in range(4):
            T.dma("pool", wob[:, q4 * 4:(q4 + 1) * 4, :], wo[:, q4 * 4:(q4 + 1) * 4, :], R=[P.dres["w_out%d" % l]], W=[r_wob])
        wr_f, r_wrf = pl.sb("wr_f", [128, KC, 16], F32)
        T.dma("sp", wr_f[:], P.dram["w_r%d" % l].rearrange("(kc p) n -> p kc n", p=128), R=[P.dres["w_r%d" % l]], W=[r_wrf])
        n2b, r_n2b = pl.sb("n2b", [128, D], F32)
        T.dma("sp", n2b[:], P.dram["n2w%d" % l].partition_broadcast(128), R=[P.dres["n2w%d" % l]], W=[r_n2b])
        aff, r_aff = pl.sb("aff", [128, NT, 16], F32)
        mt = [pl.sb("mt%d" % i, [128, D], BF16) for i in range(2)]
        mT = [pl.sb("mT%d" % i, [128, KC, 128], BF16) for i in range(2)]
        xt = [pl.sb("p3xt%d" % i, [128, D], F32) for i in range(2)]
        x1t = [pl.sb("x1t%d" % i, [128, D], F32) for i in range(2)]
        x2f, r_x2f = pl.sb("x2f", [128, D], F32)
        x2b = [pl.sb("x2b%d" % i, [128, D], BF16) for i in range(2)]
        x2T, r_x2T = pl.sb("x2T", [128, KC, 128], F32)
        junk, r_junk = pl.sb("p3junk", [128, D], BF16)
        ss, r_ss = pl.sb("p3ss", [128, 1], F32)
        mx, r_mx = pl.sb("p3mx", [128, 1], F32)
        lg, r_lg = pl.sb("p3lg", [128, 16], F32)
        ptr = [pl.ps("p3ptr%d" % i, [128, 8, 128], BF16) for i in range(2)]
        pacc = [pl.ps("p3acc%d" % i, [128, 512], F32) for i in range(2)]
        ptf = [pl.ps("p3ptf%d" % i, [128, 4, 128], F32) for i in range(2)]
        plg = pl.ps("p3lg", [128, 16], F32)
        for tt in range(NT):
            i = tt % 2
            m_t, m_r = mt[i]
            cidx, r0 = (tt * 128) // 1024, (tt * 128) % 1024
            for rk in range(2):
                T.dma("sp", m_t[:, rk * 1024:(rk + 1) * 1024], mg[cidx, rk, r0:r0 + 128, :], R=[r_mg], W=[m_r])
            x_t, x_r = xt[i]
            T.dma("sp", x_t[:], xd[tt * 128:(tt + 1) * 128, :], R=[xr], W=[x_r])
            mT_t, mT_r = mT[i]
            for hh in range(2):
                p_t, p_r = ptr[hh]
                for k in range(8):
                    kc = hh * 8 + k
                    T.op("pe", lambda e: e.transpose(out=p_t[:, k, :], in_=m_t[:, kc * 128:(kc + 1) * 128], identity=ident_bf[:]),
                         R=[m_r, r_ident_bf], W=[p_r])
                T.op("act", lambda e: e.copy(out=mT_t[:, hh * 8:(hh + 1) * 8, :], in_=p_t[:]), R=[p_r], W=[mT_r])
            x1_t, x1_r = x1t[i]
            for cb in range(4):
                pa_t, pa_r = pacc[cb % 2]
                for kc in range(KC):
                    T.op("pe", lambda e: e.matmul(pa_t[:], lhsT=mT_t[:, kc, :], rhs=wob[:, kc, cb * 512:(cb + 1) * 512],
                                                  start=(kc == 0), stop=(kc == KC - 1)), R=[mT_r, r_wob], W=[pa_r])
                T.op("act", lambda e: e.copy(out=x1_t[:, cb * 512:(cb + 1) * 512], in_=pa_t[:]), R=[pa_r], W=[x1_r])
            T.op("dve", lambda e: e.tensor_add(out=x1_t[:], in0=x1_t[:], in1=x_t[:]), R=[x1_r, x_r], W=[x1_r])
            T.dma("sp", x1d[tt * 128:(tt + 1) * 128, :], x1_t[:], R=[x1_r], W=[x1r])
            T.op("dve", lambda e: e.memset(ss[:], 0.0), W=[r_ss])
            T.op("act", lambda e: e.activation(out=junk[:], in_=x1_t[:], func=AF.Square, accum_out=ss[:]), R=[x1_r], W=[r_junk, r_ss])
            T.op("act", lambda e: e.activation(out=ss[:], in_=ss[:], func=AF.Sqrt, bias=epsc[:], scale=1.0 / D), R=[r_ss, r_epsc], W=[r_ss])
            T.op("dve", lambda e: e.reciprocal(out=ss[:], in_=ss[:]), R=[r_ss], W=[r_ss])
            T.op("dve", lambda e: e.scalar_tensor_tensor(out=x2f[:], in0=x1_t[:], scalar=ss[:], in1=n2b[:], op0=ALU.mult, op1=ALU.mult),
                 R=[x1_r, r_ss, r_n2b], W=[r_x2f])
            xb_t, xb_r = x2b[i]
            T.op("act", lambda e: e.copy(out=xb_t[:], in_=x2f[:]), R=[r_x2f], W=[xb_r])
            T.dma("sp", x2d[tt * 128:(tt + 1) * 128, :], xb_t[:], R=[xb_r], W=[x2r])
            for g4 in range(4):
                pf_t, pf_r = ptf[g4 % 2]
                for k in range(4):
                    kc = g4 * 4 + k
                    T.op("pe", lambda e: e.transpose(out=pf_t[:, k, :], in_=x2f[:, kc * 128:(kc + 1) * 128], identity=ident_f[:]),
                         R=[r_x2f, r_ident_f], W=[pf_r])
                T.op("act", lambda e: e.copy(out=x2T[:, g4 * 4:(g4 + 1) * 4, :], in_=pf_t[:]), R=[pf_r], W=[r_x2T])
            for kc in range(KC):
                T.op("pe", lambda e: e.matmul(plg[0][:], lhsT=x2T[:, kc, :], rhs=wr_f[:, kc, :], start=(kc == 0), stop=(kc == KC - 1)),
                     R=[r_x2T, r_wrf], W=[plg[1]])
            T.op("act", lambda e: e.copy(out=lg[:], in_=plg[0][:]), R=[plg[1]], W=[r_lg])
            T.op("dve", lambda e: e.reduce_max(out=mx[:], in_=lg[:], axis=AX.X), R=[r_lg], W=[r_mx])
            T.op("dve", lambda e: e.tensor_scalar(out=mx[:], in0=mx[:], scalar1=-1.0, scalar2=None, op0=ALU.mult), R=[r_mx], W=[r_mx])
            T.op("dve", lambda e: e.memset(ss[:], 0.0), W=[r_ss])
            T.op("act", lambda e: e.activation(out=lg[:], in_=lg[:], func=AF.Exp, bias=mx[:], accum_out=ss[:]), R=[r_lg, r_mx], W=[r_lg, r_ss])
            T.op("dve", lambda e: e.reciprocal(out=ss[:], in_=ss[:]), R=[r_ss], W=[r_ss])
            T.op("dve", lambda e: e.tensor_scalar(out=aff[:, tt, :], in0=lg[:], scalar1=ss[:], scalar2=None, op0=ALU.mult),
                 R=[r_lg, r_ss], W=[r_aff])
        T.dma("sp", P.dram["affd"], aff[:].rearrange("p t e -> p (t e)"), R=[r_aff], W=[P.dres["affd"]])


def phase4(P, C, l, xdst):
    T = P.T
    ident_bf, r_ident_bf = C["ident_bf"]
    masks, r_masks = C["masks"]
    x2d, x2r = P.dram["x2"], P.dres["x2"]
    wg = P.dram["w_g%d" % l]
    wu = P.dram["w_u%d" % l]
    wdn = P.dram["w_d%d" % l]
    with Pool_(P) as pl:
        aff, r_aff = pl.sb("aff4", [128, NT, 16], F32)
        T.dma("sp", aff[:].rearrange("p t e -> p (t e)"), P.dram["affd"], R=[P.dres["affd"]], W=[r_aff])
        ones_f, r_ones_f = pl.sb("ones4", [128, 128], F32)
        T.op("dve", lambda e: e.memset(ones_f[:], 1.0), W=[r_ones_f])
        iota, r_iota = pl.sb("iota", [128, 512], F32)
        T.dma("sp", iota[:], P.dram["iota"], R=[P.dres["iota"]], W=[r_iota])
        msk, r_msk = pl.sb("msk", [128, NT, NE], F32)
        gat, r_gat = pl.sb("gat", [128, NT, NE], F32)
        pos, r_pos = pl.sb("pos", [128, NT, NE], F32)
        prs = Pool_(P)
        prs.__enter__()
        lo, r_lo = prs.sb("lo", [128, NE], F32)
        hi, r_hi = prs.sb("hi", [128, NE], F32)
        mid, r_mid = prs.sb("mid", [128, NE], F32)
        cnt, r_cnt = prs.sb("cnt", [128, NE], F32)
        ge, r_ge = prs.sb("ge", [128, NE], F32)
        dl_, r_dl = prs.sb("dl_", [128, NE], F32)
        cmp, r_cmp = prs.sb("cmp", [128, NE, NT], F32)
        tot, r_tot = prs.sb("tot", [128, NT, NE], F32)
        offs, r_offs = prs.sb("offs", [128, NT, NE], F32)
        pcn = prs.ps("pcn", [128, NE], F32)
        ppos = prs.ps("ppos", [128, NT * NE], F32)
        T.op("dve", lambda e: e.memset(lo[:], 0.0), W=[r_lo])
        T.op("dve", lambda e: e.memset(hi[:], 1.0), W=[r_hi])
        for it in range(26):
            T.op("dve", lambda e: e.tensor_add(out=mid[:], in0=lo[:], in1=hi[:]), R=[r_lo, r_hi], W=[r_mid])
            T.op("dve", lambda e: e.tensor_scalar(out=mid[:], in0=mid[:], scalar1=0.5, scalar2=None, op0=ALU.mult), R=[r_mid], W=[r_mid])
            for ex in range(NE):
                T.op("dve", lambda e: e.tensor_scalar(out=cmp[:, ex, :], in0=aff[:, :, ex], scalar1=mid[:, ex:ex + 1], scalar2=None,
                                                      op0=ALU.is_ge), R=[r_aff, r_mid], W=[r_cmp])
            T.op("dve", lambda e: e.reduce_sum(out=cnt[:], in_=cmp[:], axis=AX.X), R=[r_cmp], W=[r_cnt])
            T.op("pe", lambda e: e.matmul(pcn[0][:], lhsT=ones_f[:], rhs=cnt[:], start=True, stop=True), R=[r_ones_f, r_cnt], W=[pcn[1]])
            T.op("dve", lambda e: e.tensor_scalar(out=ge[:], in0=pcn[0][:], scalar1=float(CAP), scalar2=None, op0=ALU.is_ge),
                 R=[pcn[1]], W=[r_ge])
            T.op("dve", lambda e: e.tensor_sub(out=dl_[:], in0=mid[:], in1=lo[:]), R=[r_mid, r_lo], W=[r_dl])
            T.op("dve", lambda e: e.tensor_mul(out=dl_[:], in0=dl_[:], in1=ge[:]), R=[r_dl, r_ge], W=[r_dl])
            T.op("dve", lambda e: e.tensor_add(out=lo[:], in0=lo[:], in1=dl_[:]), R=[r_lo, r_dl], W=[r_lo])
            T.op("dve", lambda e: e.tensor_sub(out=dl_[:], in0=hi[:], in1=mid[:]), R=[r_mid, r_hi], W=[r_dl])
            T.op("dve", lambda e: e.tensor_mul(out=dl_[:], in0=dl_[:], in1=ge[:]), R=[r_dl, r_ge], W=[r_dl])
            T.op("dve", lambda e: e.tensor_add(out=hi[:], in0=mid[:], in1=dl_[:]), R=[r_mid, r_dl], W=[r_hi])
        for ex in range(NE):
            T.op("dve", lambda e: e.tensor_scalar(out=msk[:, :, ex], in0=aff[:, :, ex], scalar1=lo[:, ex:ex + 1], scalar2=None,
                                                  op0=ALU.is_ge), R=[r_aff, r_lo], W=[r_msk])
        T.op("dve", lambda e: e.tensor_mul(out=gat[:], in0=msk[:], in1=aff[:, :, 0:NE]), R=[r_msk, r_aff], W=[r_gat])
        mflat = msk[:].rearrange("p t e -> p (t e)")
        T.op("pe", lambda e: e.matmul(ppos[0][:], lhsT=masks[:, 8, :], rhs=mflat, start=True, stop=True), R=[r_masks, r_msk], W=[ppos[1]])
        T.op("act", lambda e: e.copy(out=pos[:].rearrange("p t e -> p (t e)"), in_=ppos[0][:]), R=[ppos[1]], W=[r_pos])
        T.op("pe", lambda e: e.matmul(ppos[0][:], lhsT=ones_f[:], rhs=mflat, start=True, stop=True), R=[r_ones_f, r_msk], W=[ppos[1]])
        T.op("act", lambda e: e.copy(out=tot[:].rearrange("p t e -> p (t e)"), in_=ppos[0][:]), R=[ppos[1]], W=[r_tot])
        T.op("dve", lambda e: e.memset(offs[:, 0, :], 0.0), W=[r_offs])
        for tt in range(1, NT):
            T.op("dve", lambda e: e.tensor_add(out=offs[:, tt, :], in0=offs[:, tt - 1, :], in1=tot[:, tt - 1, :]), R=[r_offs, r_tot], W=[r_offs])
        T.op("dve", lambda e: e.tensor_add(out=pos[:], in0=pos[:], in1=offs[:]), R=[r_pos, r_offs], W=[r_pos])
        prs.__exit__(None, None, None)
        ye = [pl.sb("ye%d" % ex, [128, 4, D], BF16) for ex in range(NE)]
        with Pool_(P) as pa:
            x2t = [pa.sb("x2t%d" % i, [128, D], BF16) for i in range(1)] * 2
            sel = [pa.sb("sel%d" % i, [128, 512], BF16) for i in range(2)]
            xsT, r_xsT = pa.sb("xsT", [128, KC, 512], BF16)
            hT, r_hT = pa.sb("hT", [128, KC, 512], BF16)
            wgb = [pa.sb("wgb%d" % i, [128, KC, 128], BF16) for i in range(2)]
            wub = [pa.sb("wub%d" % i, [128, KC, 128], BF16) for i in range(2)]
            wdb = [pa.sb("wdb%d" % i, [128, KC, 128], BF16) for i in range(2)]
            sg, r_sg = pa.sb("sg", [128, 512], F32)
            su, r_su = pa.sb("su", [128, 512], F32)
            psel = [pa.ps("psel%d" % i, [128, 512], F32) for i in range(8)]
            for ex in range(NE):
                wge = wg[ex].rearrange("(kc p) n -> p kc n", p=128)
                wue = wu[ex].rearrange("(kc p) n -> p kc n", p=128)
                wde = wdn[ex].rearrange("(kc p) n -> p kc n", p=128)
                for half in range(2):
                    for tt in range(NT):
                        i = tt % 2
                        xt_t, xt_r = x2t[i]
                        s_t, s_r = sel[i]
                        T.dma("sp", xt_t[:], x2d[tt * 128:(tt + 1) * 128, :], R=[x2r], W=[xt_r])
                        T.op("dve", lambda e: e.tensor_scalar(out=s_t[:], in0=iota[:], scalar1=pos[:, tt, ex:ex + 1],
                                                              scalar2=msk[:, tt, ex:ex + 1], op0=ALU.is_equal, op1=ALU.mult),
                             R=[r_iota, r_pos, r_msk], W=[s_r])
                        for k in range(8):
                            kc = half * 8 + k
                            T.op("pe", lambda e: e.matmul(psel[k][0][:], lhsT=xt_t[:, kc * 128:(kc + 1) * 128], rhs=s_t[:],
                                                          start=(tt == 0), stop=(tt == NT - 1)), R=[xt_r, s_r], W=[psel[k][1]])
                    for k in range(8):
                        kc = half * 8 + k
                        T.op("act", lambda e: e.copy(out=xsT[:, kc, :], in_=psel[k][0][:]), R=[psel[k][1]], W=[r_xsT])
                for f in range(KC):
                    i = f % 2
                    g_t, g_r = wgb[i]
                    u_t, u_r = wub[i]
                    T.dma("pool", g_t[:], wge[:, :, f * 128:(f + 1) * 128], R=[P.dres["w_g%d" % l]], W=[g_r])
                    T.dma("pool", u_t[:], wue[:, :, f * 128:(f + 1) * 128], R=[P.dres["w_u%d" % l]], W=[u_r])
                    pg_t, pg_r = psel[(2 * f) % 8]
                    pu_t, pu_r = psel[(2 * f + 1) % 8]
                    for kc in range(KC):
                        T.op("pe", lambda e: e.matmul(pg_t[:], lhsT=g_t[:, kc, :], rhs=xsT[:, kc, :], start=(kc == 0), stop=(kc == KC - 1)),
                             R=[g_r, r_xsT], W=[pg_r])
                    for kc in range(KC):
                        T.op("pe", lambda e: e.matmul(pu_t[:], lhsT=u_t[:, kc, :], rhs=xsT[:, kc, :], start=(kc == 0), stop=(kc == KC - 1)),
                             R=[u_r, r_xsT], W=[pu_r])
                    T.op("act", lambda e: e.activation(out=sg[:], in_=pg_t[:], func=AF.Silu), R=[pg_r], W=[r_sg])
                    T.op("act", lambda e: e.copy(out=su[:], in_=pu_t[:]), R=[pu_r], W=[r_su])
                    T.op("dve", lambda e: e.tensor_mul(out=hT[:, f, :], in0=sg[:], in1=su[:]), R=[r_sg, r_su], W=[r_hT])
                for cb in range(16):
                    d_t, d_r = wdb[cb % 2]
                    T.dma("pool", d_t[:], wde[:, :, cb * 128:(cb + 1) * 128], R=[P.dres["w_d%d" % l]], W=[d_r])
                    for cs in range(4):
                        py_t, py_r = psel[(cb * 4 + cs) % 8]
                        for f in range(KC):
                            T.op("pe", lambda e: e.matmul(py_t[:, 0:128], lhsT=hT[:, f, cs * 128:(cs + 1) * 128], rhs=d_t[:, f, :],
                                                          start=(f == 0), stop=(f == KC - 1)), R=[r_hT, d_r], W=[py_r])
                        T.op("act", lambda e: e.copy(out=ye[ex][0][:, cs, cb * 128:(cb + 1) * 128], in_=py_t[:, 0:128]), R=[py_r], W=[ye[ex][1]])
        with Pool_(P) as pb:
            selg = [pb.sb("selg%d" % i, [128, 512], BF16) for i in range(2)]
            selT = [pb.sb("selT%d" % i, [128, 4, 128], BF16) for i in range(2)]
            yt = [pb.sb("yt%d" % i, [128, D], BF16) for i in range(2)]
            pT4 = [pb.ps("pT4_%d" % i, [128, 4, 128], BF16) for i in range(2)]
            py = [pb.ps("py%d" % i, [128, 512], F32) for i in range(4)]
            seq = [(tt, ex) for tt in range(NT) for ex in range(NE)]

            def prep(k):
                tt, ex = seq[k]
                i = k % 2
                s_t, s_r = selg[i]
                T.op("dve", lambda e: e.tensor_scalar(out=s_t[:], in0=iota[:], scalar1=pos[:, tt, ex:ex + 1],
                                                      scalar2=gat[:, tt, ex:ex + 1], op0=ALU.is_equal, op1=ALU.mult),
                     R=[r_iota, r_pos, r_gat], W=[s_r])
                p_t, p_r = pT4[i]
                for cs in range(4):
                    T.op("pe", lambda e: e.transpose(out=p_t[:, cs, :], in_=s_t[:, cs * 128:(cs + 1) * 128], identity=ident_bf[:]),
                         R=[s_r, r_ident_bf], W=[p_r])
                st_t, st_r = selT[i]
                T.op("act", lambda e: e.copy(out=st_t[:], in_=p_t[:]), R=[p_r], W=[st_r])

            prep(0)
            for k in range(len(seq)):
                tt, ex = seq[k]
                if k + 1 < len(seq):
                    prep(k + 1)
                st_t, st_r = selT[k % 2]
                for cb in range(4):
                    for cs in range(4):
                        T.op("pe", lambda e: e.matmul(py[cb][0][:], lhsT=st_t[:, cs, :], rhs=ye[ex][0][:, cs, cb * 512:(cb + 1) * 512],
                                                      start=(ex == 0 and cs == 0), stop=(ex == NE - 1 and cs == 3)),
                             R=[st_r, ye[ex][1]], W=[py[cb][1]])
                if ex == NE - 1:
                    y_t, y_r = yt[tt % 2]
                    for cb in range(4):
                        T.op("act", lambda e: e.copy(out=y_t[:, cb * 512:(cb + 1) * 512], in_=py[cb][0][:]), R=[py[cb][1]], W=[y_r])
                    T.dma("sp", P.dram["ypart"][tt * 128:(tt + 1) * 128, :], y_t[:], R=[y_r], W=[P.dres["ypart"]])
    pair_allgather(P, "ypart", "yg", S, 512)
    with Pool_(P) as pl:
        xt = [pl.sb("p5x%d" % i, [128, D], F32) for i in range(2)]
        ya = [pl.sb("p5a%d" % i, [128, 2, D], BF16) for i in range(2)]
        for tt in range(NT):
            i = tt % 2
            x_t, x_r = xt[i]
            a_t, a_r = ya[i]
            cidx, r0 = (tt * 128) // 512, (tt * 128) % 512
            T.dma("sp", x_t[:], P.dram["x1"][tt * 128:(tt + 1) * 128, :], R=[P.dres["x1"]], W=[x_r])
            for rk in range(2):
                T.dma("sp", a_t[:, rk, :], P.dram["yg"][cidx, rk, r0:r0 + 128, :], R=[P.dres["yg"]], W=[a_r])
            T.op("dve", lambda e: e.tensor_add(out=x_t[:], in0=x_t[:], in1=a_t[:, 0, :]), R=[x_r, a_r], W=[x_r])
            T.op("dve", lambda e: e.tensor_add(out=x_t[:], in0=x_t[:], in1=a_t[:, 1, :]), R=[x_r, a_r], W=[x_r])
            T.dma("sp", P.dram[xdst][tt * 128:(tt + 1) * 128, :], x_t[:], R=[x_r], W=[P.dres[xdst]])


def phase_final(P, C, xsrc):
    T = P.T
    epsc, r_epsc = C["epsc"]
    with Pool_(P) as pl:
        fw, r_fw = pl.sb("fw", [128, D], F32)
        T.dma("sp", fw[:], P.dram["fnw"].partition_broadcast(128), R=[P.dres["fnw"]], W=[r_fw])
        xt = [pl.sb("p6x%d" % i, [128, D], F32) for i in range(2)]
        junk, r_junk = pl.sb("p6j", [128, D], BF16)
        ss, r_ss = pl.sb("p6s", [128, 1], F32)
        for tt in range(NT):
            x_t, x_r = xt[tt % 2]
            T.dma("sp", x_t[:], P.dram[xsrc][tt * 128:(tt + 1) * 128, :], R=[P.dres[xsrc]], W=[x_r])
            T.op("dve", lambda e: e.memset(ss[:], 0.0), W=[r_ss])
            T.op("act", lambda e: e.activation(out=junk[:], in_=x_t[:], func=AF.Square, accum_out=ss[:]), R=[x_r], W=[r_junk, r_ss])
            T.op("act", lambda e: e.activation(out=ss[:], in_=ss[:], func=AF.Sqrt, bias=epsc[:], scale=1.0 / D), R=[r_ss, r_epsc], W=[r_ss])
            T.op("dve", lambda e: e.reciprocal(out=ss[:], in_=ss[:]), R=[r_ss], W=[r_ss])
            T.op("dve", lambda e: e.scalar_tensor_tensor(out=x_t[:], in0=x_t[:], scalar=ss[:], in1=fw[:], op0=ALU.mult, op1=ALU.mult),
                 R=[x_r, r_ss, r_fw], W=[x_r])
            T.dma("sp", P.dram["out"][tt * 128:(tt + 1) * 128, :], x_t[:], R=[x_r], W=[P.dres["out"]])


def build_program(layers=range(DEPTH), final=True, test_outputs=()):
    P = Prog(test_outputs=test_outputs)
    with P.es:
        declare_io(P, layers=layers)
        declare_scratch(P)
        with Pool_(P) as pc:
            C = load_consts(P, pc)
            src = "x"
            for l in layers:
                dst = "xa" if src != "xa" else "xb"
                P.marks = getattr(P, "marks", [])
                phase1(P, C, l, src)
                P.marks.append(("p1_%d" % l, dict(P.T.cnt)))
                phase_attn(P, C, l)
                P.marks.append(("attn_%d" % l, dict(P.T.cnt)))
                phase_gdn(P, C, l)
                P.marks.append(("gdn_%d" % l, dict(P.T.cnt)))
                phase3(P, C, l, src)
                P.marks.append(("p3_%d" % l, dict(P.T.cnt)))
                phase4(P, C, l, dst)
                P.marks.append(("p4_%d" % l, dict(P.T.cnt)))
                src = dst
            if final:
                phase_final(P, C, src)
        P.T.barrier()
    return P


_CACHE = {}


def kernel(**inputs):
    inputs = {k: np.asarray(v) for k, v in inputs.items()}
    if "P" not in _CACHE:
        _CACHE["P"] = build_program()
    P = _CACHE["P"]
    consts = host_consts()
    need = set(k for k in P.dram.keys())
    in_maps = []
    for c in range(8):
        m = core_inputs(inputs, c // 2, c % 2, consts)
        in_maps.append({k: v for k, v in m.items() if k in need})
    res = run_bass_kernel_spmd(P.nc, in_maps, core_ids=list(range(8)))
    out = np.stack([np.asarray(res.results[2 * b]["out"]) for b in range(4)], 0)
    return out.astype(np.float32)
```
